# Optimizing a Trainium2 kernel written in Bass

```python
import jax
import jax.numpy as jnp
from jax import lax
import numpy as np

D_MODEL = 1024
BATCH = 2
SEQ = 8192
DEPTH = 2

MIX_HALF = D_MODEL // 2
GMLP_GROUPS = 4
GMLP_DIM = MIX_HALF // GMLP_GROUPS
GMLP_CHUNK = 128
HGRN_HEADS = 4
HGRN_DK = 128
HGRN_DV = MIX_HALF // HGRN_HEADS
HGRN_CHUNK = 32
A_WIDTH = GMLP_GROUPS * GMLP_DIM
B_KWIDTH = HGRN_HEADS * HGRN_DK
B_VWIDTH = HGRN_HEADS * HGRN_DV
MIX_SPLITS = (A_WIDTH, A_WIDTH, B_KWIDTH, B_KWIDTH, B_VWIDTH, B_VWIDTH)
MIX_IN = 2 * A_WIDTH + 2 * B_KWIDTH + 2 * B_VWIDTH
MIX_OUT = A_WIDTH + B_VWIDTH
RWKV_HEAD = 64
RWKV_HEADS = D_MODEL // RWKV_HEAD
RWKV_DECAY_LORA = 64
RWKV_AAA_LORA = 64
RWKV_GATE_LORA = 128
D_FF = ((8 * D_MODEL // 3 + 127) // 128) * 128
N_EXPERTS = 8
TOP_K = 2
D_FF_EXPERT = D_FF // 2
N_EVEN = (DEPTH + 1) // 2
N_ODD = DEPTH // 2
RMS_EPS = 1e-6
LN_EPS = 1e-5
GN_EPS = 64e-5
F32 = jnp.float32

kernel_name = 'hybrid_gmlp_hgrn2_rwkv7_moe'


def rms_norm(x, g):
    xf = x.astype(F32)
    y = xf * lax.rsqrt(jnp.mean(xf * xf, axis=-1, keepdims=True) + RMS_EPS)
    return (y * g.astype(F32)).astype(x.dtype)


def swiglu(h, w_gate, w_up, w_down):
    return (jax.nn.silu(h @ w_gate) * (h @ w_up)) @ w_down


def gmlp_spatial_gate(u, v, norm_g, w_s, b_s):
    bsz, t, g, c = v.shape
    n = t // GMLP_CHUNK
    vf = v.astype(F32)
    mu = jnp.mean(vf, axis=-1, keepdims=True)
    var = jnp.mean(jnp.square(vf - mu), axis=-1, keepdims=True)
    vn = ((vf - mu) * lax.rsqrt(var + LN_EPS) * norm_g.astype(F32)).reshape(bsz, n, GMLP_CHUNK, g, c)
    causal = jnp.tril(jnp.ones((GMLP_CHUNK, GMLP_CHUNK), dtype=bool))
    w = jnp.where(causal[None], w_s.astype(F32), 0.0)
    mixed = jnp.einsum('gts,bnsgc->bntgc', w, vn) + b_s.astype(F32).T[None, None, :, :, None]
    return u * mixed.reshape(bsz, t, g, c).astype(u.dtype)


def hgrn2_chunkwise(q, k, v, log_f):
    bsz, t, h, dk = q.shape
    dv = v.shape[-1]
    c = HGRN_CHUNK
    n = t // c
    q, k, log_f = (a.reshape(bsz, n, c, h, dk) for a in (q, k, log_f))
    v = v.reshape(bsz, n, c, h, dv)
    b = jnp.cumsum(log_f, axis=2)
    b_end = b[:, :, -1:]
    q_dec = q * jnp.exp(b)
    scores = jnp.einsum('bnthk,bnshk->bnhts', q_dec, k * jnp.exp(-b))
    causal = jnp.tril(jnp.ones((c, c), dtype=bool))
    scores = jnp.where(causal, scores, 0.0)
    o_intra = jnp.einsum('bnhts,bnshv->bnthv', scores, v)
    inc = jnp.einsum('bnshk,bnshv->nbhkv', k * jnp.exp(b_end - b), v)
    chunk_decay = jnp.exp(b_end[:, :, 0]).transpose(1, 0, 2, 3)

    def step(s, inp):
        d, ds = inp
        return d[..., None] * s + ds, s

    s0 = jnp.zeros((bsz, h, dk, dv), F32)
    _, s_start = lax.scan(step, s0, (chunk_decay, inc))
    o_inter = jnp.einsum('bnthk,nbhkv->bnthv', q_dec, s_start)
    return (o_intra + o_inter).reshape(bsz, t, h, dv)


def even_mixer(h, w_in, w_out, gmlp_norm_g, gmlp_w_s, gmlp_b_s, lb, onorm_g):
    bsz, t, _ = h.shape
    z = h @ w_in
    cuts = np.cumsum(MIX_SPLITS)[:-1].tolist()
    zu, zv, zq, zf, zi, zg = jnp.split(z, cuts, axis=-1)
    u = jax.nn.gelu(zu).reshape(bsz, t, GMLP_GROUPS, GMLP_DIM)
    v = jax.nn.gelu(zv).reshape(bsz, t, GMLP_GROUPS, GMLP_DIM)
    y_a = gmlp_spatial_gate(u, v, gmlp_norm_g, gmlp_w_s, gmlp_b_s).reshape(bsz, t, A_WIDTH)
    f = lb + (1.0 - lb) * jax.nn.sigmoid(zf.astype(F32))

    def heads(a, d):
        return a.reshape(bsz, t, HGRN_HEADS, d)

    o = hgrn2_chunkwise(heads(jax.nn.silu(zq.astype(F32)), HGRN_DK), heads(1.0 - f, HGRN_DK),
                        heads(zi.astype(F32), HGRN_DV), heads(jnp.log(f), HGRN_DK))
    o = o * lax.rsqrt(jnp.mean(o * o, axis=-1, keepdims=True) + RMS_EPS) * onorm_g.astype(F32)
    o = o * jax.nn.silu(heads(zg.astype(F32), HGRN_DV))
    y = jnp.concatenate([y_a, o.reshape(bsz, t, B_VWIDTH).astype(h.dtype)], axis=-1)
    return y @ w_out


def rwkv7_time_mix(h, mix, w_r, w_k, w_v, w_o, w0, w1, w2, a0, a1, a2, g1, g2,
                   k_k, k_a, r_k, ln_g, ln_b):
    bsz, t, d = h.shape
    nh, hs = RWKV_HEADS, RWKV_HEAD
    dx = jnp.pad(h[:, :-1], ((0, 0), (1, 0), (0, 0))) - h
    xr, xw, xk, xv, xa, xg = (h + dx * mix[i] for i in range(6))
    r = xr @ w_r
    k = xk @ w_k
    v = xv @ w_v
    w_log = -jax.nn.softplus(-(w0 + jnp.tanh(xw @ w1) @ w2)) - 0.5
    a = jax.nn.sigmoid(a0 + (xa @ a1) @ a2)
    g = jax.nn.sigmoid(xg @ g1) @ g2

    def heads(z):
        return z.astype(F32).reshape(bsz, t, nh, hs)

    kk = heads(k * k_k)
    kk = kk / jnp.maximum(jnp.sqrt(jnp.sum(kk * kk, axis=-1, keepdims=True)), 1e-12)
    k = heads(k * (1.0 + (a - 1.0) * k_a))
    r, v, a = heads(r), heads(v), heads(a)
    decay = jnp.exp(-jnp.exp(heads(w_log)))

    def tm(z):
        return jnp.swapaxes(z, 0, 1)

    def step(s, inp):
        r_t, w_t, k_t, v_t, kk_t, b_t = inp
        s = (s * w_t[:, :, None, :]
             - jnp.einsum('bhvk,bhk->bhv', s, kk_t)[..., None] * b_t[:, :, None, :]
             + v_t[..., None] * k_t[:, :, None, :])
        return s, jnp.einsum('bhvk,bhk->bhv', s, r_t)

    s0 = jnp.zeros((bsz, nh, hs, hs), F32)
    _, y = lax.scan(step, s0, (tm(r), tm(decay), tm(k), tm(v), tm(kk), tm(kk * a)))
    y = tm(y)
    mu = jnp.mean(y, axis=-1, keepdims=True)
    var = jnp.mean(jnp.square(y - mu), axis=-1, keepdims=True)
    y = (y - mu) * lax.rsqrt(var + GN_EPS) * ln_g.astype(F32).reshape(nh, hs) + ln_b.astype(F32).reshape(nh, hs)
    y = y + jnp.sum(r * k * r_k.astype(F32), axis=-1, keepdims=True) * v
    return (y.reshape(bsz, t, d).astype(h.dtype) * g) @ w_o


def moe_swiglu(h, router, router_b, w_gate, w_up, w_down):
    logits = (h @ router).astype(F32) + router_b.astype(F32)
    top_logit, top_idx = lax.top_k(logits, TOP_K)
    top_w = jax.nn.softmax(top_logit, axis=-1)
    gates = jnp.sum(jax.nn.one_hot(top_idx, N_EXPERTS, dtype=F32) * top_w[..., None], axis=-2)
    y = jnp.zeros_like(h)
    for e in range(N_EXPERTS):
        y = y + gates[..., e:e + 1].astype(h.dtype) * swiglu(h, w_gate[e], w_up[e], w_down[e])
    return y


def _normal(key, shape, scale):
    return scale * jax.random.normal(key, shape, F32)


def setup_inputs(seed: int = 0) -> dict:
    key = jax.random.key(seed)
    ks = jax.random.split(key, 40)
    D = D_MODEL
    inp = {}
    inp['x'] = jax.random.normal(ks[0], (BATCH, SEQ, D), F32)
    inp['norm_mix_g'] = 1.0 + _normal(ks[1], (DEPTH, D), 0.01)
    inp['norm_ffn_g'] = 1.0 + _normal(ks[2], (DEPTH, D), 0.01)
    inp['norm_out_g'] = 1.0 + _normal(ks[3], (D,), 0.01)
    inp['mix_w_in'] = _normal(ks[4], (N_EVEN, D, MIX_IN), D ** -0.5)
    inp['mix_w_out'] = _normal(ks[5], (N_EVEN, MIX_OUT, D), MIX_OUT ** -0.5)
    inp['gmlp_norm_g'] = 1.0 + _normal(ks[6], (N_EVEN, GMLP_GROUPS, GMLP_DIM), 0.01)
    inp['gmlp_w_s'] = _normal(ks[7], (N_EVEN, GMLP_GROUPS, GMLP_CHUNK, GMLP_CHUNK), GMLP_CHUNK ** -0.5)
    inp['gmlp_b_s'] = 1.0 + _normal(ks[8], (N_EVEN, GMLP_GROUPS, GMLP_CHUNK), 0.01)
    inp['hgrn_lb_logits'] = _normal(ks[9], (N_EVEN + 1, B_KWIDTH), 0.5)
    inp['hgrn_onorm_g'] = 1.0 + _normal(ks[10], (N_EVEN, HGRN_DV), 0.01)
    inp['ffn_w_gate'] = _normal(ks[11], (N_EVEN, D, D_FF), D ** -0.5)
    inp['ffn_w_up'] = _normal(ks[12], (N_EVEN, D, D_FF), D ** -0.5)
    inp['ffn_w_down'] = _normal(ks[13], (N_EVEN, D_FF, D), D_FF ** -0.5)
    inp['rwkv_mix'] = jax.random.uniform(ks[14], (N_ODD, 6, D), F32)
    inp['rwkv_w_r'] = _normal(ks[15], (N_ODD, D, D), D ** -0.5)
    inp['rwkv_w_k'] = _normal(ks[16], (N_ODD, D, D), D ** -0.5)
    inp['rwkv_w_v'] = _normal(ks[17], (N_ODD, D, D), D ** -0.5)
    inp['rwkv_w_o'] = _normal(ks[18], (N_ODD, D, D), D ** -0.5)
    inp['rwkv_w0'] = -1.0 + _normal(ks[19], (N_ODD, D), 0.5)
    inp['rwkv_w1'] = _normal(ks[20], (N_ODD, D, RWKV_DECAY_LORA), D ** -0.5)
    inp['rwkv_w2'] = _normal(ks[21], (N_ODD, RWKV_DECAY_LORA, D), 0.1 * RWKV_DECAY_LORA ** -0.5)
    inp['rwkv_a0'] = _normal(ks[22], (N_ODD, D), 0.1)
    inp['rwkv_a1'] = _normal(ks[23], (N_ODD, D, RWKV_AAA_LORA), D ** -0.5)
    inp['rwkv_a2'] = _normal(ks[24], (N_ODD, RWKV_AAA_LORA, D), 0.1 * RWKV_AAA_LORA ** -0.5)
    inp['rwkv_g1'] = _normal(ks[25], (N_ODD, D, RWKV_GATE_LORA), D ** -0.5)
    inp['rwkv_g2'] = _normal(ks[26], (N_ODD, RWKV_GATE_LORA, D), RWKV_GATE_LORA ** -0.5)
    inp['rwkv_k_k'] = 0.85 + _normal(ks[27], (N_ODD, D), 0.05)
    inp['rwkv_k_a'] = 1.0 + _normal(ks[28], (N_ODD, D), 0.05)
    inp['rwkv_r_k'] = _normal(ks[29], (N_ODD, RWKV_HEADS, RWKV_HEAD), 0.1)
    inp['rwkv_ln_g'] = 1.0 + _normal(ks[30], (N_ODD, D), 0.01)
    inp['rwkv_ln_b'] = _normal(ks[31], (N_ODD, D), 0.01)
    inp['moe_router'] = _normal(ks[32], (N_ODD, D, N_EXPERTS), D ** -0.5)
    inp['moe_router_b'] = _normal(ks[33], (N_ODD, N_EXPERTS), 0.01)
    inp['moe_w_gate'] = _normal(ks[34], (N_ODD, N_EXPERTS, D, D_FF_EXPERT), D ** -0.5)
    inp['moe_w_up'] = _normal(ks[35], (N_ODD, N_EXPERTS, D, D_FF_EXPERT), D ** -0.5)
    inp['moe_w_down'] = _normal(ks[36], (N_ODD, N_EXPERTS, D_FF_EXPERT, D), D_FF_EXPERT ** -0.5)
    return inp


def reference(x, norm_mix_g, norm_ffn_g, norm_out_g,
              mix_w_in, mix_w_out, gmlp_norm_g, gmlp_w_s, gmlp_b_s, hgrn_lb_logits, hgrn_onorm_g,
              ffn_w_gate, ffn_w_up, ffn_w_down,
              rwkv_mix, rwkv_w_r, rwkv_w_k, rwkv_w_v, rwkv_w_o, rwkv_w0, rwkv_w1, rwkv_w2,
              rwkv_a0, rwkv_a1, rwkv_a2, rwkv_g1, rwkv_g2, rwkv_k_k, rwkv_k_a, rwkv_r_k,
              rwkv_ln_g, rwkv_ln_b,
              moe_router, moe_router_b, moe_w_gate, moe_w_up, moe_w_down):
    lower_bounds = jnp.cumsum(jax.nn.softmax(hgrn_lb_logits.astype(F32), axis=0), axis=0)
    h = x
    for layer in range(DEPTH):
        j = layer // 2
        if layer % 2 == 0:
            h = h + even_mixer(rms_norm(h, norm_mix_g[layer]), mix_w_in[j], mix_w_out[j],
                               gmlp_norm_g[j], gmlp_w_s[j], gmlp_b_s[j], lower_bounds[j], hgrn_onorm_g[j])
            h = h + swiglu(rms_norm(h, norm_ffn_g[layer]), ffn_w_gate[j], ffn_w_up[j], ffn_w_down[j])
        else:
            h = h + rwkv7_time_mix(rms_norm(h, norm_mix_g[layer]), rwkv_mix[j], rwkv_w_r[j], rwkv_w_k[j],
                                   rwkv_w_v[j], rwkv_w_o[j], rwkv_w0[j], rwkv_w1[j], rwkv_w2[j],
                                   rwkv_a0[j], rwkv_a1[j], rwkv_a2[j], rwkv_g1[j], rwkv_g2[j],
                                   rwkv_k_k[j], rwkv_k_a[j], rwkv_r_k[j], rwkv_ln_g[j], rwkv_ln_b[j])
            h = h + moe_swiglu(rms_norm(h, norm_ffn_g[layer]), moe_router[j], moe_router_b[j],
                               moe_w_gate[j], moe_w_up[j], moe_w_down[j])
    return rms_norm(h, norm_out_g)
```

```python
import contextlib
import numpy as np
import concourse.bass as bass
import concourse.mybir as mybir
from concourse.bass_utils import run_bass_kernel_spmd

F32 = mybir.dt.float32
BF16 = mybir.dt.bfloat16
I32 = mybir.dt.int32
AF = mybir.ActivationFunctionType
ALU = mybir.AluOpType
AX = mybir.AxisListType


class Trk:
    __slots__ = ("name", "w", "r", "bank")

    def __init__(self, name, bank=None):
        self.name = name
        self.w = []
        self.r = []
        self.bank = bank


class Bank:
    def __init__(self):
        self.last = {}


class Buf:
    def __init__(self, t, name):
        self.t = t
        self.trk = Trk(name)

    def __getitem__(self, idx):
        return self.t[idx]


class KB:
    ENG = ("pe", "act", "dve", "pool", "sp")

    def __init__(self, nc, same_engine_sync=True):
        self.nc = nc
        self.es = contextlib.ExitStack()
        self.sems = {}
        self.cnt = {}
        self.ops = {e: [] for e in self.ENG}
        self.known = {e: {} for e in self.ENG}
        self.same_engine_sync = same_engine_sync
        for e in self.ENG:
            self.sems[e] = self.es.enter_context(nc.semaphore("sem_" + e))
            self.cnt[e] = 0
        self.free_sems = []
        self.phase_keys = []
        self.nsem = 0
        self.pes = None
        self.prefix = ""

    def push(self, prefix=""):
        assert self.pes is None
        self.pes = contextlib.ExitStack()
        self.phase_keys = []
        self.prefix = prefix

    def barrier(self):
        allk = [(k_, v) for k_, v in self.cnt.items() if v > 0]
        for e in self.ENG:
            waits = []
            kn = self.known[e]
            for k_, v in allk:
                if k_ != e and kn.get(k_, 0) < v:
                    kn[k_] = v
                    waits.append((k_, v))
            if waits:
                self.ops[e].append((waits, None, None, 0))

    def pop(self):
        self.barrier()
        self.pes.close()
        self.pes = None
        for key in self.phase_keys:
            self.free_sems.append((self.sems[key], self.cnt[key]))
        self.phase_keys = []

    def sb(self, name, shape, dt, glob=False):
        name = self.prefix + name
        es = self.es if (glob or self.pes is None) else self.pes
        t = es.enter_context(self.nc.sbuf_tensor(name, list(shape), dt))
        return Buf(t, name)

    def ps(self, name, shape, dt=F32):
        name = self.prefix + name
        es = self.es if self.pes is None else self.pes
        t = es.enter_context(self.nc.psum_tensor(name, list(shape), dt))
        b = Buf(t, name)
        b.trk.bank = Bank()
        return b

    def trk(self, name, bank=None):
        return Trk(self.prefix + name, bank.trk.bank if bank is not None else None)

    def dsem(self, name):
        key = "d_" + name
        if key not in self.sems:
            if self.free_sems:
                h, c = self.free_sems.pop()
                self.sems[key] = h
                self.cnt[key] = c
            else:
                self.nsem += 1
                self.sems[key] = self.es.enter_context(self.nc.semaphore("dsem%d" % self.nsem))
                self.cnt[key] = 0
            if self.pes is not None:
                self.phase_keys.append(key)
        return key

    @staticmethod
    def _t(b):
        return b.trk if isinstance(b, Buf) else b

    def _deps(self, eng, reads, writes):
        need = {}

        def add(tok):
            k, v = tok
            if k == eng and (eng == "pe" or not self.same_engine_sync):
                return
            if need.get(k, 0) < v:
                need[k] = v
        for b in reads:
            for tok in self._t(b).w:
                add(tok)
        for b in writes:
            t = self._t(b)
            for tok in t.w:
                add(tok)
            for tok in t.r:
                add(tok)
        for b in list(reads) + list(writes):
            bk = self._t(b).bank
            if bk is not None:
                for e2, v in bk.last.items():
                    if e2 != eng and need.get(e2, 0) < v:
                        need[e2] = v
        waits = []
        kn = self.known[eng]
        for k, v in need.items():
            if kn.get(k, 0) < v:
                kn[k] = v
                waits.append((k, v))
        return waits

    def _commit(self, tok, reads, writes, dma_group=False):
        for b in list(reads) + list(writes):
            bk = self._t(b).bank
            if bk is not None and not tok[0].startswith("d_"):
                bk.last[tok[0]] = tok[1]
        for b in reads:
            t = self._t(b)
            t.r = [x for x in t.r if x[0] != tok[0]] + [tok]
        for b in writes:
            t = self._t(b)
            if dma_group and t.w and all(x[0].startswith("d_") for x in t.w) and not t.r:
                t.w = [x for x in t.w if x[0] != tok[0]] + [tok]
            else:
                t.w = [tok]
                t.r = []

    def op(self, eng, fn, reads=(), writes=()):
        waits = self._deps(eng, reads, writes)
        self.cnt[eng] += 1
        tok = (eng, self.cnt[eng])
        self.ops[eng].append((waits, fn, eng, 1))
        self._commit(tok, reads, writes)

    def dma(self, eng, out, in_, reads=(), writes=(), sem=None, **kw):
        if sem is None:
            b = (list(writes) + list(reads))[0]
            sem = self._t(b).name
        elif not sem.startswith(self.prefix):
            sem = self.prefix + sem
        key = self.dsem(sem)
        waits = self._deps_dma(eng, reads, writes)
        self.cnt[key] += 16
        tok = (key, self.cnt[key])
        self.ops[eng].append((waits, lambda e: e.dma_start(out=out(e) if callable(out) else out,
                                                            in_=in_(e) if callable(in_) else in_, **kw), key, 16))
        self._commit(tok, reads, writes, dma_group=True)

    def coll(self, fn, name, reads=(), writes=()):
        key = self.dsem("cc_" + name)
        waits = self._deps_dma("pool", reads, writes)
        self.cnt[key] += 1
        tok = (key, self.cnt[key])
        self.ops["pool"].append((waits, fn, key, 1))
        self._commit(tok, reads, writes, dma_group=True)

    def _deps_dma(self, eng, reads, writes):
        need = {}

        def add(tok):
            k, v = tok
            if need.get(k, 0) < v:
                need[k] = v
        for b in reads:
            for tok in self._t(b).w:
                add(tok)
        for b in writes:
            t = self._t(b)
            if not (t.w and all(x[0].startswith("d_") for x in t.w) and not t.r):
                for tok in t.w:
                    add(tok)
            for tok in t.r:
                add(tok)
        waits = []
        kn = self.known[eng]
        for k, v in need.items():
            if kn.get(k, 0) < v:
                kn[k] = v
                waits.append((k, v))
        return waits

    def wait_all(self, eng, bufs):
        need = {}
        for b in bufs:
            t = self._t(b)
            for k, v in t.w + t.r:
                if need.get(k, 0) < v:
                    need[k] = v
        waits = [(k, v) for k, v in need.items()]
        self.ops[eng].append((waits, None, None, 0))

    def emit(self):
        nc = self.nc
        sems = self.sems
        ops = self.ops
        with nc.Block() as block:
            def run(e, lst):
                for waits, fn, key, inc in lst:
                    for k, v in waits:
                        e.wait_ge(sems[k], v)
                    if fn is not None:
                        fn(e).then_inc(sems[key], inc)

            @block.tensor
            def _(e):
                run(e, ops["pe"])

            @block.scalar
            def _(e):
                run(e, ops["act"])

            @block.vector
            def _(e):
                run(e, ops["dve"])

            @block.gpsimd
            def _(e):
                run(e, ops["pool"])

            @block.sync
            def _(e):
                self.pid_sp = e.partition_id() % 4
                run(e, ops["sp"])

    def close(self):
        self.es.close()


def _act(self, out, in_, func, R, W, scale=None, bias=None, accum=None):
    kw = {}
    if scale is not None:
        kw["scale"] = scale
    if bias is not None:
        kw["bias"] = bias
    if accum is not None:
        kw["accum_out"] = accum
    self.op("act", lambda e: e.activation(out=out, in_=in_, func=func, **kw), reads=R, writes=W)


def _tt(self, eng, out, in0, in1, op, R, W):
    self.op(eng, lambda e: e.tensor_tensor(out=out, in0=in0, in1=in1, op=op), reads=R, writes=W)


def _ts(self, eng, out, in0, s1, s2, op0, op1, R, W):
    if op1 is None:
        self.op(eng, lambda e: e.tensor_scalar(out=out, in0=in0, scalar1=s1, scalar2=None, op0=op0), reads=R, writes=W)
    else:
        self.op(eng, lambda e: e.tensor_scalar(out=out, in0=in0, scalar1=s1, scalar2=s2, op0=op0, op1=op1), reads=R, writes=W)


def _stt(self, out, in0, scalar, in1, op0, op1, R, W):
    self.op("dve", lambda e: e.scalar_tensor_tensor(out=out, in0=in0, scalar=scalar, in1=in1, op0=op0, op1=op1),
            reads=R, writes=W)


def _mm(self, out, lhsT, rhs, start, stop, R, W):
    self.op("pe", lambda e: e.matmul(out, lhsT=lhsT, rhs=rhs, start=start, stop=stop), reads=R, writes=W)


def _tr(self, out, in_, ident, R, W):
    self.op("pe", lambda e: e.transpose(out=out, in_=in_, identity=ident), reads=R, writes=W)


def _cp(self, eng, out, in_, R, W):
    if eng == "act":
        self.op("act", lambda e: e.copy(out=out, in_=in_), reads=R, writes=W)
    else:
        self.op(eng, lambda e: e.tensor_copy(out=out, in_=in_), reads=R, writes=W)


def _recip(self, out, in_, R, W):
    self.op("dve", lambda e: e.reciprocal(out=out, in_=in_), reads=R, writes=W)


KB.act = _act
KB.tt = _tt
KB.ts = _ts
KB.stt = _stt
KB.mm = _mm
KB.tr = _tr
KB.cp = _cp
KB.recip = _recip


D = 1024
NT = 16
TOK = 2048
RMS_EPS = 1e-6


def make_ident(k, name="ident32", dt=F32):
    ident = k.sb(name, [128, 128], dt)
    k.op("pool", lambda e: e.memset(ident[:], 0.0), writes=[ident])
    k.op("pool", lambda e: e.affine_select(out=ident[:], in_=ident[:], pattern=[[-1, 128]],
                                           compare_op=ALU.not_equal, fill=1.0, base=0,
                                           channel_multiplier=1), reads=[ident], writes=[ident])
    return ident


T = 8192
LN_EPS = 1e-5
GN_EPS = 64e-5
CW = 0.6065306597126334


def phase_mix0(nc, k, x, win, gcol, wsT, gng, bsc, lbl, ong, y_loc, yloc_t, gather):
    ntiles = 64
    ident = make_ident(k)
    identb = k.sb("identb", [128, 128], BF16)
    k.cp("dve", identb[:], ident[:], [ident], [identb])
    winb = k.sb("winb", [128, 8, 768], BF16)
    g1 = k.sb("g1", [128, 8], F32)
    wsf = k.sb("wsf", [128, 128], F32)
    wsb = k.sb("wsb", [128, 128], BF16)
    gnb = k.sb("gnb", [128, 128], F32)
    onb = k.sb("onb", [128, 128], F32)
    bs = k.sb("bs", [128, 1], F32)
    lb2 = k.sb("lb2", [128, 2], F32)
    lbd = k.sb("lbd", [128, 1], F32)
    lb = k.sb("lb", [128, 1], F32)
    oml = k.sb("oml", [128, 1], F32)
    maskT = k.sb("maskT", [128, 128], F32)
    rmask = k.sb("rmask", [128, 128], F32)
    epsr = k.sb("epsr", [128, 1], F32)
    epsl = k.sb("epsl", [128, 1], F32)
    S32 = k.sb("S32", [128, 128], F32)
    Sbf = [k.sb(f"Sbf{i}", [128, 128], BF16) for i in range(4)]

    win_v = win.rearrange("(c p) f -> p c f", p=128)
    for c in range(0, 8, 2):
        k.dma("pool", winb[:, c:c + 2, :], win_v[:, c:c + 2, :], writes=[winb])
    k.dma("sp", g1[:], gcol, writes=[g1])
    k.dma("sp", wsf[:], wsT, writes=[wsf])
    k.dma("sp", gnb[:], gng.partition_broadcast(128), writes=[gnb])
    k.dma("sp", onb[:], ong.partition_broadcast(128), writes=[onb])
    k.dma("sp", bs[:], bsc, writes=[bs])
    k.dma("sp", lb2[:], lbl, writes=[lb2])

    k.op("dve", lambda e: e.memset(epsr[:], RMS_EPS), writes=[epsr])
    k.op("dve", lambda e: e.memset(epsl[:], LN_EPS), writes=[epsl])
    k.op("dve", lambda e: e.memset(S32[:], 0.0), writes=[S32])
    k.op("dve", lambda e: e.memset(Sbf[0][:], 0.0), writes=[Sbf[0]])
    k.op("pool", lambda e: e.affine_select(out=wsf[:], in_=wsf[:], pattern=[[1, 128]], compare_op=ALU.is_ge,
                                           fill=0.0, base=0, channel_multiplier=-1), reads=[wsf], writes=[wsf])
    k.cp("dve", wsb[:], wsf[:], [wsf], [wsb])
    k.op("pool", lambda e: e.memset(maskT[:], 1.0), writes=[maskT])
    k.op("pool", lambda e: e.affine_select(out=maskT[:], in_=maskT[:], pattern=[[1, 128]], compare_op=ALU.is_ge,
                                           fill=0.0, base=0, channel_multiplier=-1), reads=[maskT], writes=[maskT])
    for n in range(1, 4):
        k.op("pool", lambda e, n=n: e.affine_select(out=maskT[:, 32 * n:32 * n + 32], in_=maskT[:, 32 * n:32 * n + 32],
                                                    pattern=[[0, 32]], compare_op=ALU.is_ge, fill=0.0,
                                                    base=-32 * n, channel_multiplier=1),
             reads=[maskT], writes=[maskT])
    k.op("pool", lambda e: e.memset(rmask[:], 1.0), writes=[rmask])
    for n in range(4):
        k.op("pool", lambda e, n=n: e.memset(rmask[:, 32 * n:32 * n + 1], 0.0), reads=[rmask], writes=[rmask])
    rm3 = k.sb("rm3", [128, 1], F32)
    k.op("pool", lambda e: e.memset(rm3[:], 1.0), writes=[rm3])
    k.op("pool", lambda e: e.affine_select(out=rm3[:], in_=rm3[:], pattern=[[0, 1]], compare_op=ALU.is_ge, fill=0.0,
                                           base=-96, channel_multiplier=1), reads=[rm3], writes=[rm3])
    qdec3 = k.sb("qdec3", [128, 64], BF16)
    k.op("pool", lambda e: e.memset(qdec3[:], 0.0), writes=[qdec3])
    kend3 = k.sb("kend3", [128, 128], BF16)
    k.tt("dve", lbd[:], lb2[:, 0:1], lb2[:, 1:2], ALU.subtract, [lb2], [lbd])
    k.act(lb[:], lbd[:], AF.Sigmoid, [lbd], [lb])
    k.ts("dve", oml[:], lb[:], -1.0, 1.0, ALU.mult, ALU.add, [lb], [oml])

    xt = [k.sb(f"xt{i}", [128, D], F32) for i in range(3)]
    junk = k.sb("junk", [128, D], F32)
    xn32 = [k.sb(f"xn32_{i}", [128, D], F32) for i in range(2)]
    xnT = [k.sb(f"xnT{i}", [128, 8, 128], BF16) for i in range(2)]
    ss = k.sb("ss", [128, 1], F32)
    sd = k.sb("sd", [128, 1], F32)
    rstd = k.sb("rstd", [128, 1], F32)
    uv = k.sb("uv", [128, 256], F32)
    bst = k.sb("bst", [128, 6], F32)
    mv = k.sb("mv", [128, 2], F32)
    sdv = k.sb("sdv", [128, 1], F32)
    rv = k.sb("rv", [128, 1], F32)
    vn = k.sb("vn", [128, 128], F32)
    vnb = k.sb("vnb", [128, 128], BF16)
    ycat = k.sb("ycat", [128, 256], F32)
    sigT = k.sb("sigT", [128, 128], F32)
    fT = k.sb("fT", [128, 128], F32)
    logfT = k.sb("logfT", [128, 128], F32)
    bT = k.sb("bT", [128, 128], F32)
    E = k.sb("E", [128, 128], F32)
    Einv = k.sb("Einv", [128, 128], F32)
    sq = k.sb("sq", [128, 128], F32)
    qdec = k.sb("qdec", [128, 128], BF16)
    omf = k.sb("omf", [128, 128], F32)
    kdec32 = k.sb("kdec32", [128, 128], F32)
    kdecb = k.sb("kdecb", [128, 128], BF16)
    dcol = k.sb("dcol", [128, 4], F32)
    kendT = k.sb("kendT", [128, 128], BF16)
    kend = k.sb("kend", [128, 128], BF16)
    vb = k.sb("vb", [128, 128], BF16)
    scT = k.sb("scT", [128, 128], BF16)
    sso = k.sb("sso", [128, 1], F32)
    sdo = k.sb("sdo", [128, 1], F32)
    ro = k.sb("ro", [128, 1], F32)
    on = k.sb("on", [128, 128], F32)
    sgt = k.sb("sgt", [128, 128], F32)
    junk2 = k.sb("junk2", [128, 128], F32)
    yTb = [k.sb(f"yTb{i}", [128, 2, 512], BF16) for i in range(2)]

    pt = k.ps("pt", [128, D], F32)
    ptok = k.ps("ptok", [128, 512], F32)
    pqf = k.ps("pqf", [128, 512], F32)
    pE = k.ps("pE", [128, 512], F32)
    pEb = k.ps("pEb", [128, 1024], BF16)
    po = k.ps("po", [128, 512], F32)
    pinc = [k.ps(f"pinc{i}", [128, 512], F32) for i in range(1)]
    pm_t, psc_t, pTy_t = k.trk("pm", pE), k.trk("psc", pE), k.trk("pTy", pE)

    x_v = x.rearrange("(n p) d -> n p d", p=128)
    scur = 0
    for i in range(ntiles):
        xb = xt[i % 3]
        k.dma("sp", xb[:], x_v[i], writes=[xb])
        k.act(junk[:], xb[:], AF.Square, [xb], [junk, ss], accum=ss[:])
        k.act(sd[:], ss[:], AF.Sqrt, [ss, epsr], [sd], scale=1.0 / D, bias=epsr[:])
        k.recip(rstd[:], sd[:], [sd], [rstd])
        xn = xn32[i % 2]
        k.act(xn[:], xb[:], AF.Copy, [xb, rstd], [xn], scale=rstd[:])
        for dc in range(8):
            k.tr(pt[:, dc * 128:(dc + 1) * 128], xn[:, dc * 128:(dc + 1) * 128], ident[:], [xn, ident], [pt])
        xT = xnT[i % 2]
        for dc in range(8):
            k.ts("dve", xT[:, dc, :], pt[:, dc * 128:(dc + 1) * 128], g1[:, dc:dc + 1], None, ALU.mult, None,
                 [pt, g1], [xT])
        for dc in range(8):
            k.mm(ptok[:], xT[:, dc, :], winb[:, dc, 0:512], dc == 0, dc == 7, [xT, winb], [ptok])
        for dc in range(8):
            k.mm(pqf[:, 0:128], winb[:, dc, 512:640], xT[:, dc, :], dc == 0, dc == 7, [xT, winb], [pqf])
        for dc in range(8):
            k.mm(pqf[:, 128:256], winb[:, dc, 640:768], xT[:, dc, :], dc == 0, dc == 7, [xT, winb], [pqf])
        k.act(uv[:], ptok[:, 0:256], AF.Gelu_apprx_tanh, [ptok], [uv])
        k.op("dve", lambda e: e.bn_stats(out=bst[:], in_=uv[:, 128:256]), reads=[uv], writes=[bst])
        k.op("dve", lambda e: e.bn_aggr(out=mv[:], in_=bst[:]), reads=[bst], writes=[mv])
        k.act(sdv[:], mv[:, 1:2], AF.Sqrt, [mv, epsl], [sdv], scale=1.0, bias=epsl[:])
        k.recip(rv[:], sdv[:], [sdv], [rv])
        k.ts("dve", vn[:], uv[:, 128:256], mv[:, 0:1], rv[:, 0:1], ALU.subtract, ALU.mult, [uv, mv, rv], [vn])
        k.tt("pool", vnb[:], vn[:], gnb[:], ALU.mult, [vn, gnb], [vnb])
        k.mm(pE[:, 0:128], wsb[:], vnb[:], True, True, [wsb, vnb], [pm_t])
        k.stt(ycat[:, 0:128], pE[:, 0:128], bs[:, 0:1], uv[:, 0:128], ALU.add, ALU.mult, [pm_t, bs, uv], [ycat])
        k.act(sigT[:], pqf[:, 128:256], AF.Sigmoid, [pqf], [sigT])
        k.act(sq[:], pqf[:, 0:128], AF.Silu, [pqf], [sq])
        k.act(sgt[:], ptok[:, 384:512], AF.Silu, [ptok], [sgt])
        k.cp("act", vb[:], ptok[:, 256:384], [ptok], [vb])
        k.ts("dve", fT[:], sigT[:], oml[:, 0:1], lb[:, 0:1], ALU.mult, ALU.add, [sigT, oml, lb], [fT])
        k.act(logfT[:], fT[:], AF.Ln, [fT], [logfT])
        k.op("dve", lambda e: e.tensor_tensor_scan(out=bT[:], data0=rmask[:], data1=logfT[:], initial=0.0,
                                                   op0=ALU.mult, op1=ALU.add), reads=[rmask, logfT], writes=[bT])
        k.act(E[:], bT[:], AF.Exp, [bT], [E])
        k.act(Einv[:], bT[:], AF.Exp, [bT], [Einv], scale=-1.0)
        k.op("act", lambda e: e.activation(out=dcol[:], in_=bT[:, 31:128:32], func=AF.Exp), reads=[bT], writes=[dcol])
        k.tt("dve", qdec[:], sq[:], E[:], ALU.mult, [sq, E], [qdec])
        k.cp("pool", qdec3[:, 32:64], qdec[:, 96:128], [qdec], [qdec3])
        k.ts("pool", omf[:], fT[:], -1.0, 1.0, ALU.mult, ALU.add, [fT], [omf])
        k.tt("dve", kdec32[:], omf[:], Einv[:], ALU.mult, [omf, Einv], [kdec32])
        k.cp("pool", kdecb[:], kdec32[:], [kdec32], [kdecb])
        for n in range(4):
            k.ts("dve", kendT[:, 32 * n:32 * n + 32], kdec32[:, 32 * n:32 * n + 32], dcol[:, n:n + 1], None, ALU.mult, None,
                 [kdec32, dcol], [kendT])
        k.tr(pEb[:, 0:128], kendT[:], identb[:], [kendT, identb], [pEb])
        k.cp("act", kend[:], pEb[:, 0:128], [pEb], [kend])
        k.ts("dve", kend3[:], pEb[:, 0:128], rm3[:, 0:1], None, ALU.mult, None, [pEb, rm3], [kend3])
        k.mm(pE[:, 128:256], kdecb[:], qdec[:], True, True, [kdecb, qdec], [psc_t])
        k.tt("dve", scT[:], pE[:, 128:256], maskT[:], ALU.mult, [psc_t, maskT], [scT])
        k.mm(po[:, 0:128], scT[:], vb[:], True, False, [scT, vb], [po])
        for n in range(4):
            sb_cur = Sbf[scur]
            pi_ = pinc[0]
            if n < 3:
                k.mm(po[32 * n:32 * n + 32, 0:128], qdec[:, 32 * n:32 * n + 32], sb_cur[:], False, False,
                     [qdec, sb_cur], [po])
                k.mm(pi_[:, 0:128], kend[32 * n:32 * n + 32, :], vb[32 * n:32 * n + 32, :], True, True, [kend, vb], [pi_])
            else:
                k.mm(po[64:128, 0:128], qdec3[:, 0:64], sb_cur[:], False, True, [qdec3, sb_cur], [po])
                k.mm(pi_[:, 0:128], kend3[64:128, :], vb[64:128, :], True, True, [kend3, vb], [pi_])
            k.stt(S32[:], S32[:], dcol[:, n:n + 1], pi_[:, 0:128], ALU.mult, ALU.add, [S32, dcol, pi_], [S32])
            scur = (scur + 1) % 4
            k.cp("act", Sbf[scur][:], S32[:], [S32], [Sbf[scur]])
        k.act(junk2[:], po[:, 0:128], AF.Square, [po], [junk2, sso], accum=sso[:])
        k.act(sdo[:], sso[:], AF.Sqrt, [sso, epsr], [sdo], scale=1.0 / 128, bias=epsr[:])
        k.recip(ro[:], sdo[:], [sdo], [ro])
        k.stt(on[:], po[:, 0:128], ro[:, 0:1], onb[:], ALU.mult, ALU.mult, [po, ro, onb], [on])
        k.tt("dve", ycat[:, 128:256], on[:], sgt[:], ALU.mult, [on, sgt], [ycat])
        for c in range(2):
            k.tr(pE[:, 256 + c * 128:256 + (c + 1) * 128], ycat[:, c * 128:(c + 1) * 128], ident[:], [ycat, ident], [pTy_t])
        yb = yTb[(i // 4) % 2]
        j = i % 4
        k.op("act", lambda e, yb=yb, j=j: e.copy(out=yb[:, :, j * 128:(j + 1) * 128],
                                                in_=pE[:, 256:512].rearrange("p (c t) -> p c t", c=2)),
             reads=[pTy_t], writes=[yb])
        if j == 3:
            qr = i // 16
            off = ((i % 16) // 4) * 512
            k.dma("sp", y_loc[qr].rearrange("(c p) t -> p c t", p=128)[:, :, off:off + 512], yb[:, :, :], reads=[yb],
                  writes=[yloc_t[qr]], sem=f"yTb{(i // 4) % 2}")
            if i % 16 == 15:
                gather(qr)


def phase_rwkv(nc, k, hn_all, hnall_t, mixc, wproj, w2, a2, g2, cvec, lng, lnb, y_loc, yloc_t, gather):
    ntiles = 64
    ident = make_ident(k)
    identb = k.sb("identb", [128, 128], BF16)
    k.cp("dve", identb[:], ident[:], [ident], [identb])
    ident4 = k.sb("ident4", [128, 4, 128], F32)
    for h in range(4):
        k.cp("pool", ident4[:, h, :], ident[:], [ident], [ident4])

    mx6 = k.sb("mx6", [128, 8, 6], F32)
    omx6 = k.sb("omx6", [128, 8, 6], F32)
    k.dma("sp", mx6[:], mixc, writes=[mx6])
    k.ts("dve", omx6[:], mx6[:], -1.0, 1.0, ALU.mult, ALU.add, [mx6], [omx6])
    Wa = k.sb("Wa", [128, 8, 1024], BF16)
    Wb = k.sb("Wb", [128, 8, 1024], BF16)
    wst = [k.sb(f"wst{i}", [128, 1024], F32) for i in range(2)]
    blocks = [(0, 256, 0), (256, 512, 2), (512, 768, 3), (768, 832, 1), (832, 896, 4), (896, 1024, 5)]
    wp_v = wproj.rearrange("(c p) f -> p c f", p=128)
    for dc in range(8):
        st = wst[dc % 2]
        k.dma("sp", st[:], wp_v[:, dc, :], writes=[st])
        for (c0, c1, mi) in blocks:
            k.ts("dve", Wb[:, dc, c0:c1], st[:, c0:c1], mx6[:, dc, mi:mi + 1], None, ALU.mult, None, [st, mx6], [Wb])
            k.ts("pool", Wa[:, dc, c0:c1], st[:, c0:c1], omx6[:, dc, mi:mi + 1], None, ALU.mult, None, [st, omx6], [Wa])
    w2b = k.sb("w2b", [128, 256], BF16)
    a2b = k.sb("a2b", [128, 256], BF16)
    g2b = k.sb("g2b", [128, 256], BF16)
    k.dma("pool", w2b[0:64, :], w2, writes=[w2b])
    k.dma("pool", a2b[64:128, :], a2, writes=[a2b])
    k.dma("pool", g2b[:], g2, writes=[g2b])
    cv = k.sb("cv", [128, 2, 5], F32)
    k.dma("sp", cv[:], cvec, writes=[cv])
    omka = k.sb("omka", [128, 2], F32)
    k.ts("dve", omka[:], cv[:, :, 3], -1.0, 1.0, ALU.mult, ALU.add, [cv], [omka])
    lngb = k.sb("lngb", [128, 256], F32)
    lnbb = k.sb("lnbb", [128, 256], F32)
    k.dma("sp", lngb[:], lng.partition_broadcast(128), writes=[lngb])
    k.dma("sp", lnbb[:], lnb.partition_broadcast(128), writes=[lnbb])

    def mk_mask(name, strict):
        m = k.sb(name, [128, 128], F32)
        k.op("pool", lambda e: e.memset(m[:], 1.0), writes=[m])
        k.op("pool", lambda e: e.affine_select(out=m[:], in_=m[:], pattern=[[1, 128]],
                                               compare_op=ALU.is_gt if strict else ALU.is_ge, fill=0.0, base=0,
                                               channel_multiplier=-1), reads=[m], writes=[m])
        k.op("pool", lambda e: e.affine_select(out=m[:, 64:128], in_=m[:, 64:128], pattern=[[0, 64]],
                                               compare_op=ALU.is_ge, fill=0.0, base=-64, channel_multiplier=1),
             reads=[m], writes=[m])
        return m
    maskS = mk_mask("maskS", True)
    maskI = mk_mask("maskI", False)
    maskL = k.sb("maskL", [128, 128], F32)
    k.op("pool", lambda e: e.memset(maskL[:], 1.0), writes=[maskL])
    k.op("pool", lambda e: e.affine_select(out=maskL[:], in_=maskL[:], pattern=[[-1, 128]], compare_op=ALU.is_gt,
                                           fill=0.0, base=0, channel_multiplier=1), reads=[maskL], writes=[maskL])
    k.op("pool", lambda e: e.affine_select(out=maskL[:, 0:64], in_=maskL[:, 0:64], pattern=[[0, 64]],
                                           compare_op=ALU.is_ge, fill=0.0, base=63, channel_multiplier=-1),
         reads=[maskL], writes=[maskL])
    maskL4 = k.sb("maskL4", [128, 4, 128], F32)
    for h in range(4):
        k.cp("pool", maskL4[:, h, :], maskL[:], [maskL], [maskL4])
    mask4 = k.sb("mask4", [128, 512], F32)
    k.cp("pool", mask4[:, 0:128], maskS[:], [maskS], [mask4])
    k.cp("pool", mask4[:, 128:256], maskI[:], [maskI], [mask4])
    k.cp("pool", mask4[:, 256:384], maskS[:], [maskS], [mask4])
    k.ts("pool", mask4[:, 384:512], maskI[:], -1.0, None, ALU.mult, None, [maskI], [mask4])
    rmask = k.sb("rmask", [128, 2, 128], F32)
    k.op("pool", lambda e: e.memset(rmask[:], 1.0), writes=[rmask])
    for p in range(2):
        for c in range(2):
            k.op("pool", lambda e, p=p, c=c: e.memset(rmask[:, p, 64 * c:64 * c + 1], 0.0), reads=[rmask], writes=[rmask])
    bones = k.sb("bones", [128, 128], BF16)
    k.op("pool", lambda e: e.memset(bones[:], 0.0), writes=[bones])
    k.op("pool", lambda e: e.memset(bones[0:64, 0:64], 1.0), reads=[bones], writes=[bones])
    k.op("pool", lambda e: e.memset(bones[64:128, 64:128], 1.0), reads=[bones], writes=[bones])
    epsg = k.sb("epsg", [128, 1], F32)
    k.op("dve", lambda e: e.memset(epsg[:], GN_EPS), writes=[epsg])
    epsk = k.sb("epsk", [128, 1], F32)
    k.op("dve", lambda e: e.memset(epsk[:], 1e-24), writes=[epsk])

    M32 = k.sb("M32", [128, 2, 64], F32)
    Mb = [k.sb(f"Mb{i}", [128, 4, 64], BF16) for i in range(2)]
    k.op("dve", lambda e: e.memset(M32[:], 0.0), writes=[M32])
    k.op("dve", lambda e: e.memset(Mb[0][:], 0.0), writes=[Mb[0]])
    hmask = k.sb("hmask", [128, 2], F32)
    k.op("pool", lambda e: e.memset(hmask[:], 0.0), writes=[hmask])
    k.op("pool", lambda e: e.memset(hmask[0:64, 0:1], 1.0), reads=[hmask], writes=[hmask])
    k.op("pool", lambda e: e.memset(hmask[64:128, 1:2], 1.0), reads=[hmask], writes=[hmask])
    mcur = 0

    hx = [k.sb(f"hx{i}", [128, 8, 128], BF16) for i in range(2)]
    hs = k.sb("hs", [128, 8, 128], BF16)
    k.op("pool", lambda e: e.memset(hs[:, :, 0:1], 0.0), writes=[hs])
    l1 = k.sb("l1", [128, 128], BF16)
    sg1 = k.sb("sg1", [128, 128], BF16)
    sigw = k.sb("sigw", [128, 2, 128], F32)
    a_ = k.sb("a_", [128, 2, 128], F32)
    kku = k.sb("kku", [128, 2, 128], F32)
    sqk = k.sb("sqk", [128, 2, 128], BF16)
    sdk = k.sb("sdk", [128, 2, 128], F32)
    rn = k.sb("rn", [128, 2, 128], F32)
    kk = k.sb("kk", [128, 2, 128], F32)
    b_ = k.sb("b_", [128, 2, 128], F32)
    kmf = k.sb("kmf", [128, 2, 128], F32)
    kmod = k.sb("kmod", [128, 2, 128], F32)
    r32 = k.sb("r32", [128, 2, 128], F32)
    gc = k.sb("gc", [128, 2, 128], F32)
    gcx = k.sb("gcx", [128, 2, 128], F32)
    E = k.sb("E", [128, 2, 128], F32)
    Einv = k.sb("Einv", [128, 2, 128], F32)
    Eex = k.sb("Eex", [128, 2, 128], F32)
    KR = k.sb("KR", [128, 2, 2, 128], BF16)
    Ki32 = k.sb("Ki32", [128, 2, 128], F32)
    Bi32 = k.sb("Bi32", [128, 2, 128], F32)
    Ki = k.sb("Ki", [128, 2, 128], BF16)
    Bi = k.sb("Bi", [128, 2, 128], BF16)
    KeT = k.sb("KeT", [128, 2, 128], BF16)
    nBeT = k.sb("nBeT", [128, 2, 128], BF16)
    dcol = k.sb("dcol", [128, 2, 2], F32)
    ndcol = k.sb("ndcol", [128, 2, 2], F32)
    rk = k.sb("rk", [128, 2, 128], BF16)
    rk32 = k.sb("rk32", [128, 2, 128], F32)
    AT = k.sb("AT", [128, 4, 512], BF16)
    P = [k.sb(f"P{i}", [128, 4, 128], BF16) for i in range(2)]
    PT = [k.sb(f"PT{i}", [128, 4, 128], BF16) for i in range(2)]
    R = [k.sb(f"R{i}", [128, 4, 128], BF16) for i in range(2)]
    Vb = k.sb("Vb", [128, 256], BF16)
    V32 = k.sb("V32", [128, 256], F32)
    Wt = k.sb("Wt", [128, 256], BF16)
    Ut = k.sb("Ut", [128, 256], BF16)
    k.op("dve", lambda e: e.memset(Wt[:], 0.0), writes=[Wt])
    k.op("dve", lambda e: e.memset(Ut[:], 0.0), writes=[Ut])
    Ke = k.sb("Ke", [128, 256], BF16)
    nBe = k.sb("nBe", [128, 256], BF16)
    bon = k.sb("bon", [128, 4], F32)
    bst = k.sb("bst", [128, 4, 6], F32)
    mv = k.sb("mv", [128, 4, 2], F32)
    sdg = k.sb("sdg", [128, 4], F32)
    rg = k.sb("rg", [128, 4], F32)
    yn = k.sb("yn", [128, 256], F32)
    yg = k.sb("yg", [128, 256], F32)
    yTb = [k.sb(f"yTb{i}", [128, 2, 512], BF16) for i in range(2)]

    b0 = k.ps("b0", [128, 512], F32)
    b1 = k.ps("b1", [128, 512], F32)
    b2 = k.ps("b2", [128, 512], F32)
    b3 = k.ps("b3", [128, 512], F32)
    b4 = k.ps("b4", [128, 512], F32)
    b5 = k.ps("b5", [128, 512], F32)
    b5b = None
    b6 = k.ps("b6", [128, 512], F32)
    b7 = k.ps("b7", [128, 512], F32)
    b45b = k.ps("b45b", [128, 1024], BF16) if False else None


    for i in range(ntiles):
        t0 = i * 128
        hxc = hx[i % 2]
        hxp = hx[(i + 1) % 2]
        rr, mm_, tt0 = i // 16, (i % 16) // 4, (i % 4) * 128
        k.dma("sp", hxc[:], hn_all[mm_][rr].rearrange("(c p) t -> p c t", p=128)[:, :, tt0:tt0 + 128],
              reads=[hnall_t[mm_]], writes=[hxc], sem=hxc.trk.name)
        k.cp("pool", hs[:, :, 1:128], hxc[:, :, 0:127], [hxc], [hs])
        if i > 0:
            k.cp("pool", hs[:, :, 0:1], hxp[:, :, 127:128], [hxp], [hs])
        def proj(out, c0, c1, fm):
            for dc in range(8):
                for (Wx, hh) in ((Wa, hxc), (Wb, hs)):
                    first = dc == 0 and Wx is Wa
                    last = dc == 7 and Wx is Wb
                    if fm:
                        k.mm(out, Wx[:, dc, c0:c1], hh[:, dc, :], first, last, [Wx, hh], [out_b])
                    else:
                        k.mm(out, hh[:, dc, :], Wx[:, dc, c0:c1], first, last, [Wx, hh], [out_b])
        out_b = b0
        for j in range(4):
            proj(b0[:, j * 128:(j + 1) * 128], j * 128, (j + 1) * 128, True)
        out_b = b1
        proj(b1[:, 0:128], 768, 896, True)
        proj(b1[:, 128:256], 896, 1024, True)
        out_b = b2
        proj(b2[:, 0:256], 512, 768, False)
        k.act(l1[0:64, :], b1[0:64, 0:128], AF.Tanh, [b1], [l1])
        k.cp("act", l1[64:128, :], b1[64:128, 0:128], [b1], [l1])
        k.act(sg1[:], b1[:, 128:256], AF.Sigmoid, [b1], [sg1])
        k.cp("act", Vb[:], b2[:, 0:256], [b2], [Vb])
        k.cp("dve", V32[:], b2[:, 0:256], [b2], [V32])
        for p in range(2):
            k.mm(b3[:, p * 128:(p + 1) * 128], w2b[0:64, p * 128:(p + 1) * 128], l1[0:64, :], True, True, [w2b, l1], [b3])
        for p in range(2):
            k.mm(b4[:, p * 128:(p + 1) * 128], a2b[64:128, p * 128:(p + 1) * 128], l1[64:128, :], True, True,
                 [a2b, l1], [b4])
        k.mm(b2[:, 256:512], sg1[:], g2b[:], True, True, [sg1, g2b], [b2])
        for p in range(2):
            k.act(sigw[:, p, :], b3[:, p * 128:(p + 1) * 128], AF.Sigmoid, [b3, cv], [sigw], bias=cv[:, p, 0:1])
            k.act(a_[:, p, :], b4[:, p * 128:(p + 1) * 128], AF.Sigmoid, [b4, cv], [a_], bias=cv[:, p, 1:2])
        k.cp("act", r32[:], b0[:, 0:256].rearrange("p (a t) -> p a t", a=2), [b0], [r32])
        for p in range(2):
            k.ts("dve", kku[:, p, :], b0[:, 256 + p * 128:256 + (p + 1) * 128], cv[:, p, 2:3], None, ALU.mult, None,
                 [b0, cv], [kku])
        k.tt("pool", sqk[:], kku[:], kku[:], ALU.mult, [kku], [sqk])
        for p in range(2):
            k.mm(b3[:, p * 128:(p + 1) * 128], bones[:], sqk[:, p, :], True, True, [bones, sqk], [b3])
        k.act(sdk[:], b3[:, 0:256].rearrange("p (a t) -> p a t", a=2), AF.Sqrt, [b3, epsk], [sdk], bias=epsk[:], scale=1.0)
        k.recip(rn[:], sdk[:], [sdk], [rn])
        k.tt("dve", kk[:], kku[:], rn[:], ALU.mult, [kku, rn], [kk])
        k.tt("pool", b_[:], kk[:], a_[:], ALU.mult, [kk, a_], [b_])
        for p in range(2):
            k.ts("pool", kmf[:, p, :], a_[:, p, :], cv[:, p, 3:4], omka[:, p:p + 1], ALU.mult, ALU.add, [a_, cv, omka], [kmf])
        k.tt("dve", kmod[:], kmf[:], b0[:, 256:512].rearrange("p (a t) -> p a t", a=2), ALU.mult, [kmf, b0], [kmod])
        for p in range(2):
            k.op("dve", lambda e, p=p: e.tensor_tensor_scan(out=gc[:, p, :], data0=rmask[:, p, :], data1=sigw[:, p, :],
                                                          initial=0.0, op0=ALU.mult, op1=ALU.add),
                 reads=[rmask, sigw], writes=[gc])
        k.tt("pool", gcx[:], gc[:], sigw[:], ALU.subtract, [gc, sigw], [gcx])
        k.act(E[:], gc[:], AF.Exp, [gc], [E], scale=-CW)
        k.act(Einv[:], gc[:], AF.Exp, [gc], [Einv], scale=CW)
        k.act(Eex[:], gcx[:], AF.Exp, [gcx], [Eex], scale=-CW)
        k.cp("pool", dcol[:], E[:, :, 63:128:64], [E], [dcol])
        k.ts("pool", ndcol[:], dcol[:], -1.0, None, ALU.mult, None, [dcol], [ndcol])
        k.tt("dve", KR[:, :, 0, :], kk[:], Eex[:], ALU.mult, [kk, Eex], [KR])
        k.tt("dve", KR[:, :, 1, :], r32[:], E[:], ALU.mult, [r32, E], [KR])
        k.tt("dve", Ki32[:], kmod[:], Einv[:], ALU.mult, [kmod, Einv], [Ki32])
        k.tt("pool", Bi32[:], b_[:], Einv[:], ALU.mult, [b_, Einv], [Bi32])
        k.cp("act", Ki[:], Ki32[:], [Ki32], [Ki])
        k.cp("act", Bi[:], Bi32[:], [Bi32], [Bi])
        for p in range(2):
            for c in range(2):
                k.ts("dve", KeT[:, p, 64 * c:64 * c + 64], Ki32[:, p, 64 * c:64 * c + 64], dcol[:, p, c:c + 1], None,
                     ALU.mult, None, [Ki32, dcol], [KeT])
                k.ts("pool", nBeT[:, p, 64 * c:64 * c + 64], Bi32[:, p, 64 * c:64 * c + 64], ndcol[:, p, c:c + 1], None,
                     ALU.mult, None, [Bi32, ndcol], [nBeT])
        k.tt("pool", rk32[:], r32[:], kmod[:], ALU.mult, [r32, kmod], [rk32])
        for p in range(2):
            k.ts("pool", rk[:, p, :], rk32[:, p, :], cv[:, p, 4:5], None, ALU.mult, None, [rk32, cv], [rk])
        for p in range(2):
            k.mm(b4[:, p * 128:(p + 1) * 128], KeT[:, p, :], identb[:], True, True, [KeT, identb], [b4])
            k.mm(b4[:, 256 + p * 128:256 + (p + 1) * 128], nBeT[:, p, :], identb[:], True, True, [nBeT, identb], [b4])
        k.cp("act", Ke[:], b4[:, 0:256], [b4], [Ke])
        k.cp("act", nBe[:], b4[:, 256:512], [b4], [nBe])
        for p in range(2):
            k.mm(b7[:, 256 + 2 * p:256 + 2 * p + 2], rk[:, p, :], bones[:, 0:128:64], True, True, [rk, bones], [b7])
        k.cp("dve", bon[:], b7[:, 256:260], [b7], [bon])
        for h in range(4):
            p, q = h // 2, h % 2
            bk = b5 if h % 2 == 0 else b6
            ks = slice(q * 64, (q + 1) * 64)
            k.mm(bk[:, 0:256], Ki[ks, p, :], KR[ks, p, :, :].rearrange("k a t -> k (a t)"), True, True, [Ki, KR], [bk])
            k.mm(bk[:, 256:512], Bi[ks, p, :], KR[ks, p, :, :].rearrange("k a t -> k (a t)"), True, True, [Bi, KR], [bk])
            k.tt("dve", AT[:, h, :], bk[:], mask4[:], ALU.mult, [bk, mask4], [AT])
            k.mm(b4[:, h * 128:(h + 1) * 128], KR[ks, p, 0, :], Bi[ks, p, :], True, True, [KR, Bi], [b4])
        k.tt("dve", PT[0][:], b4[:].rearrange("p (h t) -> p h t", h=4), maskL4[:], ALU.mult, [b4, maskL4], [PT[0]])
        k.cp("pool", P[0][:], AT[:, :, 256:384], [AT], [P[0]])
        k.tt("pool", R[0][:], ident4[:], P[0][:], ALU.subtract, [ident4, P[0]], [R[0]])
        for j in range(5):
            pc, pn = P[j % 2], P[(j + 1) % 2]
            ptc, ptn = PT[j % 2], PT[(j + 1) % 2]
            rc, rn_ = R[j % 2], R[(j + 1) % 2]
            for h in range(4):
                k.mm(b0[:, h * 128:(h + 1) * 128], pc[:, h, :], ptc[:, h, :], True, True, [pc, ptc], [b0])
            k.cp("act", ptn[:], b0[:].rearrange("p (h t) -> p h t", h=4), [b0], [ptn])
            if j < 4:
                for h in range(4):
                    k.mm(b1[:, h * 128:(h + 1) * 128], ptc[:, h, :], pc[:, h, :], True, True, [pc, ptc], [b1])
                k.cp("act", pn[:], b1[:].rearrange("p (h t) -> p h t", h=4), [b1], [pn])
            for h in range(4):
                k.mm(b3[:, h * 128:(h + 1) * 128], ptn[:, h, :], rc[:, h, :], True, True, [ptn, rc], [b3])
            k.tt("dve", rn_[:], b3[:].rearrange("p (h t) -> p h t", h=4), rc[:], ALU.add, [b3, rc], [rn_])
        Rf = R[5 % 2]
        for c in range(2):
            cs = slice(64 * c, 64 * c + 64)
            mb = Mb[mcur]
            for h in range(4):
                p, q = h // 2, h % 2
                ks = slice(q * 64, (q + 1) * 64)
                hv = slice(h * 64, (h + 1) * 64)
                k.mm(b6[cs, hv], AT[:, h, 64 * c:64 * c + 64], Vb[:, hv], True, False, [AT, Vb], [b6])
                k.mm(b6[cs, hv], KR[:, p, 0, cs], mb[:, h, :], False, True, [KR, mb], [b6])
            k.cp("act", Wt[cs, :], b6[cs, 0:256], [b6], [Wt])
            for h in range(4):
                hv = slice(h * 64, (h + 1) * 64)
                k.mm(b6[cs, 256 + h * 64:256 + (h + 1) * 64], Rf[:, h, cs], Wt[:, hv], True, True, [Rf, Wt], [b6])
            k.cp("act", Ut[cs, :], b6[cs, 256:512], [b6], [Ut])
            for h in range(4):
                p, q = h // 2, h % 2
                ks = slice(q * 64, (q + 1) * 64)
                hv = slice(h * 64, (h + 1) * 64)
                k.mm(b7[cs, hv], KR[:, p, 1, cs], mb[:, h, :], True, False, [KR, mb], [b7])
                k.mm(b7[cs, hv], AT[:, h, 128 + 64 * c:128 + 64 * c + 64], Vb[:, hv], False, False, [AT, Vb], [b7])
                k.mm(b7[cs, hv], AT[:, h, 384 + 64 * c:384 + 64 * c + 64], Ut[:, hv], False, True, [AT, Ut], [b7])
            for h in range(4):
                p, q = h // 2, h % 2
                ks = slice(q * 64, (q + 1) * 64)
                hv = slice(h * 64, (h + 1) * 64)
                k.mm(b5[ks, p * 64:(p + 1) * 64], Ke[cs, hv], Vb[cs, hv], True, False, [Ke, Vb], [b5])
                k.mm(b5[ks, p * 64:(p + 1) * 64], nBe[cs, hv], Ut[cs, hv], False, True, [nBe, Ut], [b5])
            for p in range(2):
                k.stt(M32[:, p, :], M32[:, p, :], dcol[:, p, c:c + 1], b5[:, p * 64:(p + 1) * 64], ALU.mult, ALU.add,
                      [M32, dcol, b5], [M32])
            mcur = (mcur + 1) % 2
            for q in range(2):
                k.ts("pool", Mb[mcur][:, q:4:2, :], M32[:], hmask[:, q:q + 1], None, ALU.mult, None, [M32, hmask], [Mb[mcur]])
        for h in range(4):
            k.op("dve", lambda e, h=h: e.bn_stats(out=bst[:, h, :], in_=b7[:, h * 64:(h + 1) * 64]), reads=[b7], writes=[bst])
            k.op("dve", lambda e, h=h: e.bn_aggr(out=mv[:, h, :], in_=bst[:, h, :]), reads=[bst], writes=[mv])
        k.act(sdg[:], mv[:, :, 1], AF.Sqrt, [mv, epsg], [sdg], bias=epsg[:], scale=1.0)
        k.recip(rg[:], sdg[:], [sdg], [rg])
        for h in range(4):
            hv = slice(h * 64, (h + 1) * 64)
            k.ts("dve", yn[:, hv], b7[:, hv], mv[:, h, 0:1], rg[:, h:h + 1], ALU.subtract, ALU.mult, [b7, mv, rg], [yn])
        k.tt("pool", yn[:], yn[:], lngb[:], ALU.mult, [yn, lngb], [yn])
        k.tt("pool", yn[:], yn[:], lnbb[:], ALU.add, [yn, lnbb], [yn])
        for h in range(4):
            hv = slice(h * 64, (h + 1) * 64)
            k.stt(yn[:, hv], V32[:, hv], bon[:, h:h + 1], yn[:, hv], ALU.mult, ALU.add, [V32, bon, yn], [yn])
        k.tt("dve", yg[:], yn[:], b2[:, 256:512], ALU.mult, [yn, b2], [yg])
        for c2 in range(2):
            k.tr(b4[:, c2 * 128:(c2 + 1) * 128], yg[:, c2 * 128:(c2 + 1) * 128], ident[:], [yg, ident], [b4])
        yb = yTb[(i // 4) % 2]
        j = i % 4
        k.op("act", lambda e, yb=yb, j=j: e.copy(out=yb[:, :, j * 128:(j + 1) * 128],
                                                in_=b4[:, 0:256].rearrange("p (c t) -> p c t", c=2)),
             reads=[b4], writes=[yb])
        if j == 3:
            qr = i // 16
            off = ((i % 16) // 4) * 512
            k.dma("sp", y_loc[qr].rearrange("(c p) t -> p c t", p=128)[:, :, off:off + 512], yb[:, :, :], reads=[yb],
                  writes=[yloc_t[qr]], sem=f"ygb{(i // 4) % 2}")
            if i % 16 == 15:
                gather(qr)


def phase_ffn(nc, k, mode, hacc, hacc_t, outT, yall, yall_t, wo, g1c, wg, wu, wd, hin=None, g2c=None, hn_loc=None,
              hnloc_t=None, gather=None, rt=None, rb=None, gout=None, out=None):
    moe = mode == "moe"
    NE, FE = (8, 1408) if moe else (1, 2816)
    G = 4
    xnT = k.sb("xnT", [128, 8, TOK], BF16)
    wob = k.sb("wob", [128, 8, D], BF16)
    wgs = [k.sb(f"wgs{i}", [128, 8, G * 128], BF16) for i in range(2)]
    wus = [k.sb(f"wus{i}", [128, 8, G * 128], BF16) for i in range(2)]
    wds = [k.sb(f"wds{i}", [128, G, D], BF16) for i in range(2)]
    actT = [k.sb(f"actT{i}", [128, G, 512], BF16) for i in range(2)]
    sg = [k.sb(f"sg{i}", [128, 512], F32) for i in range(2)]
    xn32 = [k.sb(f"xn32_{i}", [128, D], F32) for i in range(2)]
    junk = k.sb("junk", [128, D], F32)
    ss = k.sb("ss", [128, NT], F32)
    sd = k.sb("sd", [128, NT], F32)
    rstd = k.sb("rstd", [128, NT], F32)
    g1 = k.sb("g1", [128, 8], F32)
    ident = make_ident(k)
    pso = [k.ps(f"pso{i}", [128, D], F32) for i in range(2)]
    psg = [k.ps(f"psg{i}", [128, 512], F32) for i in range(2)]
    psu = [k.ps(f"psu{i}", [128, 512], F32) for i in range(2)]
    xt_t = [k.trk(f"xt{i}") for i in range(NT)]
    if moe:
        rt32 = k.sb("rt32", [128, 8, NE], F32)
        rbb = k.sb("rbb", [128, NE], F32)
        xT32 = k.sb("xT32", [128, D], F32)
        lg = k.sb("lg", [128, NT, NE], F32)
        gates = k.sb("gates", [128, NT, NE], F32)
        mx = k.sb("mx", [128, 8], F32)
        nm1 = k.sb("nm1", [128, 1], F32)
        ex = k.sb("ex", [128, NE], F32)
        msk = k.sb("msk", [128, NE], F32)
        den = k.sb("den", [128, 1], F32)
        rden = k.sb("rden", [128, 1], F32)
        goutb = k.sb("goutb", [128, D], F32)
        xlo = k.sb("xlo", [128, 8, 128], BF16)
        rth = k.sb("rth", [128, 8, NE], BF16)
        rtl = k.sb("rtl", [128, 8, NE], BF16)
        rtd = k.sb("rtd", [128, 8, NE], F32)
        obuf = [k.sb(f"obuf{i}", [128, D], F32) for i in range(2)]
    else:
        g2 = k.sb("g2", [128, 8], F32)

    for m in range(4):
        def src(e, m=m):
            qd = bass.ds(k.pid_sp, 1)
            return yall[qd].rearrange("o r (h p) t -> p (o r h) t", p=128)[:, :, m * 512:(m + 1) * 512]
        k.dma("sp", xnT[:, :, m * 512:(m + 1) * 512], src, reads=yall_t, writes=xt_t[4 * m:4 * m + 4], sem=f"xt{m}")
    if not moe:
        hin_v = hin.rearrange("(n p) d -> p n d", p=128)
        for m in range(4):
            k.dma("act", hacc[:, 4 * m:4 * m + 4, :], hin_v[:, 4 * m:4 * m + 4, :],
                  writes=hacc_t[4 * m:4 * m + 4], sem=f"hacc{m}")
    for c in range(8):
        r0 = c * 128 if moe else ((c % 2) * 512 + (c // 2) * 128)
        k.dma("pool", wob[:, c, :], wo[r0:r0 + 128, :], writes=[wob])
    k.dma("sp", g1[:], g1c, writes=[g1])
    if moe:
        k.dma("sp", rt32[:], rt.rearrange("(c p) e -> p c e", p=128), writes=[rt32])
        k.dma("sp", rbb[:], rb.partition_broadcast(128), writes=[rbb])
        k.dma("sp", goutb[:], gout.partition_broadcast(128), writes=[goutb])
        k.cp("dve", rth[:], rt32[:], [rt32], [rth])
        k.tt("dve", rtd[:], rt32[:], rth[:], ALU.subtract, [rt32, rth], [rtd])
        k.cp("dve", rtl[:], rtd[:], [rtd], [rtl])
    else:
        k.dma("sp", g2[:], g2c, writes=[g2])

    passes = []
    for e in range(NE):
        nchunk = FE // 128
        c0 = 0
        while c0 < nchunk:
            g = min(G, nchunk - c0)
            passes.append((e, c0, g))
            c0 += g

    def load_pass(pi):
        e, c0, g = passes[pi]
        s = pi % 2
        wg_v = wg[e].rearrange("(c p) f -> p c f", p=128)
        wu_v = wu[e].rearrange("(c p) f -> p c f", p=128)
        wd_v = wd[e].rearrange("(c p) d -> p c d", p=128)
        for c in range(0, 8, 4):
            k.dma("pool", wgs[s][:, c:c + 4, 0:g * 128], wg_v[:, c:c + 4, c0 * 128:(c0 + g) * 128], writes=[wgs[s]])
        for c in range(0, 8, 4):
            k.dma("pool", wus[s][:, c:c + 4, 0:g * 128], wu_v[:, c:c + 4, c0 * 128:(c0 + g) * 128], writes=[wus[s]])
        for c in range(0, g, 2):
            c1 = min(c + 2, g)
            k.dma("pool", wds[s][:, c:c1, :], wd_v[:, c0 + c:c0 + c1, :], writes=[wds[s]])

    load_pass(0)

    for i in range(NT):
        p = pso[i % 2]
        for half in range(2):
            for dc in range(8):
                k.op("pe", lambda e, p=p, half=half, dc=dc, i=i: e.matmul(
                    p[:, half * 512:(half + 1) * 512], lhsT=xnT[:, dc, i * 128:(i + 1) * 128],
                    rhs=wob[:, dc, half * 512:(half + 1) * 512], start=(dc == 0), stop=(dc == 7)),
                    reads=[xt_t[i], wob], writes=[p])
        k.op("dve", lambda e, p=p, i=i: e.tensor_tensor(out=hacc[:, i, :], in0=p[:], in1=hacc[:, i, :], op=ALU.add),
             reads=[p, hacc_t[i]], writes=[hacc_t[i]])

    def norm_stats():
        for i in range(NT):
            k.op("act", lambda e, i=i: e.activation(out=junk[:], in_=hacc[:, i, :], func=AF.Square,
                                                     accum_out=ss[:, i:i + 1]),
                 reads=[hacc_t[i]], writes=[junk, ss])
        k.op("act", lambda e: e.activation(out=sd[:], in_=ss[:], func=AF.Sqrt, scale=1.0 / D, bias=epsb[:]),
             reads=[ss, epsb], writes=[sd])
        k.op("dve", lambda e: e.reciprocal(out=rstd[:], in_=sd[:]), reads=[sd], writes=[rstd])

    epsb = k.sb("epsb", [128, 1], F32)
    k.op("dve", lambda e: e.memset(epsb[:], RMS_EPS), writes=[epsb])

    def norm_transpose(gcol, dst_tile_fn, extra=None):
        for i in range(NT):
            xb = xn32[i % 2]
            p = pso[i % 2]
            k.op("act", lambda e, i=i, xb=xb: e.activation(out=xb[:], in_=hacc[:, i, :], func=AF.Copy,
                                                           scale=rstd[:, i:i + 1]),
                 reads=[hacc_t[i], rstd], writes=[xb])
            for dc in range(8):
                k.op("pe", lambda e, dc=dc, xb=xb, p=p: e.transpose(out=p[:, dc * 128:(dc + 1) * 128],
                                                                  in_=xb[:, dc * 128:(dc + 1) * 128],
                                                                  identity=ident[:]),
                     reads=[xb, ident], writes=[p])
            for dc in range(8):
                k.op("dve", lambda e, dc=dc, i=i, p=p: e.tensor_scalar(
                    out=xnT[:, dc, i * 128:(i + 1) * 128], in0=p[:, dc * 128:(dc + 1) * 128],
                    scalar1=gcol[:, dc:dc + 1], scalar2=None, op0=ALU.mult),
                    reads=[p, gcol], writes=[xt_t[i]])
            if extra is not None:
                extra(i, p)

    norm_stats()

    def router(i, p):
        pl = psg[i % 2]
        for dc in range(8):
            k.stt(xlo[:, dc, :], p[:, dc * 128:(dc + 1) * 128], g1[:, dc:dc + 1], xnT[:, dc, i * 128:(i + 1) * 128],
                  ALU.mult, ALU.subtract, [p, g1, xt_t[i]], [xlo])
        for dc in range(8):
            k.mm(pl[:, 0:NE], xnT[:, dc, i * 128:(i + 1) * 128], rth[:, dc, :], dc == 0, False, [xt_t[i], rth], [pl])
            k.mm(pl[:, 0:NE], xlo[:, dc, :], rth[:, dc, :], False, False, [xlo, rth], [pl])
            k.mm(pl[:, 0:NE], xnT[:, dc, i * 128:(i + 1) * 128], rtl[:, dc, :], False, dc == 7, [xt_t[i], rtl], [pl])
        k.op("dve", lambda e, i=i, pl=pl: e.tensor_tensor(out=lg[:, i, :], in0=pl[:, 0:NE], in1=rbb[:], op=ALU.add),
             reads=[pl, rbb], writes=[lg])
        k.op("dve", lambda e, i=i: e.max(out=mx[:], in_=lg[:, i, :]), reads=[lg], writes=[mx])
        k.op("dve", lambda e: e.tensor_scalar(out=nm1[:], in0=mx[:, 0:1], scalar1=-1.0, scalar2=None, op0=ALU.mult),
             reads=[mx], writes=[nm1])
        k.op("act", lambda e, i=i: e.activation(out=ex[:], in_=lg[:, i, :], func=AF.Exp, bias=nm1[:], scale=1.0),
             reads=[lg, nm1], writes=[ex])
        k.op("dve", lambda e, i=i: e.tensor_scalar(out=msk[:], in0=lg[:, i, :], scalar1=mx[:, 1:2], scalar2=None,
                                                 op0=ALU.is_ge), reads=[lg, mx], writes=[msk])
        k.op("dve", lambda e: e.tensor_tensor(out=ex[:], in0=ex[:], in1=msk[:], op=ALU.mult),
             reads=[ex, msk], writes=[ex])
        k.op("dve", lambda e: e.reduce_sum(out=den[:], in_=ex[:], axis=AX.X), reads=[ex], writes=[den])
        k.op("dve", lambda e: e.reciprocal(out=rden[:], in_=den[:]), reads=[den], writes=[rden])
        k.op("dve", lambda e, i=i: e.tensor_scalar(out=gates[:, i, :], in0=ex[:], scalar1=rden[:, 0:1], scalar2=None,
                                                 op0=ALU.mult), reads=[ex, rden], writes=[gates])

    norm_transpose(g1, None, extra=router if moe else None)

    for pi, (e_idx, c0, g) in enumerate(passes):
        s = pi % 2
        if pi + 1 < len(passes):
            load_pass(pi + 1)
        for m in range(4):
            a = actT[m % 2]
            for c in range(g):
                pg = psg[c % 2]
                pu = psu[c % 2]
                for dc in range(8):
                    k.op("pe", lambda e, pg=pg, s=s, c=c, dc=dc, m=m: e.matmul(
                        pg[:], lhsT=wgs[s][:, dc, c * 128:(c + 1) * 128], rhs=xnT[:, dc, m * 512:(m + 1) * 512],
                        start=(dc == 0), stop=(dc == 7)), reads=[wgs[s]] + xt_t[4 * m:4 * m + 4], writes=[pg])
                for dc in range(8):
                    k.op("pe", lambda e, pu=pu, s=s, c=c, dc=dc, m=m: e.matmul(
                        pu[:], lhsT=wus[s][:, dc, c * 128:(c + 1) * 128], rhs=xnT[:, dc, m * 512:(m + 1) * 512],
                        start=(dc == 0), stop=(dc == 7)), reads=[wus[s]] + xt_t[4 * m:4 * m + 4], writes=[pu])
                sgb = sg[c % 2]
                k.op("act", lambda e, sgb=sgb, pg=pg: e.activation(out=sgb[:], in_=pg[:], func=AF.Silu),
                     reads=[pg], writes=[sgb])
                k.op("dve", lambda e, sgb=sgb, pu=pu, a=a, c=c: e.tensor_tensor(out=a[:, c, :], in0=pu[:], in1=sgb[:],
                                                                             op=ALU.mult),
                     reads=[pu, sgb], writes=[a])
            for tt in range(4):
                i = 4 * m + tt
                p = pso[tt % 2]
                for half in range(2):
                    for c in range(g):
                        k.op("pe", lambda e, p=p, half=half, c=c, a=a, tt=tt, s=s, g=g: e.matmul(
                            p[:, half * 512:(half + 1) * 512], lhsT=a[:, c, tt * 128:(tt + 1) * 128],
                            rhs=wds[s][:, c, half * 512:(half + 1) * 512], start=(c == 0), stop=(c == g - 1)),
                            reads=[a, wds[s]], writes=[p])
                if moe:
                    k.op("dve", lambda e, p=p, i=i, e_idx=e_idx: e.scalar_tensor_tensor(
                        out=hacc[:, i, :], in0=p[:], scalar=gates[:, i, e_idx:e_idx + 1], in1=hacc[:, i, :],
                        op0=ALU.mult, op1=ALU.add), reads=[p, gates, hacc_t[i]], writes=[hacc_t[i]])
                else:
                    k.op("dve", lambda e, p=p, i=i: e.tensor_tensor(out=hacc[:, i, :], in0=p[:], in1=hacc[:, i, :],
                                                                   op=ALU.add),
                         reads=[p, hacc_t[i]], writes=[hacc_t[i]])

    out_v = out.rearrange("(n p) d -> p n d", p=128) if moe else None
    if moe:
        norm_stats()
        for i in range(NT):
            ob = obuf[i % 2]
            k.op("dve", lambda e, i=i, ob=ob: e.scalar_tensor_tensor(
                out=ob[:], in0=hacc[:, i, :], scalar=rstd[:, i:i + 1], in1=goutb[:], op0=ALU.mult, op1=ALU.mult),
                reads=[hacc_t[i], rstd, goutb], writes=[ob])
            k.dma("sp", out_v[:, i, :], ob[:], reads=[ob], writes=[outT], sem=f"obuf{i % 2}")
    else:
        norm_stats()
        norm_transpose(g2, None)
        for m in range(4):
            k.dma("act", hn_loc[m].rearrange("(c p) t -> p c t", p=128), xnT[:, :, m * 512:(m + 1) * 512],
                  reads=xt_t[4 * m:4 * m + 4], writes=[hnloc_t[m]], sem=f"xst{m}")
            gather(m)


GROUPS = [[0, 1, 2, 3], [4, 5, 6, 7]]


def build_fused(upto=4, skip=()):
    nc = bass.Bass("TRN2", target_bir_lowering=False)

    def inp(name, shape, dt=F32):
        return nc.dram_tensor(name, list(shape), dt, kind="ExternalInput").ap()
    m_x = inp("m_x", [T, D]); m_win = inp("m_win", [D, 768]); m_gcol = inp("m_gcol", [128, 8])
    m_wsT = inp("m_wsT", [128, 128]); m_gng = inp("m_gng", [128]); m_bsc = inp("m_bsc", [128, 1])
    m_lbl = inp("m_lbl", [128, 2]); m_ong = inp("m_ong", [128])
    f_hin = inp("f_hin", [TOK, D]); f_wo = inp("f_wo", [D, D]); f_g1c = inp("f_g1c", [128, 8])
    f_wg = inp("f_wg", [1, D, 2816]); f_wu = inp("f_wu", [1, D, 2816]); f_wd = inp("f_wd", [1, 2816, D])
    f_g2c = inp("f_g2c", [128, 8])
    r_mixc = inp("r_mixc", [128, 8, 6]); r_wproj = inp("r_wproj", [D, 1024]); r_w2 = inp("r_w2", [64, 256])
    r_a2 = inp("r_a2", [64, 256]); r_g2 = inp("r_g2", [128, 256]); r_cvec = inp("r_cvec", [128, 2, 5])
    r_lng = inp("r_lng", [256]); r_lnb = inp("r_lnb", [256])
    e_wo = inp("e_wo", [D, D]); e_g1c = inp("e_g1c", [128, 8])
    e_wg = inp("e_wg", [8, D, 1408]); e_wu = inp("e_wu", [8, D, 1408]); e_wd = inp("e_wd", [8, 1408, D])
    e_rt = inp("e_rt", [D, 8]); e_rb = inp("e_rb", [8]); e_gout = inp("e_gout", [D])
    out = nc.dram_tensor("out", [TOK, D], F32, kind="ExternalOutput").ap()
    y_loc = nc.dram_tensor("y_loc", [4, 256, 2048], BF16).ap()
    y_all = nc.dram_tensor("y_all", [4, 4, 256, 2048], BF16).ap()
    hn_loc = nc.dram_tensor("hn_loc", [4, 1024, 512], BF16).ap()
    hn_all = nc.dram_tensor("hn_all", [4, 4, 1024, 512], BF16).ap()
    yg_loc = y_loc
    yg_all = y_all

    k = KB(nc)
    hacc = k.sb("hacc", [128, NT, D], F32, glob=True)
    hacc_t = [k.trk(f"hacc{i}") for i in range(NT)]
    outT = k.trk("outT")
    yloc_t = [k.trk(f"yloc{i}") for i in range(4)]
    yall_t = [k.trk(f"yall{i}") for i in range(4)]
    hnloc_t = [k.trk(f"hnloc{i}") for i in range(4)]
    hnall_t = [k.trk(f"hnall{i}") for i in range(4)]
    ygloc_t = [k.trk(f"ygloc{i}") for i in range(4)]
    ygall_t = [k.trk(f"ygall{i}") for i in range(4)]

    def mk_gather(loc, allb, loc_t, all_t, pat, nm):
        def gather(i):
            if nm in _NOCOLL:
                return
            k.coll(lambda e: e.collective_compute("AllGather", ALU.bypass, replica_groups=GROUPS,
                                                  ins=[loc[i].opt()], outs=[allb[i].rearrange(pat).opt()]),
                   f"{nm}{i}", reads=[loc_t[i]], writes=[all_t[i]])
        return gather

    if 1 not in skip:
     k.push("m_")
     phase_mix0(nc, k, m_x, m_win, m_gcol, m_wsT, m_gng, m_bsc, m_lbl, m_ong, y_loc, yloc_t,
               mk_gather(y_loc, y_all, yloc_t, yall_t, "r c t -> (r c) t", "ga"))
     k.pop()
    if upto >= 2 and 2 not in skip:
      k.push("f_")
      phase_ffn(nc, k, "ffn", hacc, hacc_t, outT, y_all, yall_t, f_wo, f_g1c, f_wg, f_wu, f_wd, hin=f_hin, g2c=f_g2c,
              hn_loc=hn_loc, hnloc_t=hnloc_t, gather=mk_gather(hn_loc, hn_all, hnloc_t, hnall_t, "r c t -> (r c) t", "gb"))
      k.pop()
    if upto >= 3:
      k.push("r_")
      phase_rwkv(nc, k, hn_all, hnall_t, r_mixc, r_wproj, r_w2, r_a2, r_g2, r_cvec, r_lng, r_lnb, yg_loc, ygloc_t,
               mk_gather(yg_loc, yg_all, ygloc_t, ygall_t, "r c t -> (r c) t", "gc"))
      k.pop()
    if upto >= 4:
      k.push("e_")
      phase_ffn(nc, k, "moe", hacc, hacc_t, outT, yg_all, ygall_t, e_wo, e_g1c, e_wg, e_wu, e_wd, rt=e_rt, rb=e_rb,
              gout=e_gout, out=out)
      k.wait_all("sp", [outT])
      k.pop()
    k.emit()
    k.close()
    return nc


_UPTO = 4
_NOCOLL = ()
_SKIP = ()
def _gc(g):
    return np.ascontiguousarray(np.asarray(g, np.float32).reshape(8, 128).T)


_NC = {}


def kernel(x, norm_mix_g, norm_ffn_g, norm_out_g,
           mix_w_in, mix_w_out, gmlp_norm_g, gmlp_w_s, gmlp_b_s, hgrn_lb_logits, hgrn_onorm_g,
           ffn_w_gate, ffn_w_up, ffn_w_down,
           rwkv_mix, rwkv_w_r, rwkv_w_k, rwkv_w_v, rwkv_w_o, rwkv_w0, rwkv_w1, rwkv_w2,
           rwkv_a0, rwkv_a1, rwkv_a2, rwkv_g1, rwkv_g2, rwkv_k_k, rwkv_k_a, rwkv_r_k,
           rwkv_ln_g, rwkv_ln_b,
           moe_router, moe_router_b, moe_w_gate, moe_w_up, moe_w_down, _trace=False):
    f32 = np.float32
    A = lambda a: np.ascontiguousarray(np.asarray(a, f32))
    x = A(x)
    xf = x.reshape(16384, 1024)
    cores = list(range(8))
    w = np.asarray(mix_w_in, f32)[0]
    colb = [0 * 512, 1 * 512, 4 * 512, 5 * 512, 2 * 512, 3 * 512]
    mixc = A(np.asarray(rwkv_mix, f32)[0].reshape(6, 8, 128).transpose(2, 1, 0))
    shared = dict(
        m_gcol=_gc(norm_mix_g[0]), m_ong=A(np.asarray(hgrn_onorm_g)[0]),
        f_wo=A(mix_w_out[0]), f_g1c=_gc(norm_ffn_g[0]), f_wg=A(ffn_w_gate), f_wu=A(ffn_w_up), f_wd=A(ffn_w_down),
        f_g2c=_gc(norm_mix_g[1]),
        r_mixc=mixc,
        e_wo=A(rwkv_w_o[0]), e_g1c=_gc(norm_ffn_g[1]), e_wg=A(moe_w_gate[0]), e_wu=A(moe_w_up[0]), e_wd=A(moe_w_down[0]),
        e_rt=A(moe_router[0]), e_rb=A(moe_router_b[0]), e_gout=A(norm_out_g))
    in_maps = []
    for c in cores:
        b, j = c // 4, c % 4
        cs = slice(256 * j, 256 * (j + 1))
        win = np.concatenate([w[:, cb + j * 128: cb + (j + 1) * 128] for cb in colb], axis=1)
        wproj = np.concatenate([np.asarray(rwkv_w_r)[0][:, cs], np.asarray(rwkv_w_k)[0][:, cs],
                                np.asarray(rwkv_w_v)[0][:, cs], np.asarray(rwkv_w1)[0], np.asarray(rwkv_a1)[0],
                                np.asarray(rwkv_g1)[0]], axis=1)
        pv = lambda v: np.asarray(v, f32).reshape(-1)[cs].reshape(2, 128).T
        cvec = np.stack([pv(rwkv_w0[0]), pv(rwkv_a0[0]), pv(rwkv_k_k[0]), pv(rwkv_k_a[0]), pv(rwkv_r_k[0])], axis=-1)
        m = dict(shared)
        m.update(m_x=x[b], m_win=A(win), m_wsT=A(np.asarray(gmlp_w_s)[0, j].T), m_gng=A(np.asarray(gmlp_norm_g)[0, j]),
                 m_bsc=A(np.asarray(gmlp_b_s)[0, j].reshape(128, 1)),
                 m_lbl=A(np.asarray(hgrn_lb_logits)[:, j * 128:(j + 1) * 128].T),
                 f_hin=A(xf[c * 2048:(c + 1) * 2048]),
                 r_wproj=A(wproj), r_w2=A(np.asarray(rwkv_w2)[0][:, cs]), r_a2=A(np.asarray(rwkv_a2)[0][:, cs]),
                 r_g2=A(np.asarray(rwkv_g2)[0][:, cs]), r_cvec=A(cvec),
                 r_lng=A(np.asarray(rwkv_ln_g)[0][cs]), r_lnb=A(np.asarray(rwkv_ln_b)[0][cs]))
        in_maps.append(m)
    if "nc" not in _NC:
        _NC["nc"] = build_fused(_UPTO, _SKIP)
    res = run_bass_kernel_spmd(_NC["nc"], in_maps, core_ids=cores, **({"trace": True} if _trace else {}))
    if _trace:
        print("exec_time_ns", res.exec_time_ns)
    out = np.concatenate([res.results[c]["out"] for c in cores], axis=0).reshape(2, 8192, 1024)
    return np.ascontiguousarray(out.astype(np.float32))
```

```python
import contextlib
import numpy as np
import concourse.bass as bass
import concourse.mybir as mybir
from concourse.bass_utils import run_bass_kernel_spmd

F32 = mybir.dt.float32
BF16 = mybir.dt.bfloat16
I32 = mybir.dt.int32
AF = mybir.ActivationFunctionType
ALU = mybir.AluOpType
AX = mybir.AxisListType


class Trk:
    __slots__ = ("name", "w", "r", "bank")

    def __init__(self, name, bank=None):
        self.name = name
        self.w = []
        self.r = []
        self.bank = bank


class Bank:
    def __init__(self):
        self.last = {}


class Buf:
    def __init__(self, t, name):
        self.t = t
        self.trk = Trk(name)

    def __getitem__(self, idx):
        return self.t[idx]


class KB:
    ENG = ("pe", "act", "dve", "pool", "sp")

    def __init__(self, nc, same_engine_sync=True):
        self.nc = nc
        self.es = contextlib.ExitStack()
        self.sems = {}
        self.cnt = {}
        self.ops = {e: [] for e in self.ENG}
        self.known = {e: {} for e in self.ENG}
        self.same_engine_sync = same_engine_sync
        for e in self.ENG:
            self.sems[e] = self.es.enter_context(nc.semaphore("sem_" + e))
            self.cnt[e] = 0
        self.free_sems = []
        self.phase_keys = []
        self.nsem = 0
        self.pes = None
        self.prefix = ""

    def push(self, prefix=""):
        assert self.pes is None
        self.pes = contextlib.ExitStack()
        self.phase_keys = []
        self.prefix = prefix

    def barrier(self):
        allk = [(k_, v) for k_, v in self.cnt.items() if v > 0]
        for e in self.ENG:
            waits = []
            kn = self.known[e]
            for k_, v in allk:
                if k_ != e and kn.get(k_, 0) < v:
                    kn[k_] = v
                    waits.append((k_, v))
            if waits:
                self.ops[e].append((waits, None, None, 0))

    def pop(self):
        self.barrier()
        self.pes.close()
        self.pes = None
        for key in self.phase_keys:
            self.free_sems.append((self.sems[key], self.cnt[key]))
        self.phase_keys = []

    def sb(self, name, shape, dt, glob=False):
        name = self.prefix + name
        es = self.es if (glob or self.pes is None) else self.pes
        t = es.enter_context(self.nc.sbuf_tensor(name, list(shape), dt))
        return Buf(t, name)

    def ps(self, name, shape, dt=F32):
        name = self.prefix + name
        es = self.es if self.pes is None else self.pes
        t = es.enter_context(self.nc.psum_tensor(name, list(shape), dt))
        b = Buf(t, name)
        b.trk.bank = Bank()
        return b

    def trk(self, name, bank=None):
        return Trk(self.prefix + name, bank.trk.bank if bank is not None else None)

    def dsem(self, name):
        key = "d_" + name
        if key not in self.sems:
            if self.free_sems:
                h, c = self.free_sems.pop()
                self.sems[key] = h
                self.cnt[key] = c
            else:
                self.nsem += 1
                self.sems[key] = self.es.enter_context(self.nc.semaphore("dsem%d" % self.nsem))
                self.cnt[key] = 0
            if self.pes is not None:
                self.phase_keys.append(key)
        return key

    @staticmethod
    def _t(b):
        return b.trk if isinstance(b, Buf) else b

    def _deps(self, eng, reads, writes):
        need = {}

        def add(tok):
            k, v = tok
            if k == eng and (eng == "pe" or not self.same_engine_sync):
                return
            if need.get(k, 0) < v:
                need[k] = v
        for b in reads:
            for tok in self._t(b).w:
                add(tok)
        for b in writes:
            t = self._t(b)
            for tok in t.w:
                add(tok)
            for tok in t.r:
                add(tok)
        for b in list(reads) + list(writes):
            bk = self._t(b).bank
            if bk is not None:
                for e2, v in bk.last.items():
                    if e2 != eng and need.get(e2, 0) < v:
                        need[e2] = v
        waits = []
        kn = self.known[eng]
        for k, v in need.items():
            if kn.get(k, 0) < v:
                kn[k] = v
                waits.append((k, v))
        return waits

    def _commit(self, tok, reads, writes, dma_group=False):
        for b in list(reads) + list(writes):
            bk = self._t(b).bank
            if bk is not None and not tok[0].startswith("d_"):
                bk.last[tok[0]] = tok[1]
        for b in reads:
            t = self._t(b)
            t.r = [x for x in t.r if x[0] != tok[0]] + [tok]
        for b in writes:
            t = self._t(b)
            if dma_group and t.w and all(x[0].startswith("d_") for x in t.w) and not t.r:
                t.w = [x for x in t.w if x[0] != tok[0]] + [tok]
            else:
                t.w = [tok]
                t.r = []

    def op(self, eng, fn, reads=(), writes=()):
        waits = self._deps(eng, reads, writes)
        self.cnt[eng] += 1
        tok = (eng, self.cnt[eng])
        self.ops[eng].append((waits, fn, eng, 1))
        self._commit(tok, reads, writes)

    def dma(self, eng, out, in_, reads=(), writes=(), sem=None, **kw):
        if sem is None:
            b = (list(writes) + list(reads))[0]
            sem = self._t(b).name
        elif not sem.startswith(self.prefix):
            sem = self.prefix + sem
        key = self.dsem(sem)
        waits = self._deps_dma(eng, reads, writes)
        self.cnt[key] += 16
        tok = (key, self.cnt[key])
        self.ops[eng].append((waits, lambda e: e.dma_start(out=out(e) if callable(out) else out,
                                                            in_=in_(e) if callable(in_) else in_, **kw), key, 16))
        self._commit(tok, reads, writes, dma_group=True)

    def coll(self, fn, name, reads=(), writes=()):
        key = self.dsem("cc_" + name)
        waits = self._deps_dma("pool", reads, writes)
        self.cnt[key] += 1
        tok = (key, self.cnt[key])
        self.ops["pool"].append((waits, fn, key, 1))
        self._commit(tok, reads, writes, dma_group=True)

    def _deps_dma(self, eng, reads, writes):
        need = {}

        def add(tok):
            k, v = tok
            if need.get(k, 0) < v:
                need[k] = v
        for b in reads:
            for tok in self._t(b).w:
                add(tok)
        for b in writes:
            t = self._t(b)
            if not (t.w and all(x[0].startswith("d_") for x in t.w) and not t.r):
                for tok in t.w:
                    add(tok)
            for tok in t.r:
                add(tok)
        waits = []
        kn = self.known[eng]
        for k, v in need.items():
            if kn.get(k, 0) < v:
                kn[k] = v
                waits.append((k, v))
        return waits

    def wait_all(self, eng, bufs):
        need = {}
        for b in bufs:
            t = self._t(b)
            for k, v in t.w + t.r:
                if need.get(k, 0) < v:
                    need[k] = v
        waits = [(k, v) for k, v in need.items()]
        self.ops[eng].append((waits, None, None, 0))

    def emit(self):
        nc = self.nc
        sems = self.sems
        ops = self.ops
        with nc.Block() as block:
            def run(e, lst):
                for waits, fn, key, inc in lst:
                    for k, v in waits:
                        e.wait_ge(sems[k], v)
                    if fn is not None:
                        fn(e).then_inc(sems[key], inc)

            @block.tensor
            def _(e):
                run(e, ops["pe"])

            @block.scalar
            def _(e):
                run(e, ops["act"])

            @block.vector
            def _(e):
                run(e, ops["dve"])

            @block.gpsimd
            def _(e):
                run(e, ops["pool"])

            @block.sync
            def _(e):
                self.pid_sp = e.partition_id() % 4
                run(e, ops["sp"])

    def close(self):
        self.es.close()


def _act(self, out, in_, func, R, W, scale=None, bias=None, accum=None):
    kw = {}
    if scale is not None:
        kw["scale"] = scale
    if bias is not None:
        kw["bias"] = bias
    if accum is not None:
        kw["accum_out"] = accum
    self.op("act", lambda e: e.activation(out=out, in_=in_, func=func, **kw), reads=R, writes=W)


def _tt(self, eng, out, in0, in1, op, R, W):
    self.op(eng, lambda e: e.tensor_tensor(out=out, in0=in0, in1=in1, op=op), reads=R, writes=W)


def _ts(self, eng, out, in0, s1, s2, op0, op1, R, W):
    if op1 is None:
        self.op(eng, lambda e: e.tensor_scalar(out=out, in0=in0, scalar1=s1, scalar2=None, op0=op0), reads=R, writes=W)
    else:
        self.op(eng, lambda e: e.tensor_scalar(out=out, in0=in0, scalar1=s1, scalar2=s2, op0=op0, op1=op1), reads=R, writes=W)


def _stt(self, out, in0, scalar, in1, op0, op1, R, W):
    self.op("dve", lambda e: e.scalar_tensor_tensor(out=out, in0=in0, scalar=scalar, in1=in1, op0=op0, op1=op1),
            reads=R, writes=W)


def _mm(self, out, lhsT, rhs, start, stop, R, W):
    self.op("pe", lambda e: e.matmul(out, lhsT=lhsT, rhs=rhs, start=start, stop=stop), reads=R, writes=W)


def _tr(self, out, in_, ident, R, W):
    self.op("pe", lambda e: e.transpose(out=out, in_=in_, identity=ident), reads=R, writes=W)


def _cp(self, eng, out, in_, R, W):
    if eng == "act":
        self.op("act", lambda e: e.copy(out=out, in_=in_), reads=R, writes=W)
    else:
        self.op(eng, lambda e: e.tensor_copy(out=out, in_=in_), reads=R, writes=W)


def _recip(self, out, in_, R, W):
    self.op("dve", lambda e: e.reciprocal(out=out, in_=in_), reads=R, writes=W)


KB.act = _act
KB.tt = _tt
KB.ts = _ts
KB.stt = _stt
KB.mm = _mm
KB.tr = _tr
KB.cp = _cp
KB.recip = _recip


D = 1024
NT = 16
TOK = 2048
RMS_EPS = 1e-6


def make_ident(k, name="ident32", dt=F32):
    ident = k.sb(name, [128, 128], dt)
    k.op("pool", lambda e: e.memset(ident[:], 0.0), writes=[ident])
    k.op("pool", lambda e: e.affine_select(out=ident[:], in_=ident[:], pattern=[[-1, 128]],
                                           compare_op=ALU.not_equal, fill=1.0, base=0,
                                           channel_multiplier=1), reads=[ident], writes=[ident])
    return ident


T = 8192
LN_EPS = 1e-5
GN_EPS = 64e-5
CW = 0.6065306597126334


def phase_mix0(nc, k, x, win, gcol, wsT, gng, bsc, lbl, ong, y_loc, yloc_t, gather):
    ntiles = 64
    ident = make_ident(k)
    identb = k.sb("identb", [128, 128], BF16)
    k.cp("dve", identb[:], ident[:], [ident], [identb])
    winb = k.sb("winb", [128, 8, 768], BF16)
    g1 = k.sb("g1", [128, 8], F32)
    wsf = k.sb("wsf", [128, 128], F32)
    wsb = k.sb("wsb", [128, 128], BF16)
    gnb = k.sb("gnb", [128, 128], F32)
    onb = k.sb("onb", [128, 128], F32)
    bs = k.sb("bs", [128, 1], F32)
    lb2 = k.sb("lb2", [128, 2], F32)
    lbd = k.sb("lbd", [128, 1], F32)
    lb = k.sb("lb", [128, 1], F32)
    oml = k.sb("oml", [128, 1], F32)
    maskT = k.sb("maskT", [128, 128], F32)
    rmask = k.sb("rmask", [128, 128], F32)
    epsr = k.sb("epsr", [128, 1], F32)
    epsl = k.sb("epsl", [128, 1], F32)
    S32 = k.sb("S32", [128, 128], F32)
    Sbf = [k.sb(f"Sbf{i}", [128, 128], BF16) for i in range(4)]

    win_v = win.rearrange("(c p) f -> p c f", p=128)
    for c in range(0, 8, 2):
        k.dma("pool", winb[:, c:c + 2, :], win_v[:, c:c + 2, :], writes=[winb])
    k.dma("sp", g1[:], gcol, writes=[g1])
    k.dma("sp", wsf[:], wsT, writes=[wsf])
    k.dma("sp", gnb[:], gng.partition_broadcast(128), writes=[gnb])
    k.dma("sp", onb[:], ong.partition_broadcast(128), writes=[onb])
    k.dma("sp", bs[:], bsc, writes=[bs])
    k.dma("sp", lb2[:], lbl, writes=[lb2])

    k.op("dve", lambda e: e.memset(epsr[:], RMS_EPS), writes=[epsr])
    k.op("dve", lambda e: e.memset(epsl[:], LN_EPS), writes=[epsl])
    k.op("dve", lambda e: e.memset(S32[:], 0.0), writes=[S32])
    k.op("dve", lambda e: e.memset(Sbf[0][:], 0.0), writes=[Sbf[0]])
    k.op("pool", lambda e: e.affine_select(out=wsf[:], in_=wsf[:], pattern=[[1, 128]], compare_op=ALU.is_ge,
                                           fill=0.0, base=0, channel_multiplier=-1), reads=[wsf], writes=[wsf])
    k.cp("dve", wsb[:], wsf[:], [wsf], [wsb])
    k.op("pool", lambda e: e.memset(maskT[:], 1.0), writes=[maskT])
    k.op("pool", lambda e: e.affine_select(out=maskT[:], in_=maskT[:], pattern=[[1, 128]], compare_op=ALU.is_ge,
                                           fill=0.0, base=0, channel_multiplier=-1), reads=[maskT], writes=[maskT])
    for n in range(1, 4):
        k.op("pool", lambda e, n=n: e.affine_select(out=maskT[:, 32 * n:32 * n + 32], in_=maskT[:, 32 * n:32 * n + 32],
                                                    pattern=[[0, 32]], compare_op=ALU.is_ge, fill=0.0,
                                                    base=-32 * n, channel_multiplier=1),
             reads=[maskT], writes=[maskT])
    k.op("pool", lambda e: e.memset(rmask[:], 1.0), writes=[rmask])
    for n in range(4):
        k.op("pool", lambda e, n=n: e.memset(rmask[:, 32 * n:32 * n + 1], 0.0), reads=[rmask], writes=[rmask])
    rm3 = k.sb("rm3", [128, 1], F32)
    k.op("pool", lambda e: e.memset(rm3[:], 1.0), writes=[rm3])
    k.op("pool", lambda e: e.affine_select(out=rm3[:], in_=rm3[:], pattern=[[0, 1]], compare_op=ALU.is_ge, fill=0.0,
                                           base=-96, channel_multiplier=1), reads=[rm3], writes=[rm3])
    qdec3 = k.sb("qdec3", [128, 64], BF16)
    k.op("pool", lambda e: e.memset(qdec3[:], 0.0), writes=[qdec3])
    kend3 = k.sb("kend3", [128, 128], BF16)
    k.tt("dve", lbd[:], lb2[:, 0:1], lb2[:, 1:2], ALU.subtract, [lb2], [lbd])
    k.act(lb[:], lbd[:], AF.Sigmoid, [lbd], [lb])
    k.ts("dve", oml[:], lb[:], -1.0, 1.0, ALU.mult, ALU.add, [lb], [oml])

    xt = [k.sb(f"xt{i}", [128, D], F32) for i in range(3)]
    junk = k.sb("junk", [128, D], F32)
    xn32 = [k.sb(f"xn32_{i}", [128, D], F32) for i in range(2)]
    xnT = [k.sb(f"xnT{i}", [128, 8, 128], BF16) for i in range(2)]
    ss = k.sb("ss", [128, 1], F32)
    sd = k.sb("sd", [128, 1], F32)
    rstd = k.sb("rstd", [128, 1], F32)
    uv = k.sb("uv", [128, 256], F32)
    bst = k.sb("bst", [128, 6], F32)
    mv = k.sb("mv", [128, 2], F32)
    sdv = k.sb("sdv", [128, 1], F32)
    rv = k.sb("rv", [128, 1], F32)
    vn = k.sb("vn", [128, 128], F32)
    vnb = k.sb("vnb", [128, 128], BF16)
    ycat = k.sb("ycat", [128, 256], F32)
    sigT = k.sb("sigT", [128, 128], F32)
    fT = k.sb("fT", [128, 128], F32)
    logfT = k.sb("logfT", [128, 128], F32)
    bT = k.sb("bT", [128, 128], F32)
    E = k.sb("E", [128, 128], F32)
    Einv = k.sb("Einv", [128, 128], F32)
    sq = k.sb("sq", [128, 128], F32)
    qdec = k.sb("qdec", [128, 128], BF16)
    omf = k.sb("omf", [128, 128], F32)
    kdec32 = k.sb("kdec32", [128, 128], F32)
    kdecb = k.sb("kdecb", [128, 128], BF16)
    dcol = k.sb("dcol", [128, 4], F32)
    kendT = k.sb("kendT", [128, 128], BF16)
    kend = k.sb("kend", [128, 128], BF16)
    vb = k.sb("vb", [128, 128], BF16)
    scT = k.sb("scT", [128, 128], BF16)
    sso = k.sb("sso", [128, 1], F32)
    sdo = k.sb("sdo", [128, 1], F32)
    ro = k.sb("ro", [128, 1], F32)
    on = k.sb("on", [128, 128], F32)
    sgt = k.sb("sgt", [128, 128], F32)
    junk2 = k.sb("junk2", [128, 128], F32)
    yTb = [k.sb(f"yTb{i}", [128, 2, 512], BF16) for i in range(2)]

    pt = k.ps("pt", [128, D], F32)
    ptok = k.ps("ptok", [128, 512], F32)
    pqf = k.ps("pqf", [128, 512], F32)
    pE = k.ps("pE", [128, 512], F32)
    pEb = k.ps("pEb", [128, 1024], BF16)
    po = k.ps("po", [128, 512], F32)
    pinc = [k.ps(f"pinc{i}", [128, 512], F32) for i in range(1)]
    pm_t, psc_t, pTy_t = k.trk("pm", pE), k.trk("psc", pE), k.trk("pTy", pE)

    x_v = x.rearrange("(n p) d -> n p d", p=128)
    scur = 0
    for i in range(ntiles):
        xb = xt[i % 3]
        k.dma("sp", xb[:], x_v[i], writes=[xb])
        k.act(junk[:], xb[:], AF.Square, [xb], [junk, ss], accum=ss[:])
        k.act(sd[:], ss[:], AF.Sqrt, [ss, epsr], [sd], scale=1.0 / D, bias=epsr[:])
        k.recip(rstd[:], sd[:], [sd], [rstd])
        xn = xn32[i % 2]
        k.act(xn[:], xb[:], AF.Copy, [xb, rstd], [xn], scale=rstd[:])
        for dc in range(8):
            k.tr(pt[:, dc * 128:(dc + 1) * 128], xn[:, dc * 128:(dc + 1) * 128], ident[:], [xn, ident], [pt])
        xT = xnT[i % 2]
        for dc in range(8):
            k.ts("dve", xT[:, dc, :], pt[:, dc * 128:(dc + 1) * 128], g1[:, dc:dc + 1], None, ALU.mult, None,
                 [pt, g1], [xT])
        for dc in range(8):
            k.mm(ptok[:], xT[:, dc, :], winb[:, dc, 0:512], dc == 0, dc == 7, [xT, winb], [ptok])
        for dc in range(8):
            k.mm(pqf[:, 0:128], winb[:, dc, 512:640], xT[:, dc, :], dc == 0, dc == 7, [xT, winb], [pqf])
        for dc in range(8):
            k.mm(pqf[:, 128:256], winb[:, dc, 640:768], xT[:, dc, :], dc == 0, dc == 7, [xT, winb], [pqf])
        k.act(uv[:], ptok[:, 0:256], AF.Gelu_apprx_tanh, [ptok], [uv])
        k.op("dve", lambda e: e.bn_stats(out=bst[:], in_=uv[:, 128:256]), reads=[uv], writes=[bst])
        k.op("dve", lambda e: e.bn_aggr(out=mv[:], in_=bst[:]), reads=[bst], writes=[mv])
        k.act(sdv[:], mv[:, 1:2], AF.Sqrt, [mv, epsl], [sdv], scale=1.0, bias=epsl[:])
        k.recip(rv[:], sdv[:], [sdv], [rv])
        k.ts("dve", vn[:], uv[:, 128:256], mv[:, 0:1], rv[:, 0:1], ALU.subtract, ALU.mult, [uv, mv, rv], [vn])
        k.tt("pool", vnb[:], vn[:], gnb[:], ALU.mult, [vn, gnb], [vnb])
        k.mm(pE[:, 0:128], wsb[:], vnb[:], True, True, [wsb, vnb], [pm_t])
        k.stt(ycat[:, 0:128], pE[:, 0:128], bs[:, 0:1], uv[:, 0:128], ALU.add, ALU.mult, [pm_t, bs, uv], [ycat])
        k.act(sigT[:], pqf[:, 128:256], AF.Sigmoid, [pqf], [sigT])
        k.act(sq[:], pqf[:, 0:128], AF.Silu, [pqf], [sq])
        k.act(sgt[:], ptok[:, 384:512], AF.Silu, [ptok], [sgt])
        k.cp("act", vb[:], ptok[:, 256:384], [ptok], [vb])
        k.ts("dve", fT[:], sigT[:], oml[:, 0:1], lb[:, 0:1], ALU.mult, ALU.add, [sigT, oml, lb], [fT])
        k.act(logfT[:], fT[:], AF.Ln, [fT], [logfT])
        k.op("dve", lambda e: e.tensor_tensor_scan(out=bT[:], data0=rmask[:], data1=logfT[:], initial=0.0,
                                                   op0=ALU.mult, op1=ALU.add), reads=[rmask, logfT], writes=[bT])
        k.act(E[:], bT[:], AF.Exp, [bT], [E])
        k.act(Einv[:], bT[:], AF.Exp, [bT], [Einv], scale=-1.0)
        k.op("act", lambda e: e.activation(out=dcol[:], in_=bT[:, 31:128:32], func=AF.Exp), reads=[bT], writes=[dcol])
        k.tt("dve", qdec[:], sq[:], E[:], ALU.mult, [sq, E], [qdec])
        k.cp("pool", qdec3[:, 32:64], qdec[:, 96:128], [qdec], [qdec3])
        k.ts("pool", omf[:], fT[:], -1.0, 1.0, ALU.mult, ALU.add, [fT], [omf])
        k.tt("dve", kdec32[:], omf[:], Einv[:], ALU.mult, [omf, Einv], [kdec32])
        k.cp("pool", kdecb[:], kdec32[:], [kdec32], [kdecb])
        for n in range(4):
            k.ts("dve", kendT[:, 32 * n:32 * n + 32], kdec32[:, 32 * n:32 * n + 32], dcol[:, n:n + 1], None, ALU.mult, None,
                 [kdec32, dcol], [kendT])
        k.tr(pEb[:, 0:128], kendT[:], identb[:], [kendT, identb], [pEb])
        k.cp("act", kend[:], pEb[:, 0:128], [pEb], [kend])
        k.ts("dve", kend3[:], pEb[:, 0:128], rm3[:, 0:1], None, ALU.mult, None, [pEb, rm3], [kend3])
        k.mm(pE[:, 128:256], kdecb[:], qdec[:], True, True, [kdecb, qdec], [psc_t])
        k.tt("dve", scT[:], pE[:, 128:256], maskT[:], ALU.mult, [psc_t, maskT], [scT])
        k.mm(po[:, 0:128], scT[:], vb[:], True, False, [scT, vb], [po])
        for n in range(4):
            sb_cur = Sbf[scur]
            pi_ = pinc[0]
            if n < 3:
                k.mm(po[32 * n:32 * n + 32, 0:128], qdec[:, 32 * n:32 * n + 32], sb_cur[:], False, False,
                     [qdec, sb_cur], [po])
                k.mm(pi_[:, 0:128], kend[32 * n:32 * n + 32, :], vb[32 * n:32 * n + 32, :], True, True, [kend, vb], [pi_])
            else:
                k.mm(po[64:128, 0:128], qdec3[:, 0:64], sb_cur[:], False, True, [qdec3, sb_cur], [po])
                k.mm(pi_[:, 0:128], kend3[64:128, :], vb[64:128, :], True, True, [kend3, vb], [pi_])
            k.stt(S32[:], S32[:], dcol[:, n:n + 1], pi_[:, 0:128], ALU.mult, ALU.add, [S32, dcol, pi_], [S32])
            scur = (scur + 1) % 4
            k.cp("act", Sbf[scur][:], S32[:], [S32], [Sbf[scur]])
        k.act(junk2[:], po[:, 0:128], AF.Square, [po], [junk2, sso], accum=sso[:])
        k.act(sdo[:], sso[:], AF.Sqrt, [sso, epsr], [sdo], scale=1.0 / 128, bias=epsr[:])
        k.recip(ro[:], sdo[:], [sdo], [ro])
        k.stt(on[:], po[:, 0:128], ro[:, 0:1], onb[:], ALU.mult, ALU.mult, [po, ro, onb], [on])
        k.tt("dve", ycat[:, 128:256], on[:], sgt[:], ALU.mult, [on, sgt], [ycat])
        for c in range(2):
            k.tr(pE[:, 256 + c * 128:256 + (c + 1) * 128], ycat[:, c * 128:(c + 1) * 128], ident[:], [ycat, ident], [pTy_t])
        yb = yTb[(i // 4) % 2]
        j = i % 4
        k.op("act", lambda e, yb=yb, j=j: e.copy(out=yb[:, :, j * 128:(j + 1) * 128],
                                                in_=pE[:, 256:512].rearrange("p (c t) -> p c t", c=2)),
             reads=[pTy_t], writes=[yb])
        if j == 3:
            qr = i // 16
            off = ((i % 16) // 4) * 512
            k.dma("sp", y_loc[qr].rearrange("(c p) t -> p c t", p=128)[:, :, off:off + 512], yb[:, :, :], reads=[yb],
                  writes=[yloc_t[qr]], sem=f"yTb{(i // 4) % 2}")
            if i % 16 == 15:
                gather(qr)


def phase_rwkv(nc, k, hn_all, hnall_t, mixc, wproj, w2, a2, g2, cvec, lng, lnb, y_loc, yloc_t, gather):
    ntiles = 64
    ident = make_ident(k)
    identb = k.sb("identb", [128, 128], BF16)
    k.cp("dve", identb[:], ident[:], [ident], [identb])
    ident4 = k.sb("ident4", [128, 4, 128], F32)
    for h in range(4):
        k.cp("pool", ident4[:, h, :], ident[:], [ident], [ident4])

    mx6 = k.sb("mx6", [128, 8, 6], F32)
    omx6 = k.sb("omx6", [128, 8, 6], F32)
    k.dma("sp", mx6[:], mixc, writes=[mx6])
    k.ts("dve", omx6[:], mx6[:], -1.0, 1.0, ALU.mult, ALU.add, [mx6], [omx6])
    Wa = k.sb("Wa", [128, 8, 1024], BF16)
    Wb = k.sb("Wb", [128, 8, 1024], BF16)
    wst = [k.sb(f"wst{i}", [128, 1024], F32) for i in range(2)]
    blocks = [(0, 256, 0), (256, 512, 2), (512, 768, 3), (768, 832, 1), (832, 896, 4), (896, 1024, 5)]
    wp_v = wproj.rearrange("(c p) f -> p c f", p=128)
    for dc in range(8):
        st = wst[dc % 2]
        k.dma("sp", st[:], wp_v[:, dc, :], writes=[st])
        for (c0, c1, mi) in blocks:
            k.ts("dve", Wb[:, dc, c0:c1], st[:, c0:c1], mx6[:, dc, mi:mi + 1], None, ALU.mult, None, [st, mx6], [Wb])
            k.ts("pool", Wa[:, dc, c0:c1], st[:, c0:c1], omx6[:, dc, mi:mi + 1], None, ALU.mult, None, [st, omx6], [Wa])
    w2b = k.sb("w2b", [128, 256], BF16)
    a2b = k.sb("a2b", [128, 256], BF16)
    g2b = k.sb("g2b", [128, 256], BF16)
    k.dma("pool", w2b[0:64, :], w2, writes=[w2b])
    k.dma("pool", a2b[64:128, :], a2, writes=[a2b])
    k.dma("pool", g2b[:], g2, writes=[g2b])
    cv = k.sb("cv", [128, 2, 5], F32)
    k.dma("sp", cv[:], cvec, writes=[cv])
    omka = k.sb("omka", [128, 2], F32)
    k.ts("dve", omka[:], cv[:, :, 3], -1.0, 1.0, ALU.mult, ALU.add, [cv], [omka])
    lngb = k.sb("lngb", [128, 256], F32)
    lnbb = k.sb("lnbb", [128, 256], F32)
    k.dma("sp", lngb[:], lng.partition_broadcast(128), writes=[lngb])
    k.dma("sp", lnbb[:], lnb.partition_broadcast(128), writes=[lnbb])

    def mk_mask(name, strict):
        m = k.sb(name, [128, 128], F32)
        k.op("pool", lambda e: e.memset(m[:], 1.0), writes=[m])
        k.op("pool", lambda e: e.affine_select(out=m[:], in_=m[:], pattern=[[1, 128]],
                                               compare_op=ALU.is_gt if strict else ALU.is_ge, fill=0.0, base=0,
                                               channel_multiplier=-1), reads=[m], writes=[m])
        k.op("pool", lambda e: e.affine_select(out=m[:, 64:128], in_=m[:, 64:128], pattern=[[0, 64]],
                                               compare_op=ALU.is_ge, fill=0.0, base=-64, channel_multiplier=1),
             reads=[m], writes=[m])
        return m
    maskS = mk_mask("maskS", True)
    maskI = mk_mask("maskI", False)
    maskL = k.sb("maskL", [128, 128], F32)
    k.op("pool", lambda e: e.memset(maskL[:], 1.0), writes=[maskL])
    k.op("pool", lambda e: e.affine_select(out=maskL[:], in_=maskL[:], pattern=[[-1, 128]], compare_op=ALU.is_gt,
                                           fill=0.0, base=0, channel_multiplier=1), reads=[maskL], writes=[maskL])
    k.op("pool", lambda e: e.affine_select(out=maskL[:, 0:64], in_=maskL[:, 0:64], pattern=[[0, 64]],
                                           compare_op=ALU.is_ge, fill=0.0, base=63, channel_multiplier=-1),
         reads=[maskL], writes=[maskL])
    maskL4 = k.sb("maskL4", [128, 4, 128], F32)
    for h in range(4):
        k.cp("pool", maskL4[:, h, :], maskL[:], [maskL], [maskL4])
    mask4 = k.sb("mask4", [128, 512], F32)
    k.cp("pool", mask4[:, 0:128], maskS[:], [maskS], [mask4])
    k.cp("pool", mask4[:, 128:256], maskI[:], [maskI], [mask4])
    k.cp("pool", mask4[:, 256:384], maskS[:], [maskS], [mask4])
    k.ts("pool", mask4[:, 384:512], maskI[:], -1.0, None, ALU.mult, None, [maskI], [mask4])
    rmask = k.sb("rmask", [128, 2, 128], F32)
    k.op("pool", lambda e: e.memset(rmask[:], 1.0), writes=[rmask])
    for p in range(2):
        for c in range(2):
            k.op("pool", lambda e, p=p, c=c: e.memset(rmask[:, p, 64 * c:64 * c + 1], 0.0), reads=[rmask], writes=[rmask])
    bones = k.sb("bones", [128, 128], BF16)
    k.op("pool", lambda e: e.memset(bones[:], 0.0), writes=[bones])
    k.op("pool", lambda e: e.memset(bones[0:64, 0:64], 1.0), reads=[bones], writes=[bones])
    k.op("pool", lambda e: e.memset(bones[64:128, 64:128], 1.0), reads=[bones], writes=[bones])
    epsg = k.sb("epsg", [128, 1], F32)
    k.op("dve", lambda e: e.memset(epsg[:], GN_EPS), writes=[epsg])
    epsk = k.sb("epsk", [128, 1], F32)
    k.op("dve", lambda e: e.memset(epsk[:], 1e-24), writes=[epsk])

    M32 = k.sb("M32", [128, 2, 64], F32)
    Mb = [k.sb(f"Mb{i}", [128, 4, 64], BF16) for i in range(2)]
    k.op("dve", lambda e: e.memset(M32[:], 0.0), writes=[M32])
    k.op("dve", lambda e: e.memset(Mb[0][:], 0.0), writes=[Mb[0]])
    hmask = k.sb("hmask", [128, 2], F32)
    k.op("pool", lambda e: e.memset(hmask[:], 0.0), writes=[hmask])
    k.op("pool", lambda e: e.memset(hmask[0:64, 0:1], 1.0), reads=[hmask], writes=[hmask])
    k.op("pool", lambda e: e.memset(hmask[64:128, 1:2], 1.0), reads=[hmask], writes=[hmask])
    mcur = 0

    hx = [k.sb(f"hx{i}", [128, 8, 128], BF16) for i in range(2)]
    hs = k.sb("hs", [128, 8, 128], BF16)
    k.op("pool", lambda e: e.memset(hs[:, :, 0:1], 0.0), writes=[hs])
    l1 = k.sb("l1", [128, 128], BF16)
    sg1 = k.sb("sg1", [128, 128], BF16)
    sigw = k.sb("sigw", [128, 2, 128], F32)
    a_ = k.sb("a_", [128, 2, 128], F32)
    kku = k.sb("kku", [128, 2, 128], F32)
    sqk = k.sb("sqk", [128, 2, 128], BF16)
    sdk = k.sb("sdk", [128, 2, 128], F32)
    rn = k.sb("rn", [128, 2, 128], F32)
    kk = k.sb("kk", [128, 2, 128], F32)
    b_ = k.sb("b_", [128, 2, 128], F32)
    kmf = k.sb("kmf", [128, 2, 128], F32)
    kmod = k.sb("kmod", [128, 2, 128], F32)
    r32 = k.sb("r32", [128, 2, 128], F32)
    gc = k.sb("gc", [128, 2, 128], F32)
    gcx = k.sb("gcx", [128, 2, 128], F32)
    E = k.sb("E", [128, 2, 128], F32)
    Einv = k.sb("Einv", [128, 2, 128], F32)
    Eex = k.sb("Eex", [128, 2, 128], F32)
    KR2 = [k.sb(f"KR{_i}", [128, 2, 2, 128], BF16) for _i in range(2)]
    Ki32 = k.sb("Ki32", [128, 2, 128], F32)
    Bi32 = k.sb("Bi32", [128, 2, 128], F32)
    Ki = k.sb("Ki", [128, 2, 128], BF16)
    Bi = k.sb("Bi", [128, 2, 128], BF16)
    KeT = k.sb("KeT", [128, 2, 128], BF16)
    nBeT = k.sb("nBeT", [128, 2, 128], BF16)
    dcol2 = [k.sb(f"dcol{_i}", [128, 2, 2], F32) for _i in range(2)]
    ndcol = k.sb("ndcol", [128, 2, 2], F32)
    rk = k.sb("rk", [128, 2, 128], BF16)
    rk32 = k.sb("rk32", [128, 2, 128], F32)
    AT2 = [k.sb(f"AT{_i}", [128, 4, 512], BF16) for _i in range(2)]
    P = [k.sb(f"P{i}", [128, 4, 128], BF16) for i in range(2)]
    PT = [k.sb(f"PT{i}", [128, 4, 128], BF16) for i in range(2)]
    R = [k.sb(f"R{i}", [128, 4, 128], BF16) for i in range(2)]
    Vb2 = [k.sb(f"Vb{_i}", [128, 256], BF16) for _i in range(2)]
    V322 = [k.sb(f"V32{_i}", [128, 256], F32) for _i in range(2)]
    Wt = k.sb("Wt", [128, 256], BF16)
    Ut = k.sb("Ut", [128, 256], BF16)
    k.op("dve", lambda e: e.memset(Wt[:], 0.0), writes=[Wt])
    k.op("dve", lambda e: e.memset(Ut[:], 0.0), writes=[Ut])
    Ke2 = [k.sb(f"Ke{_i}", [128, 256], BF16) for _i in range(2)]
    nBe2 = [k.sb(f"nBe{_i}", [128, 256], BF16) for _i in range(2)]
    bon2 = [k.sb(f"bon{_i}", [128, 4], F32) for _i in range(2)]
    bst = k.sb("bst", [128, 4, 6], F32)
    mv = k.sb("mv", [128, 4, 2], F32)
    sdg = k.sb("sdg", [128, 4], F32)
    rg = k.sb("rg", [128, 4], F32)
    yn = k.sb("yn", [128, 256], F32)
    gate2 = [k.sb(f"gate{_i}", [128, 256], F32) for _i in range(2)]
    Rfin2 = [k.sb(f"Rfin{_i}", [128, 4, 128], BF16) for _i in range(2)]
    yg = k.sb("yg", [128, 256], F32)
    yTb = [k.sb(f"yTb{i}", [128, 2, 512], BF16) for i in range(2)]

    b0 = k.ps("b0", [128, 512], F32)
    b1 = k.ps("b1", [128, 512], F32)
    b2 = k.ps("b2", [128, 512], F32)
    b3 = k.ps("b3", [128, 512], F32)
    b4 = k.ps("b4", [128, 512], F32)
    b5 = k.ps("b5", [128, 512], F32)
    b5b = None
    b6 = k.ps("b6", [128, 512], F32)
    b7 = k.ps("b7", [128, 512], F32)
    b45b = k.ps("b45b", [128, 1024], BF16) if False else None


    def bind(i):
        return (KR2[i % 2], AT2[i % 2], Vb2[i % 2], V322[i % 2], Ke2[i % 2], nBe2[i % 2], dcol2[i % 2], bon2[i % 2],
                gate2[i % 2], Rfin2[i % 2])

    def stageA(i):
        KR, AT, Vb, V32, Ke, nBe, dcol, bon, gate, Rfin = bind(i)
        t0 = i * 128
        hxc = hx[i % 2]
        hxp = hx[(i + 1) % 2]
        rr, mm_, tt0 = i // 16, (i % 16) // 4, (i % 4) * 128
        k.dma("sp", hxc[:], hn_all[mm_][rr].rearrange("(c p) t -> p c t", p=128)[:, :, tt0:tt0 + 128],
              reads=[hnall_t[mm_]], writes=[hxc], sem=hxc.trk.name)
        k.cp("pool", hs[:, :, 1:128], hxc[:, :, 0:127], [hxc], [hs])
        if i > 0:
            k.cp("pool", hs[:, :, 0:1], hxp[:, :, 127:128], [hxp], [hs])
        def proj(out, c0, c1, fm):
            for dc in range(8):
                for (Wx, hh) in ((Wa, hxc), (Wb, hs)):
                    first = dc == 0 and Wx is Wa
                    last = dc == 7 and Wx is Wb
                    if fm:
                        k.mm(out, Wx[:, dc, c0:c1], hh[:, dc, :], first, last, [Wx, hh], [out_b])
                    else:
                        k.mm(out, hh[:, dc, :], Wx[:, dc, c0:c1], first, last, [Wx, hh], [out_b])
        out_b = b0
        for j in range(4):
            proj(b0[:, j * 128:(j + 1) * 128], j * 128, (j + 1) * 128, True)
        yield
        out_b = b1
        proj(b1[:, 0:128], 768, 896, True)
        proj(b1[:, 128:256], 896, 1024, True)
        yield
        out_b = b2
        proj(b2[:, 0:256], 512, 768, False)
        yield
        k.act(l1[0:64, :], b1[0:64, 0:128], AF.Tanh, [b1], [l1])
        k.cp("act", l1[64:128, :], b1[64:128, 0:128], [b1], [l1])
        k.act(sg1[:], b1[:, 128:256], AF.Sigmoid, [b1], [sg1])
        k.cp("act", Vb[:], b2[:, 0:256], [b2], [Vb])
        k.cp("dve", V32[:], b2[:, 0:256], [b2], [V32])
        yield
        for p in range(2):
            k.mm(b3[:, p * 128:(p + 1) * 128], w2b[0:64, p * 128:(p + 1) * 128], l1[0:64, :], True, True, [w2b, l1], [b3])
        for p in range(2):
            k.mm(b4[:, p * 128:(p + 1) * 128], a2b[64:128, p * 128:(p + 1) * 128], l1[64:128, :], True, True,
                 [a2b, l1], [b4])
        k.mm(b2[:, 256:512], sg1[:], g2b[:], True, True, [sg1, g2b], [b2])
        k.cp("act", gate[:], b2[:, 256:512], [b2], [gate])
        for p in range(2):
            k.act(sigw[:, p, :], b3[:, p * 128:(p + 1) * 128], AF.Sigmoid, [b3, cv], [sigw], bias=cv[:, p, 0:1])
            k.act(a_[:, p, :], b4[:, p * 128:(p + 1) * 128], AF.Sigmoid, [b4, cv], [a_], bias=cv[:, p, 1:2])
        yield
        k.cp("act", r32[:], b0[:, 0:256].rearrange("p (a t) -> p a t", a=2), [b0], [r32])
        for p in range(2):
            k.ts("dve", kku[:, p, :], b0[:, 256 + p * 128:256 + (p + 1) * 128], cv[:, p, 2:3], None, ALU.mult, None,
                 [b0, cv], [kku])
        k.tt("pool", sqk[:], kku[:], kku[:], ALU.mult, [kku], [sqk])
        for p in range(2):
            k.mm(b3[:, p * 128:(p + 1) * 128], bones[:], sqk[:, p, :], True, True, [bones, sqk], [b3])
        k.act(sdk[:], b3[:, 0:256].rearrange("p (a t) -> p a t", a=2), AF.Sqrt, [b3, epsk], [sdk], bias=epsk[:], scale=1.0)
        k.recip(rn[:], sdk[:], [sdk], [rn])
        k.tt("dve", kk[:], kku[:], rn[:], ALU.mult, [kku, rn], [kk])
        k.tt("pool", b_[:], kk[:], a_[:], ALU.mult, [kk, a_], [b_])
        for p in range(2):
            k.ts("pool", kmf[:, p, :], a_[:, p, :], cv[:, p, 3:4], omka[:, p:p + 1], ALU.mult, ALU.add, [a_, cv, omka], [kmf])
        k.tt("dve", kmod[:], kmf[:], b0[:, 256:512].rearrange("p (a t) -> p a t", a=2), ALU.mult, [kmf, b0], [kmod])
        yield
        for p in range(2):
            k.op("dve", lambda e, p=p: e.tensor_tensor_scan(out=gc[:, p, :], data0=rmask[:, p, :], data1=sigw[:, p, :],
                                                          initial=0.0, op0=ALU.mult, op1=ALU.add),
                 reads=[rmask, sigw], writes=[gc])
        k.tt("pool", gcx[:], gc[:], sigw[:], ALU.subtract, [gc, sigw], [gcx])
        k.act(E[:], gc[:], AF.Exp, [gc], [E], scale=-CW)
        k.act(Einv[:], gc[:], AF.Exp, [gc], [Einv], scale=CW)
        k.act(Eex[:], gcx[:], AF.Exp, [gcx], [Eex], scale=-CW)
        k.cp("pool", dcol[:], E[:, :, 63:128:64], [E], [dcol])
        k.ts("pool", ndcol[:], dcol[:], -1.0, None, ALU.mult, None, [dcol], [ndcol])
        k.tt("dve", KR[:, :, 0, :], kk[:], Eex[:], ALU.mult, [kk, Eex], [KR])
        k.tt("dve", KR[:, :, 1, :], r32[:], E[:], ALU.mult, [r32, E], [KR])
        k.tt("dve", Ki32[:], kmod[:], Einv[:], ALU.mult, [kmod, Einv], [Ki32])
        k.tt("pool", Bi32[:], b_[:], Einv[:], ALU.mult, [b_, Einv], [Bi32])
        k.cp("act", Ki[:], Ki32[:], [Ki32], [Ki])
        k.cp("act", Bi[:], Bi32[:], [Bi32], [Bi])
        for p in range(2):
            for c in range(2):
                k.ts("dve", KeT[:, p, 64 * c:64 * c + 64], Ki32[:, p, 64 * c:64 * c + 64], dcol[:, p, c:c + 1], None,
                     ALU.mult, None, [Ki32, dcol], [KeT])
                k.ts("pool", nBeT[:, p, 64 * c:64 * c + 64], Bi32[:, p, 64 * c:64 * c + 64], ndcol[:, p, c:c + 1], None,
                     ALU.mult, None, [Bi32, ndcol], [nBeT])
        yield
        k.tt("pool", rk32[:], r32[:], kmod[:], ALU.mult, [r32, kmod], [rk32])
        for p in range(2):
            k.ts("pool", rk[:, p, :], rk32[:, p, :], cv[:, p, 4:5], None, ALU.mult, None, [rk32, cv], [rk])
        yield
        for p in range(2):
            k.mm(b4[:, p * 128:(p + 1) * 128], KeT[:, p, :], identb[:], True, True, [KeT, identb], [b4])
            k.mm(b4[:, 256 + p * 128:256 + (p + 1) * 128], nBeT[:, p, :], identb[:], True, True, [nBeT, identb], [b4])
        k.cp("act", Ke[:], b4[:, 0:256], [b4], [Ke])
        k.cp("act", nBe[:], b4[:, 256:512], [b4], [nBe])
        for p in range(2):
            k.mm(b3[:, 256 + 2 * p:256 + 2 * p + 2], rk[:, p, :], bones[:, 0:128:64], True, True, [rk, bones], [b3])
        k.cp("dve", bon[:], b3[:, 256:260], [b3], [bon])
        yield
        for h in range(4):
            p, q = h // 2, h % 2
            bk = b0 if h % 2 == 0 else b1
            ks = slice(q * 64, (q + 1) * 64)
            k.mm(bk[:, 0:256], Ki[ks, p, :], KR[ks, p, :, :].rearrange("k a t -> k (a t)"), True, True, [Ki, KR], [bk])
            k.mm(bk[:, 256:512], Bi[ks, p, :], KR[ks, p, :, :].rearrange("k a t -> k (a t)"), True, True, [Bi, KR], [bk])
            k.tt("dve", AT[:, h, :], bk[:], mask4[:], ALU.mult, [bk, mask4], [AT])
            k.mm(b4[:, h * 128:(h + 1) * 128], KR[ks, p, 0, :], Bi[ks, p, :], True, True, [KR, Bi], [b4])
            yield
        k.tt("dve", PT[0][:], b4[:].rearrange("p (h t) -> p h t", h=4), maskL4[:], ALU.mult, [b4, maskL4], [PT[0]])
        k.cp("pool", P[0][:], AT[:, :, 256:384], [AT], [P[0]])
        k.tt("pool", R[0][:], ident4[:], P[0][:], ALU.subtract, [ident4, P[0]], [R[0]])
        yield
        for j in range(5):
            pc, pn = P[j % 2], P[(j + 1) % 2]
            ptc, ptn = PT[j % 2], PT[(j + 1) % 2]
            rc, rn_ = R[j % 2], (Rfin if j == 4 else R[(j + 1) % 2])
            for h in range(4):
                k.mm(b0[:, h * 128:(h + 1) * 128], pc[:, h, :], ptc[:, h, :], True, True, [pc, ptc], [b0])
            k.cp("act", ptn[:], b0[:].rearrange("p (h t) -> p h t", h=4), [b0], [ptn])
            if j < 4:
                for h in range(4):
                    k.mm(b1[:, h * 128:(h + 1) * 128], ptc[:, h, :], pc[:, h, :], True, True, [pc, ptc], [b1])
                k.cp("act", pn[:], b1[:].rearrange("p (h t) -> p h t", h=4), [b1], [pn])
            for h in range(4):
                k.mm(b3[:, h * 128:(h + 1) * 128], ptn[:, h, :], rc[:, h, :], True, True, [ptn, rc], [b3])
            k.tt("dve", rn_[:], b3[:].rearrange("p (h t) -> p h t", h=4), rc[:], ALU.add, [b3, rc], [rn_])
            yield
        yield

    def stageB(i):
        nonlocal mcur
        KR, AT, Vb, V32, Ke, nBe, dcol, bon, gate, Rfin = bind(i)
        Rf = Rfin
        for c in range(2):
            cs = slice(64 * c, 64 * c + 64)
            mb = Mb[mcur]
            for h in range(4):
                p, q = h // 2, h % 2
                ks = slice(q * 64, (q + 1) * 64)
                hv = slice(h * 64, (h + 1) * 64)
                k.mm(b6[cs, hv], AT[:, h, 64 * c:64 * c + 64], Vb[:, hv], True, False, [AT, Vb], [b6])
                k.mm(b6[cs, hv], KR[:, p, 0, cs], mb[:, h, :], False, True, [KR, mb], [b6])
            k.cp("act", Wt[cs, :], b6[cs, 0:256], [b6], [Wt])
            yield
            for h in range(4):
                hv = slice(h * 64, (h + 1) * 64)
                k.mm(b6[cs, 256 + h * 64:256 + (h + 1) * 64], Rf[:, h, cs], Wt[:, hv], True, True, [Rf, Wt], [b6])
            k.cp("act", Ut[cs, :], b6[cs, 256:512], [b6], [Ut])
            yield
            for h in range(4):
                p, q = h // 2, h % 2
                ks = slice(q * 64, (q + 1) * 64)
                hv = slice(h * 64, (h + 1) * 64)
                k.mm(b7[cs, hv], KR[:, p, 1, cs], mb[:, h, :], True, False, [KR, mb], [b7])
                k.mm(b7[cs, hv], AT[:, h, 128 + 64 * c:128 + 64 * c + 64], Vb[:, hv], False, False, [AT, Vb], [b7])
                k.mm(b7[cs, hv], AT[:, h, 384 + 64 * c:384 + 64 * c + 64], Ut[:, hv], False, True, [AT, Ut], [b7])
            for h in range(4):
                p, q = h // 2, h % 2
                ks = slice(q * 64, (q + 1) * 64)
                hv = slice(h * 64, (h + 1) * 64)
                k.mm(b5[ks, p * 64:(p + 1) * 64], Ke[cs, hv], Vb[cs, hv], True, False, [Ke, Vb], [b5])
                k.mm(b5[ks, p * 64:(p + 1) * 64], nBe[cs, hv], Ut[cs, hv], False, True, [nBe, Ut], [b5])
            for p in range(2):
                k.stt(M32[:, p, :], M32[:, p, :], dcol[:, p, c:c + 1], b5[:, p * 64:(p + 1) * 64], ALU.mult, ALU.add,
                      [M32, dcol, b5], [M32])
            yield
            mcur = (mcur + 1) % 2
            for q in range(2):
                k.ts("pool", Mb[mcur][:, q:4:2, :], M32[:], hmask[:, q:q + 1], None, ALU.mult, None, [M32, hmask], [Mb[mcur]])
        yield
        for h in range(4):
            k.op("dve", lambda e, h=h: e.bn_stats(out=bst[:, h, :], in_=b7[:, h * 64:(h + 1) * 64]), reads=[b7], writes=[bst])
            k.op("dve", lambda e, h=h: e.bn_aggr(out=mv[:, h, :], in_=bst[:, h, :]), reads=[bst], writes=[mv])
        k.act(sdg[:], mv[:, :, 1], AF.Sqrt, [mv, epsg], [sdg], bias=epsg[:], scale=1.0)
        k.recip(rg[:], sdg[:], [sdg], [rg])
        for h in range(4):
            hv = slice(h * 64, (h + 1) * 64)
            k.ts("dve", yn[:, hv], b7[:, hv], mv[:, h, 0:1], rg[:, h:h + 1], ALU.subtract, ALU.mult, [b7, mv, rg], [yn])
        k.tt("pool", yn[:], yn[:], lngb[:], ALU.mult, [yn, lngb], [yn])
        k.tt("pool", yn[:], yn[:], lnbb[:], ALU.add, [yn, lnbb], [yn])
        for h in range(4):
            hv = slice(h * 64, (h + 1) * 64)
            k.stt(yn[:, hv], V32[:, hv], bon[:, h:h + 1], yn[:, hv], ALU.mult, ALU.add, [V32, bon, yn], [yn])
        k.tt("dve", yg[:], yn[:], gate[:], ALU.mult, [yn, gate], [yg])
        for c2 in range(2):
            k.tr(b5[:, c2 * 128:(c2 + 1) * 128], yg[:, c2 * 128:(c2 + 1) * 128], ident[:], [yg, ident], [b5])
        yb = yTb[(i // 4) % 2]
        j = i % 4
        k.op("act", lambda e, yb=yb, j=j: e.copy(out=yb[:, :, j * 128:(j + 1) * 128],
                                                in_=b5[:, 0:256].rearrange("p (c t) -> p c t", c=2)),
             reads=[b5], writes=[yb])
        if j == 3:
            qr = i // 16
            off = ((i % 16) // 4) * 512
            k.dma("sp", y_loc[qr].rearrange("(c p) t -> p c t", p=128)[:, :, off:off + 512], yb[:, :, :], reads=[yb],
                  writes=[yloc_t[qr]], sem=f"ygb{(i // 4) % 2}")
            if i % 16 == 15:
                gather(qr)
    def drain(g, n=None):
        c = 0
        while n is None or c < n:
            try:
                next(g)
            except StopIteration:
                return False
            c += 1
        return True

    drain(stageA(0))
    for i in range(ntiles):
        gB = stageB(i)
        gA = stageA(i + 1) if i + 1 < ntiles else None
        aliveA, aliveB = gA is not None, True
        while aliveA or aliveB:
            if aliveB:
                aliveB = drain(gB, 1)
            if aliveA:
                aliveA = drain(gA, 4)


def phase_ffn(nc, k, mode, hacc, hacc_t, outT, yall, yall_t, wo, g1c, wg, wu, wd, hin=None, g2c=None, hn_loc=None,
              hnloc_t=None, gather=None, rt=None, rb=None, gout=None, out=None):
    moe = mode == "moe"
    NE, FE = (8, 1408) if moe else (1, 2816)
    G = 4
    xnT = k.sb("xnT", [128, 8, TOK], BF16)
    wob = k.sb("wob", [128, 8, D], BF16)
    wgs = [k.sb(f"wgs{i}", [128, 8, G * 128], BF16) for i in range(2)]
    wus = [k.sb(f"wus{i}", [128, 8, G * 128], BF16) for i in range(2)]
    wds = [k.sb(f"wds{i}", [128, G, D], BF16) for i in range(2)]
    actT = [k.sb(f"actT{i}", [128, G, 512], BF16) for i in range(2)]
    sg = [k.sb(f"sg{i}", [128, 512], F32) for i in range(2)]
    xn32 = [k.sb(f"xn32_{i}", [128, D], F32) for i in range(2)]
    junk = k.sb("junk", [128, D], F32)
    ss = k.sb("ss", [128, NT], F32)
    sd = k.sb("sd", [128, NT], F32)
    rstd = k.sb("rstd", [128, NT], F32)
    g1 = k.sb("g1", [128, 8], F32)
    ident = make_ident(k)
    pso = [k.ps(f"pso{i}", [128, D], F32) for i in range(2)]
    psg = [k.ps(f"psg{i}", [128, 512], F32) for i in range(2)]
    psu = [k.ps(f"psu{i}", [128, 512], F32) for i in range(2)]
    xt_t = [k.trk(f"xt{i}") for i in range(NT)]
    if moe:
        rt32 = k.sb("rt32", [128, 8, NE], F32)
        rbb = k.sb("rbb", [128, NE], F32)
        xT32 = k.sb("xT32", [128, D], F32)
        lg = k.sb("lg", [128, NT, NE], F32)
        gates = k.sb("gates", [128, NT, NE], F32)
        mx = k.sb("mx", [128, 8], F32)
        nm1 = k.sb("nm1", [128, 1], F32)
        ex = k.sb("ex", [128, NE], F32)
        msk = k.sb("msk", [128, NE], F32)
        den = k.sb("den", [128, 1], F32)
        rden = k.sb("rden", [128, 1], F32)
        goutb = k.sb("goutb", [128, D], F32)
        xlo = k.sb("xlo", [128, 8, 128], BF16)
        rth = k.sb("rth", [128, 8, NE], BF16)
        rtl = k.sb("rtl", [128, 8, NE], BF16)
        rtd = k.sb("rtd", [128, 8, NE], F32)
        obuf = [k.sb(f"obuf{i}", [128, D], F32) for i in range(2)]
    else:
        g2 = k.sb("g2", [128, 8], F32)

    for m in range(4):
        def src(e, m=m):
            qd = bass.ds(k.pid_sp, 1)
            return yall[qd].rearrange("o r (h p) t -> p (o r h) t", p=128)[:, :, m * 512:(m + 1) * 512]
        k.dma("sp", xnT[:, :, m * 512:(m + 1) * 512], src, reads=yall_t, writes=xt_t[4 * m:4 * m + 4], sem=f"xt{m}")
    if not moe:
        hin_v = hin.rearrange("(n p) d -> p n d", p=128)
        for m in range(4):
            k.dma("act", hacc[:, 4 * m:4 * m + 4, :], hin_v[:, 4 * m:4 * m + 4, :],
                  writes=hacc_t[4 * m:4 * m + 4], sem=f"hacc{m}")
    for c in range(8):
        r0 = c * 128 if moe else ((c % 2) * 512 + (c // 2) * 128)
        k.dma("pool", wob[:, c, :], wo[r0:r0 + 128, :], writes=[wob])
    k.dma("sp", g1[:], g1c, writes=[g1])
    if moe:
        k.dma("sp", rt32[:], rt.rearrange("(c p) e -> p c e", p=128), writes=[rt32])
        k.dma("sp", rbb[:], rb.partition_broadcast(128), writes=[rbb])
        k.dma("sp", goutb[:], gout.partition_broadcast(128), writes=[goutb])
        k.cp("dve", rth[:], rt32[:], [rt32], [rth])
        k.tt("dve", rtd[:], rt32[:], rth[:], ALU.subtract, [rt32, rth], [rtd])
        k.cp("dve", rtl[:], rtd[:], [rtd], [rtl])
    else:
        k.dma("sp", g2[:], g2c, writes=[g2])

    passes = []
    for e in range(NE):
        nchunk = FE // 128
        c0 = 0
        while c0 < nchunk:
            g = min(G, nchunk - c0)
            passes.append((e, c0, g))
            c0 += g

    def load_pass(pi):
        e, c0, g = passes[pi]
        s = pi % 2
        wg_v = wg[e].rearrange("(c p) f -> p c f", p=128)
        wu_v = wu[e].rearrange("(c p) f -> p c f", p=128)
        wd_v = wd[e].rearrange("(c p) d -> p c d", p=128)
        for c in range(0, 8, 4):
            k.dma("pool", wgs[s][:, c:c + 4, 0:g * 128], wg_v[:, c:c + 4, c0 * 128:(c0 + g) * 128], writes=[wgs[s]])
        for c in range(0, 8, 4):
            k.dma("pool", wus[s][:, c:c + 4, 0:g * 128], wu_v[:, c:c + 4, c0 * 128:(c0 + g) * 128], writes=[wus[s]])
        for c in range(0, g, 2):
            c1 = min(c + 2, g)
            k.dma("pool", wds[s][:, c:c1, :], wd_v[:, c0 + c:c0 + c1, :], writes=[wds[s]])

    load_pass(0)

    for i in range(NT):
        p = pso[i % 2]
        for half in range(2):
            for dc in range(8):
                k.op("pe", lambda e, p=p, half=half, dc=dc, i=i: e.matmul(
                    p[:, half * 512:(half + 1) * 512], lhsT=xnT[:, dc, i * 128:(i + 1) * 128],
                    rhs=wob[:, dc, half * 512:(half + 1) * 512], start=(dc == 0), stop=(dc == 7)),
                    reads=[xt_t[i], wob], writes=[p])
        k.op("dve", lambda e, p=p, i=i: e.tensor_tensor(out=hacc[:, i, :], in0=p[:], in1=hacc[:, i, :], op=ALU.add),
             reads=[p, hacc_t[i]], writes=[hacc_t[i]])

    def norm_stats():
        for i in range(NT):
            k.op("act", lambda e, i=i: e.activation(out=junk[:], in_=hacc[:, i, :], func=AF.Square,
                                                     accum_out=ss[:, i:i + 1]),
                 reads=[hacc_t[i]], writes=[junk, ss])
        k.op("act", lambda e: e.activation(out=sd[:], in_=ss[:], func=AF.Sqrt, scale=1.0 / D, bias=epsb[:]),
             reads=[ss, epsb], writes=[sd])
        k.op("dve", lambda e: e.reciprocal(out=rstd[:], in_=sd[:]), reads=[sd], writes=[rstd])

    epsb = k.sb("epsb", [128, 1], F32)
    k.op("dve", lambda e: e.memset(epsb[:], RMS_EPS), writes=[epsb])

    def norm_transpose(gcol, dst_tile_fn, extra=None):
        for i in range(NT):
            xb = xn32[i % 2]
            p = pso[i % 2]
            k.op("act", lambda e, i=i, xb=xb: e.activation(out=xb[:], in_=hacc[:, i, :], func=AF.Copy,
                                                           scale=rstd[:, i:i + 1]),
                 reads=[hacc_t[i], rstd], writes=[xb])
            for dc in range(8):
                k.op("pe", lambda e, dc=dc, xb=xb, p=p: e.transpose(out=p[:, dc * 128:(dc + 1) * 128],
                                                                  in_=xb[:, dc * 128:(dc + 1) * 128],
                                                                  identity=ident[:]),
                     reads=[xb, ident], writes=[p])
            for dc in range(8):
                k.op("dve", lambda e, dc=dc, i=i, p=p: e.tensor_scalar(
                    out=xnT[:, dc, i * 128:(i + 1) * 128], in0=p[:, dc * 128:(dc + 1) * 128],
                    scalar1=gcol[:, dc:dc + 1], scalar2=None, op0=ALU.mult),
                    reads=[p, gcol], writes=[xt_t[i]])
            if extra is not None:
                extra(i, p)

    norm_stats()

    def router(i, p):
        pl = psg[i % 2]
        for dc in range(8):
            k.stt(xlo[:, dc, :], p[:, dc * 128:(dc + 1) * 128], g1[:, dc:dc + 1], xnT[:, dc, i * 128:(i + 1) * 128],
                  ALU.mult, ALU.subtract, [p, g1, xt_t[i]], [xlo])
        for dc in range(8):
            k.mm(pl[:, 0:NE], xnT[:, dc, i * 128:(i + 1) * 128], rth[:, dc, :], dc == 0, False, [xt_t[i], rth], [pl])
            k.mm(pl[:, 0:NE], xlo[:, dc, :], rth[:, dc, :], False, False, [xlo, rth], [pl])
            k.mm(pl[:, 0:NE], xnT[:, dc, i * 128:(i + 1) * 128], rtl[:, dc, :], False, dc == 7, [xt_t[i], rtl], [pl])
        k.op("dve", lambda e, i=i, pl=pl: e.tensor_tensor(out=lg[:, i, :], in0=pl[:, 0:NE], in1=rbb[:], op=ALU.add),
             reads=[pl, rbb], writes=[lg])
        k.op("dve", lambda e, i=i: e.max(out=mx[:], in_=lg[:, i, :]), reads=[lg], writes=[mx])
        k.op("dve", lambda e: e.tensor_scalar(out=nm1[:], in0=mx[:, 0:1], scalar1=-1.0, scalar2=None, op0=ALU.mult),
             reads=[mx], writes=[nm1])
        k.op("act", lambda e, i=i: e.activation(out=ex[:], in_=lg[:, i, :], func=AF.Exp, bias=nm1[:], scale=1.0),
             reads=[lg, nm1], writes=[ex])
        k.op("dve", lambda e, i=i: e.tensor_scalar(out=msk[:], in0=lg[:, i, :], scalar1=mx[:, 1:2], scalar2=None,
                                                 op0=ALU.is_ge), reads=[lg, mx], writes=[msk])
        k.op("dve", lambda e: e.tensor_tensor(out=ex[:], in0=ex[:], in1=msk[:], op=ALU.mult),
             reads=[ex, msk], writes=[ex])
        k.op("dve", lambda e: e.reduce_sum(out=den[:], in_=ex[:], axis=AX.X), reads=[ex], writes=[den])
        k.op("dve", lambda e: e.reciprocal(out=rden[:], in_=den[:]), reads=[den], writes=[rden])
        k.op("dve", lambda e, i=i: e.tensor_scalar(out=gates[:, i, :], in0=ex[:], scalar1=rden[:, 0:1], scalar2=None,
                                                 op0=ALU.mult), reads=[ex, rden], writes=[gates])

    norm_transpose(g1, None, extra=router if moe else None)

    for pi, (e_idx, c0, g) in enumerate(passes):
        s = pi % 2
        if pi + 1 < len(passes):
            load_pass(pi + 1)
        for m in range(4):
            a = actT[m % 2]
            for c in range(g):
                pg = psg[c % 2]
                pu = psu[c % 2]
                for dc in range(8):
                    k.op("pe", lambda e, pg=pg, s=s, c=c, dc=dc, m=m: e.matmul(
                        pg[:], lhsT=wgs[s][:, dc, c * 128:(c + 1) * 128], rhs=xnT[:, dc, m * 512:(m + 1) * 512],
                        start=(dc == 0), stop=(dc == 7)), reads=[wgs[s]] + xt_t[4 * m:4 * m + 4], writes=[pg])
                for dc in range(8):
                    k.op("pe", lambda e, pu=pu, s=s, c=c, dc=dc, m=m: e.matmul(
                        pu[:], lhsT=wus[s][:, dc, c * 128:(c + 1) * 128], rhs=xnT[:, dc, m * 512:(m + 1) * 512],
                        start=(dc == 0), stop=(dc == 7)), reads=[wus[s]] + xt_t[4 * m:4 * m + 4], writes=[pu])
                sgb = sg[c % 2]
                k.op("act", lambda e, sgb=sgb, pg=pg: e.activation(out=sgb[:], in_=pg[:], func=AF.Silu),
                     reads=[pg], writes=[sgb])
                k.op("dve", lambda e, sgb=sgb, pu=pu, a=a, c=c: e.tensor_tensor(out=a[:, c, :], in0=pu[:], in1=sgb[:],
                                                                             op=ALU.mult),
                     reads=[pu, sgb], writes=[a])
            for tt in range(4):
                i = 4 * m + tt
                p = pso[tt % 2]
                for half in range(2):
                    for c in range(g):
                        k.op("pe", lambda e, p=p, half=half, c=c, a=a, tt=tt, s=s, g=g: e.matmul(
                            p[:, half * 512:(half + 1) * 512], lhsT=a[:, c, tt * 128:(tt + 1) * 128],
                            rhs=wds[s][:, c, half * 512:(half + 1) * 512], start=(c == 0), stop=(c == g - 1)),
                            reads=[a, wds[s]], writes=[p])
                if moe:
                    k.op("dve", lambda e, p=p, i=i, e_idx=e_idx: e.scalar_tensor_tensor(
                        out=hacc[:, i, :], in0=p[:], scalar=gates[:, i, e_idx:e_idx + 1], in1=hacc[:, i, :],
                        op0=ALU.mult, op1=ALU.add), reads=[p, gates, hacc_t[i]], writes=[hacc_t[i]])
                else:
                    k.op("dve", lambda e, p=p, i=i: e.tensor_tensor(out=hacc[:, i, :], in0=p[:], in1=hacc[:, i, :],
                                                                   op=ALU.add),
                         reads=[p, hacc_t[i]], writes=[hacc_t[i]])

    out_v = out.rearrange("(n p) d -> p n d", p=128) if moe else None
    if moe:
        norm_stats()
        for i in range(NT):
            ob = obuf[i % 2]
            k.op("dve", lambda e, i=i, ob=ob: e.scalar_tensor_tensor(
                out=ob[:], in0=hacc[:, i, :], scalar=rstd[:, i:i + 1], in1=goutb[:], op0=ALU.mult, op1=ALU.mult),
                reads=[hacc_t[i], rstd, goutb], writes=[ob])
            k.dma("sp", out_v[:, i, :], ob[:], reads=[ob], writes=[outT], sem=f"obuf{i % 2}")
    else:
        norm_stats()
        norm_transpose(g2, None)
        for m in range(4):
            k.dma("act", hn_loc[m].rearrange("(c p) t -> p c t", p=128), xnT[:, :, m * 512:(m + 1) * 512],
                  reads=xt_t[4 * m:4 * m + 4], writes=[hnloc_t[m]], sem=f"xst{m}")
            gather(m)


GROUPS = [[0, 1, 2, 3], [4, 5, 6, 7]]


def build_fused(upto=4, skip=()):
    nc = bass.Bass("TRN2", target_bir_lowering=False)

    def inp(name, shape, dt=F32):
        return nc.dram_tensor(name, list(shape), dt, kind="ExternalInput").ap()
    m_x = inp("m_x", [T, D]); m_win = inp("m_win", [D, 768]); m_gcol = inp("m_gcol", [128, 8])
    m_wsT = inp("m_wsT", [128, 128]); m_gng = inp("m_gng", [128]); m_bsc = inp("m_bsc", [128, 1])
    m_lbl = inp("m_lbl", [128, 2]); m_ong = inp("m_ong", [128])
    f_hin = inp("f_hin", [TOK, D]); f_wo = inp("f_wo", [D, D]); f_g1c = inp("f_g1c", [128, 8])
    f_wg = inp("f_wg", [1, D, 2816]); f_wu = inp("f_wu", [1, D, 2816]); f_wd = inp("f_wd", [1, 2816, D])
    f_g2c = inp("f_g2c", [128, 8])
    r_mixc = inp("r_mixc", [128, 8, 6]); r_wproj = inp("r_wproj", [D, 1024]); r_w2 = inp("r_w2", [64, 256])
    r_a2 = inp("r_a2", [64, 256]); r_g2 = inp("r_g2", [128, 256]); r_cvec = inp("r_cvec", [128, 2, 5])
    r_lng = inp("r_lng", [256]); r_lnb = inp("r_lnb", [256])
    e_wo = inp("e_wo", [D, D]); e_g1c = inp("e_g1c", [128, 8])
    e_wg = inp("e_wg", [8, D, 1408]); e_wu = inp("e_wu", [8, D, 1408]); e_wd = inp("e_wd", [8, 1408, D])
    e_rt = inp("e_rt", [D, 8]); e_rb = inp("e_rb", [8]); e_gout = inp("e_gout", [D])
    out = nc.dram_tensor("out", [TOK, D], F32, kind="ExternalOutput").ap()
    y_loc = nc.dram_tensor("y_loc", [4, 256, 2048], BF16).ap()
    y_all = nc.dram_tensor("y_all", [4, 4, 256, 2048], BF16).ap()
    hn_loc = nc.dram_tensor("hn_loc", [4, 1024, 512], BF16).ap()
    hn_all = nc.dram_tensor("hn_all", [4, 4, 1024, 512], BF16).ap()
    yg_loc = y_loc
    yg_all = y_all

    k = KB(nc)
    hacc = k.sb("hacc", [128, NT, D], F32, glob=True)
    hacc_t = [k.trk(f"hacc{i}") for i in range(NT)]
    outT = k.trk("outT")
    yloc_t = [k.trk(f"yloc{i}") for i in range(4)]
    yall_t = [k.trk(f"yall{i}") for i in range(4)]
    hnloc_t = [k.trk(f"hnloc{i}") for i in range(4)]
    hnall_t = [k.trk(f"hnall{i}") for i in range(4)]
    ygloc_t = [k.trk(f"ygloc{i}") for i in range(4)]
    ygall_t = [k.trk(f"ygall{i}") for i in range(4)]

    def mk_gather(loc, allb, loc_t, all_t, pat, nm):
        def gather(i):
            if nm in _NOCOLL:
                return
            k.coll(lambda e: e.collective_compute("AllGather", ALU.bypass, replica_groups=GROUPS,
                                                  ins=[loc[i].opt()], outs=[allb[i].rearrange(pat).opt()]),
                   f"{nm}{i}", reads=[loc_t[i]], writes=[all_t[i]])
        return gather

    if 1 not in skip:
     k.push("m_")
     phase_mix0(nc, k, m_x, m_win, m_gcol, m_wsT, m_gng, m_bsc, m_lbl, m_ong, y_loc, yloc_t,
               mk_gather(y_loc, y_all, yloc_t, yall_t, "r c t -> (r c) t", "ga"))
     k.pop()
    if upto >= 2 and 2 not in skip:
      k.push("f_")
      phase_ffn(nc, k, "ffn", hacc, hacc_t, outT, y_all, yall_t, f_wo, f_g1c, f_wg, f_wu, f_wd, hin=f_hin, g2c=f_g2c,
              hn_loc=hn_loc, hnloc_t=hnloc_t, gather=mk_gather(hn_loc, hn_all, hnloc_t, hnall_t, "r c t -> (r c) t", "gb"))
      k.pop()
    if upto >= 3:
      k.push("r_")
      phase_rwkv(nc, k, hn_all, hnall_t, r_mixc, r_wproj, r_w2, r_a2, r_g2, r_cvec, r_lng, r_lnb, yg_loc, ygloc_t,
               mk_gather(yg_loc, yg_all, ygloc_t, ygall_t, "r c t -> (r c) t", "gc"))
      k.pop()
    if upto >= 4:
      k.push("e_")
      phase_ffn(nc, k, "moe", hacc, hacc_t, outT, yg_all, ygall_t, e_wo, e_g1c, e_wg, e_wu, e_wd, rt=e_rt, rb=e_rb,
              gout=e_gout, out=out)
      k.wait_all("sp", [outT])
      k.pop()
    k.emit()
    k.close()
    return nc


_UPTO = 4
_NOCOLL = ()
_SKIP = ()
def _gc(g):
    return np.ascontiguousarray(np.asarray(g, np.float32).reshape(8, 128).T)


_NC = {}


def kernel(x, norm_mix_g, norm_ffn_g, norm_out_g,
           mix_w_in, mix_w_out, gmlp_norm_g, gmlp_w_s, gmlp_b_s, hgrn_lb_logits, hgrn_onorm_g,
           ffn_w_gate, ffn_w_up, ffn_w_down,
           rwkv_mix, rwkv_w_r, rwkv_w_k, rwkv_w_v, rwkv_w_o, rwkv_w0, rwkv_w1, rwkv_w2,
           rwkv_a0, rwkv_a1, rwkv_a2, rwkv_g1, rwkv_g2, rwkv_k_k, rwkv_k_a, rwkv_r_k,
           rwkv_ln_g, rwkv_ln_b,
           moe_router, moe_router_b, moe_w_gate, moe_w_up, moe_w_down, _trace=False):
    f32 = np.float32
    A = lambda a: np.ascontiguousarray(np.asarray(a, f32))
    x = A(x)
    xf = x.reshape(16384, 1024)
    cores = list(range(8))
    w = np.asarray(mix_w_in, f32)[0]
    colb = [0 * 512, 1 * 512, 4 * 512, 5 * 512, 2 * 512, 3 * 512]
    mixc = A(np.asarray(rwkv_mix, f32)[0].reshape(6, 8, 128).transpose(2, 1, 0))
    shared = dict(
        m_gcol=_gc(norm_mix_g[0]), m_ong=A(np.asarray(hgrn_onorm_g)[0]),
        f_wo=A(mix_w_out[0]), f_g1c=_gc(norm_ffn_g[0]), f_wg=A(ffn_w_gate), f_wu=A(ffn_w_up), f_wd=A(ffn_w_down),
        f_g2c=_gc(norm_mix_g[1]),
        r_mixc=mixc,
        e_wo=A(rwkv_w_o[0]), e_g1c=_gc(norm_ffn_g[1]), e_wg=A(moe_w_gate[0]), e_wu=A(moe_w_up[0]), e_wd=A(moe_w_down[0]),
        e_rt=A(moe_router[0]), e_rb=A(moe_router_b[0]), e_gout=A(norm_out_g))
    in_maps = []
    for c in cores:
        b, j = c // 4, c % 4
        cs = slice(256 * j, 256 * (j + 1))
        win = np.concatenate([w[:, cb + j * 128: cb + (j + 1) * 128] for cb in colb], axis=1)
        wproj = np.concatenate([np.asarray(rwkv_w_r)[0][:, cs], np.asarray(rwkv_w_k)[0][:, cs],
                                np.asarray(rwkv_w_v)[0][:, cs], np.asarray(rwkv_w1)[0], np.asarray(rwkv_a1)[0],
                                np.asarray(rwkv_g1)[0]], axis=1)
        pv = lambda v: np.asarray(v, f32).reshape(-1)[cs].reshape(2, 128).T
        cvec = np.stack([pv(rwkv_w0[0]), pv(rwkv_a0[0]), pv(rwkv_k_k[0]), pv(rwkv_k_a[0]), pv(rwkv_r_k[0])], axis=-1)
        m = dict(shared)
        m.update(m_x=x[b], m_win=A(win), m_wsT=A(np.asarray(gmlp_w_s)[0, j].T), m_gng=A(np.asarray(gmlp_norm_g)[0, j]),
                 m_bsc=A(np.asarray(gmlp_b_s)[0, j].reshape(128, 1)),
                 m_lbl=A(np.asarray(hgrn_lb_logits)[:, j * 128:(j + 1) * 128].T),
                 f_hin=A(xf[c * 2048:(c + 1) * 2048]),
                 r_wproj=A(wproj), r_w2=A(np.asarray(rwkv_w2)[0][:, cs]), r_a2=A(np.asarray(rwkv_a2)[0][:, cs]),
                 r_g2=A(np.asarray(rwkv_g2)[0][:, cs]), r_cvec=A(cvec),
                 r_lng=A(np.asarray(rwkv_ln_g)[0][cs]), r_lnb=A(np.asarray(rwkv_ln_b)[0][cs]))
        in_maps.append(m)
    if "nc" not in _NC:
        _NC["nc"] = build_fused(_UPTO, _SKIP)
    res = run_bass_kernel_spmd(_NC["nc"], in_maps, core_ids=cores, **({"trace": True} if _trace else {}))
    if _trace:
        print("exec_time_ns", res.exec_time_ns)
    out = np.concatenate([res.results[c]["out"] for c in cores], axis=0).reshape(2, 8192, 1024)
    return np.ascontiguousarray(out.astype(np.float32))
```

```python
import contextlib
import numpy as np
import concourse.bass as bass
import concourse.mybir as mybir
from concourse.bass_utils import run_bass_kernel_spmd

F32 = mybir.dt.float32
BF16 = mybir.dt.bfloat16
I32 = mybir.dt.int32
AF = mybir.ActivationFunctionType
ALU = mybir.AluOpType
AX = mybir.AxisListType


class Trk:
    __slots__ = ("name", "w", "r", "bank")

    def __init__(self, name, bank=None):
        self.name = name
        self.w = []
        self.r = []
        self.bank = bank


class Bank:
    def __init__(self):
        self.last = {}


class Buf:
    def __init__(self, t, name):
        self.t = t
        self.trk = Trk(name)

    def __getitem__(self, idx):
        return self.t[idx]


class KB:
    ENG = ("pe", "act", "dve", "pool", "sp")

    def __init__(self, nc, same_engine_sync=True):
        self.nc = nc
        self.es = contextlib.ExitStack()
        self.sems = {}
        self.cnt = {}
        self.ops = {e: [] for e in self.ENG}
        self.known = {e: {} for e in self.ENG}
        self.same_engine_sync = same_engine_sync
        for e in self.ENG:
            self.sems[e] = self.es.enter_context(nc.semaphore("sem_" + e))
            self.cnt[e] = 0
        self.free_sems = []
        self.phase_keys = []
        self.nsem = 0
        self.pes = None
        self.prefix = ""

    def push(self, prefix=""):
        assert self.pes is None
        self.pes = contextlib.ExitStack()
        self.phase_keys = []
        self.prefix = prefix

    def barrier(self):
        allk = [(k_, v) for k_, v in self.cnt.items() if v > 0]
        for e in self.ENG:
            waits = []
            kn = self.known[e]
            for k_, v in allk:
                if k_ != e and kn.get(k_, 0) < v:
                    kn[k_] = v
                    waits.append((k_, v))
            if waits:
                self.ops[e].append((waits, None, None, 0))

    def pop(self):
        self.barrier()
        self.pes.close()
        self.pes = None
        for key in self.phase_keys:
            self.free_sems.append((self.sems[key], self.cnt[key]))
        self.phase_keys = []

    def sb(self, name, shape, dt, glob=False):
        name = self.prefix + name
        es = self.es if (glob or self.pes is None) else self.pes
        t = es.enter_context(self.nc.sbuf_tensor(name, list(shape), dt))
        return Buf(t, name)

    def ps(self, name, shape, dt=F32):
        name = self.prefix + name
        es = self.es if self.pes is None else self.pes
        t = es.enter_context(self.nc.psum_tensor(name, list(shape), dt))
        b = Buf(t, name)
        b.trk.bank = Bank()
        return b

    def trk(self, name, bank=None):
        return Trk(self.prefix + name, bank.trk.bank if bank is not None else None)

    def dsem(self, name):
        key = "d_" + name
        if key not in self.sems:
            if self.free_sems:
                h, c = self.free_sems.pop()
                self.sems[key] = h
                self.cnt[key] = c
            else:
                self.nsem += 1
                self.sems[key] = self.es.enter_context(self.nc.semaphore("dsem%d" % self.nsem))
                self.cnt[key] = 0
            if self.pes is not None:
                self.phase_keys.append(key)
        return key

    @staticmethod
    def _t(b):
        return b.trk if isinstance(b, Buf) else b

    def _deps(self, eng, reads, writes):
        need = {}

        def add(tok):
            k, v = tok
            if k == eng and (eng == "pe" or not self.same_engine_sync):
                return
            if need.get(k, 0) < v:
                need[k] = v
        for b in reads:
            for tok in self._t(b).w:
                add(tok)
        for b in writes:
            t = self._t(b)
            for tok in t.w:
                add(tok)
            for tok in t.r:
                add(tok)
        for b in list(reads) + list(writes):
            bk = self._t(b).bank
            if bk is not None:
                for e2, v in bk.last.items():
                    if e2 != eng and need.get(e2, 0) < v:
                        need[e2] = v
        waits = []
        kn = self.known[eng]
        for k, v in need.items():
            if kn.get(k, 0) < v:
                kn[k] = v
                waits.append((k, v))
        return waits

    def _commit(self, tok, reads, writes, dma_group=False):
        for b in list(reads) + list(writes):
            bk = self._t(b).bank
            if bk is not None and not tok[0].startswith("d_"):
                bk.last[tok[0]] = tok[1]
        for b in reads:
            t = self._t(b)
            t.r = [x for x in t.r if x[0] != tok[0]] + [tok]
        for b in writes:
            t = self._t(b)
            if dma_group and t.w and all(x[0].startswith("d_") for x in t.w) and not t.r:
                t.w = [x for x in t.w if x[0] != tok[0]] + [tok]
            else:
                t.w = [tok]
                t.r = []

    def op(self, eng, fn, reads=(), writes=()):
        waits = self._deps(eng, reads, writes)
        self.cnt[eng] += 1
        tok = (eng, self.cnt[eng])
        self.ops[eng].append((waits, fn, eng, 1))
        self._commit(tok, reads, writes)

    def dma(self, eng, out, in_, reads=(), writes=(), sem=None, **kw):
        if sem is None:
            b = (list(writes) + list(reads))[0]
            sem = self._t(b).name
        elif not sem.startswith(self.prefix):
            sem = self.prefix + sem
        key = self.dsem(sem)
        waits = self._deps_dma(eng, reads, writes)
        self.cnt[key] += 16
        tok = (key, self.cnt[key])
        self.ops[eng].append((waits, lambda e: e.dma_start(out=out(e) if callable(out) else out,
                                                            in_=in_(e) if callable(in_) else in_, **kw), key, 16))
        self._commit(tok, reads, writes, dma_group=True)

    def coll(self, fn, name, reads=(), writes=()):
        key = self.dsem("cc_" + name)
        waits = self._deps_dma("pool", reads, writes)
        self.cnt[key] += 1
        tok = (key, self.cnt[key])
        self.ops["pool"].append((waits, fn, key, 1))
        self._commit(tok, reads, writes, dma_group=True)

    def _deps_dma(self, eng, reads, writes):
        need = {}

        def add(tok):
            k, v = tok
            if need.get(k, 0) < v:
                need[k] = v
        for b in reads:
            for tok in self._t(b).w:
                add(tok)
        for b in writes:
            t = self._t(b)
            if not (t.w and all(x[0].startswith("d_") for x in t.w) and not t.r):
                for tok in t.w:
                    add(tok)
            for tok in t.r:
                add(tok)
        waits = []
        kn = self.known[eng]
        for k, v in need.items():
            if kn.get(k, 0) < v:
                kn[k] = v
                waits.append((k, v))
        return waits

    def wait_all(self, eng, bufs):
        need = {}
        for b in bufs:
            t = self._t(b)
            for k, v in t.w + t.r:
                if need.get(k, 0) < v:
                    need[k] = v
        waits = [(k, v) for k, v in need.items()]
        self.ops[eng].append((waits, None, None, 0))

    def emit(self):
        nc = self.nc
        sems = self.sems
        ops = self.ops
        with nc.Block() as block:
            def run(e, lst):
                for waits, fn, key, inc in lst:
                    for k, v in waits:
                        e.wait_ge(sems[k], v)
                    if fn is not None:
                        fn(e).then_inc(sems[key], inc)

            @block.tensor
            def _(e):
                run(e, ops["pe"])

            @block.scalar
            def _(e):
                run(e, ops["act"])

            @block.vector
            def _(e):
                run(e, ops["dve"])

            @block.gpsimd
            def _(e):
                run(e, ops["pool"])

            @block.sync
            def _(e):
                self.pid_sp = e.partition_id() % 4
                run(e, ops["sp"])

    def close(self):
        self.es.close()


def _act(self, out, in_, func, R, W, scale=None, bias=None, accum=None):
    kw = {}
    if scale is not None:
        kw["scale"] = scale
    if bias is not None:
        kw["bias"] = bias
    if accum is not None:
        kw["accum_out"] = accum
    self.op("act", lambda e: e.activation(out=out, in_=in_, func=func, **kw), reads=R, writes=W)


def _tt(self, eng, out, in0, in1, op, R, W):
    self.op(eng, lambda e: e.tensor_tensor(out=out, in0=in0, in1=in1, op=op), reads=R, writes=W)


def _ts(self, eng, out, in0, s1, s2, op0, op1, R, W):
    if op1 is None:
        self.op(eng, lambda e: e.tensor_scalar(out=out, in0=in0, scalar1=s1, scalar2=None, op0=op0), reads=R, writes=W)
    else:
        self.op(eng, lambda e: e.tensor_scalar(out=out, in0=in0, scalar1=s1, scalar2=s2, op0=op0, op1=op1), reads=R, writes=W)


def _stt(self, out, in0, scalar, in1, op0, op1, R, W):
    self.op("dve", lambda e: e.scalar_tensor_tensor(out=out, in0=in0, scalar=scalar, in1=in1, op0=op0, op1=op1),
            reads=R, writes=W)


def _mm(self, out, lhsT, rhs, start, stop, R, W):
    self.op("pe", lambda e: e.matmul(out, lhsT=lhsT, rhs=rhs, start=start, stop=stop), reads=R, writes=W)


def _tr(self, out, in_, ident, R, W):
    self.op("pe", lambda e: e.transpose(out=out, in_=in_, identity=ident), reads=R, writes=W)


def _cp(self, eng, out, in_, R, W):
    if eng == "act":
        self.op("act", lambda e: e.copy(out=out, in_=in_), reads=R, writes=W)
    else:
        self.op(eng, lambda e: e.tensor_copy(out=out, in_=in_), reads=R, writes=W)


def _recip(self, out, in_, R, W):
    self.op("dve", lambda e: e.reciprocal(out=out, in_=in_), reads=R, writes=W)


KB.act = _act
KB.tt = _tt
KB.ts = _ts
KB.stt = _stt
KB.mm = _mm
KB.tr = _tr
KB.cp = _cp
KB.recip = _recip


D = 1024
NT = 16
TOK = 2048
RMS_EPS = 1e-6


def make_ident(k, name="ident32", dt=F32):
    ident = k.sb(name, [128, 128], dt)
    k.op("pool", lambda e: e.memset(ident[:], 0.0), writes=[ident])
    k.op("pool", lambda e: e.affine_select(out=ident[:], in_=ident[:], pattern=[[-1, 128]],
                                           compare_op=ALU.not_equal, fill=1.0, base=0,
                                           channel_multiplier=1), reads=[ident], writes=[ident])
    return ident


T = 8192
LN_EPS = 1e-5
GN_EPS = 64e-5
CW = 0.6065306597126334


def phase_mix0(nc, k, x, win, gcol, wsT, gng, bsc, lbl, ong, y_loc, yloc_t, gather):
    ntiles = 64
    ident = make_ident(k)
    identb = k.sb("identb", [128, 128], BF16)
    k.cp("dve", identb[:], ident[:], [ident], [identb])
    winb = k.sb("winb", [128, 8, 768], BF16)
    g1 = k.sb("g1", [128, 8], F32)
    wsf = k.sb("wsf", [128, 128], F32)
    wsb = k.sb("wsb", [128, 128], BF16)
    gnb = k.sb("gnb", [128, 128], F32)
    onb = k.sb("onb", [128, 128], F32)
    bs = k.sb("bs", [128, 1], F32)
    lb2 = k.sb("lb2", [128, 2], F32)
    lbd = k.sb("lbd", [128, 1], F32)
    lb = k.sb("lb", [128, 1], F32)
    oml = k.sb("oml", [128, 1], F32)
    maskT = k.sb("maskT", [128, 128], F32)
    rmask = k.sb("rmask", [128, 128], F32)
    epsr = k.sb("epsr", [128, 1], F32)
    epsl = k.sb("epsl", [128, 1], F32)
    S32 = k.sb("S32", [128, 128], F32)
    Sbf = [k.sb(f"Sbf{i}", [128, 128], BF16) for i in range(4)]

    win_v = win.rearrange("(c p) f -> p c f", p=128)
    for c in range(0, 8, 2):
        k.dma("pool", winb[:, c:c + 2, :], win_v[:, c:c + 2, :], writes=[winb])
    k.dma("sp", g1[:], gcol, writes=[g1])
    k.dma("sp", wsf[:], wsT, writes=[wsf])
    k.dma("sp", gnb[:], gng.partition_broadcast(128), writes=[gnb])
    k.dma("sp", onb[:], ong.partition_broadcast(128), writes=[onb])
    k.dma("sp", bs[:], bsc, writes=[bs])
    k.dma("sp", lb2[:], lbl, writes=[lb2])

    k.op("dve", lambda e: e.memset(epsr[:], RMS_EPS), writes=[epsr])
    k.op("dve", lambda e: e.memset(epsl[:], LN_EPS), writes=[epsl])
    k.op("dve", lambda e: e.memset(S32[:], 0.0), writes=[S32])
    k.op("dve", lambda e: e.memset(Sbf[0][:], 0.0), writes=[Sbf[0]])
    k.op("pool", lambda e: e.affine_select(out=wsf[:], in_=wsf[:], pattern=[[1, 128]], compare_op=ALU.is_ge,
                                           fill=0.0, base=0, channel_multiplier=-1), reads=[wsf], writes=[wsf])
    k.cp("dve", wsb[:], wsf[:], [wsf], [wsb])
    k.op("pool", lambda e: e.memset(maskT[:], 1.0), writes=[maskT])
    k.op("pool", lambda e: e.affine_select(out=maskT[:], in_=maskT[:], pattern=[[1, 128]], compare_op=ALU.is_ge,
                                           fill=0.0, base=0, channel_multiplier=-1), reads=[maskT], writes=[maskT])
    for n in range(1, 4):
        k.op("pool", lambda e, n=n: e.affine_select(out=maskT[:, 32 * n:32 * n + 32], in_=maskT[:, 32 * n:32 * n + 32],
                                                    pattern=[[0, 32]], compare_op=ALU.is_ge, fill=0.0,
                                                    base=-32 * n, channel_multiplier=1),
             reads=[maskT], writes=[maskT])
    k.op("pool", lambda e: e.memset(rmask[:], 1.0), writes=[rmask])
    for n in range(4):
        k.op("pool", lambda e, n=n: e.memset(rmask[:, 32 * n:32 * n + 1], 0.0), reads=[rmask], writes=[rmask])
    rm3 = k.sb("rm3", [128, 1], F32)
    k.op("pool", lambda e: e.memset(rm3[:], 1.0), writes=[rm3])
    k.op("pool", lambda e: e.affine_select(out=rm3[:], in_=rm3[:], pattern=[[0, 1]], compare_op=ALU.is_ge, fill=0.0,
                                           base=-96, channel_multiplier=1), reads=[rm3], writes=[rm3])
    qdec3 = k.sb("qdec3", [128, 64], BF16)
    k.op("pool", lambda e: e.memset(qdec3[:], 0.0), writes=[qdec3])
    kend3 = k.sb("kend3", [128, 128], BF16)
    k.tt("dve", lbd[:], lb2[:, 0:1], lb2[:, 1:2], ALU.subtract, [lb2], [lbd])
    k.act(lb[:], lbd[:], AF.Sigmoid, [lbd], [lb])
    k.ts("dve", oml[:], lb[:], -1.0, 1.0, ALU.mult, ALU.add, [lb], [oml])

    xt = [k.sb(f"xt{i}", [128, D], F32) for i in range(3)]
    junk = k.sb("junk", [128, D], F32)
    xn32 = [k.sb(f"xn32_{i}", [128, D], F32) for i in range(2)]
    xnT = [k.sb(f"xnT{i}", [128, 8, 128], BF16) for i in range(2)]
    ss = k.sb("ss", [128, 1], F32)
    sd = k.sb("sd", [128, 1], F32)
    rstd = k.sb("rstd", [128, 1], F32)
    uv2 = [k.sb(f"uv{_i}", [128, 256], F32) for _i in range(2)]
    bst = k.sb("bst", [128, 6], F32)
    mv = k.sb("mv", [128, 2], F32)
    sdv = k.sb("sdv", [128, 1], F32)
    rv = k.sb("rv", [128, 1], F32)
    vn = k.sb("vn", [128, 128], F32)
    vnb = k.sb("vnb", [128, 128], BF16)
    ycat = k.sb("ycat", [128, 256], F32)
    sigT2 = [k.sb(f"sigT{_i}", [128, 128], F32) for _i in range(2)]
    fT = k.sb("fT", [128, 128], F32)
    logfT = k.sb("logfT", [128, 128], F32)
    bT = k.sb("bT", [128, 128], F32)
    E = k.sb("E", [128, 128], F32)
    Einv = k.sb("Einv", [128, 128], F32)
    sq2 = [k.sb(f"sq{_i}", [128, 128], F32) for _i in range(2)]
    qdec = k.sb("qdec", [128, 128], BF16)
    omf = k.sb("omf", [128, 128], F32)
    kdec32 = k.sb("kdec32", [128, 128], F32)
    kdecb = k.sb("kdecb", [128, 128], BF16)
    dcol = k.sb("dcol", [128, 4], F32)
    kendT = k.sb("kendT", [128, 128], BF16)
    kend = k.sb("kend", [128, 128], BF16)
    vb2 = [k.sb(f"vb{_i}", [128, 128], BF16) for _i in range(2)]
    scT = k.sb("scT", [128, 128], BF16)
    sso = k.sb("sso", [128, 1], F32)
    sdo = k.sb("sdo", [128, 1], F32)
    ro = k.sb("ro", [128, 1], F32)
    on = k.sb("on", [128, 128], F32)
    sgt2 = [k.sb(f"sgt{_i}", [128, 128], F32) for _i in range(2)]
    junk2 = k.sb("junk2", [128, 128], F32)
    yTb = [k.sb(f"yTb{i}", [128, 2, 512], BF16) for i in range(2)]

    pt = k.ps("pt", [128, D], F32)
    ptok = k.ps("ptok", [128, 512], F32)
    pqf = k.ps("pqf", [128, 512], F32)
    pE = k.ps("pE", [128, 512], F32)
    pEb = k.ps("pEb", [128, 1024], BF16)
    po = k.ps("po", [128, 512], F32)
    pinc = [k.ps(f"pinc{i}", [128, 512], F32) for i in range(1)]
    pm_t, psc_t, pTy_t = k.trk("pm", pE), k.trk("psc", pE), k.trk("pTy", pE)

    x_v = x.rearrange("(n p) d -> n p d", p=128)
    scur = 0
    def bind(i):
        return (uv2[i % 2], sigT2[i % 2], sq2[i % 2], sgt2[i % 2], vb2[i % 2])

    def stageA(i):
        uv, sigT, sq, sgt, vb = bind(i)
        xb = xt[i % 3]
        k.dma("sp", xb[:], x_v[i], writes=[xb])
        k.act(junk[:], xb[:], AF.Square, [xb], [junk, ss], accum=ss[:])
        k.act(sd[:], ss[:], AF.Sqrt, [ss, epsr], [sd], scale=1.0 / D, bias=epsr[:])
        k.recip(rstd[:], sd[:], [sd], [rstd])
        yield
        xn = xn32[i % 2]
        k.act(xn[:], xb[:], AF.Copy, [xb, rstd], [xn], scale=rstd[:])
        for dc in range(8):
            k.tr(pt[:, dc * 128:(dc + 1) * 128], xn[:, dc * 128:(dc + 1) * 128], ident[:], [xn, ident], [pt])
        xT = xnT[i % 2]
        for dc in range(8):
            k.ts("dve", xT[:, dc, :], pt[:, dc * 128:(dc + 1) * 128], g1[:, dc:dc + 1], None, ALU.mult, None,
                 [pt, g1], [xT])
        yield
        for dc in range(8):
            k.mm(ptok[:], xT[:, dc, :], winb[:, dc, 0:512], dc == 0, dc == 7, [xT, winb], [ptok])
        for dc in range(8):
            k.mm(pqf[:, 0:128], winb[:, dc, 512:640], xT[:, dc, :], dc == 0, dc == 7, [xT, winb], [pqf])
        for dc in range(8):
            k.mm(pqf[:, 128:256], winb[:, dc, 640:768], xT[:, dc, :], dc == 0, dc == 7, [xT, winb], [pqf])
        yield
        k.act(uv[:], ptok[:, 0:256], AF.Gelu_apprx_tanh, [ptok], [uv])
        k.act(sigT[:], pqf[:, 128:256], AF.Sigmoid, [pqf], [sigT])
        k.act(sq[:], pqf[:, 0:128], AF.Silu, [pqf], [sq])
        k.act(sgt[:], ptok[:, 384:512], AF.Silu, [ptok], [sgt])
        k.cp("act", vb[:], ptok[:, 256:384], [ptok], [vb])
        yield

    def stageB(i):
        nonlocal scur
        uv, sigT, sq, sgt, vb = bind(i)
        k.op("dve", lambda e: e.bn_stats(out=bst[:], in_=uv[:, 128:256]), reads=[uv], writes=[bst])
        k.op("dve", lambda e: e.bn_aggr(out=mv[:], in_=bst[:]), reads=[bst], writes=[mv])
        k.act(sdv[:], mv[:, 1:2], AF.Sqrt, [mv, epsl], [sdv], scale=1.0, bias=epsl[:])
        k.recip(rv[:], sdv[:], [sdv], [rv])
        k.ts("dve", vn[:], uv[:, 128:256], mv[:, 0:1], rv[:, 0:1], ALU.subtract, ALU.mult, [uv, mv, rv], [vn])
        k.tt("pool", vnb[:], vn[:], gnb[:], ALU.mult, [vn, gnb], [vnb])
        k.mm(pE[:, 0:128], wsb[:], vnb[:], True, True, [wsb, vnb], [pm_t])
        k.stt(ycat[:, 0:128], pE[:, 0:128], bs[:, 0:1], uv[:, 0:128], ALU.add, ALU.mult, [pm_t, bs, uv], [ycat])
        yield
        k.ts("dve", fT[:], sigT[:], oml[:, 0:1], lb[:, 0:1], ALU.mult, ALU.add, [sigT, oml, lb], [fT])
        k.act(logfT[:], fT[:], AF.Ln, [fT], [logfT])
        k.op("dve", lambda e: e.tensor_tensor_scan(out=bT[:], data0=rmask[:], data1=logfT[:], initial=0.0,
                                                   op0=ALU.mult, op1=ALU.add), reads=[rmask, logfT], writes=[bT])
        yield
        k.act(E[:], bT[:], AF.Exp, [bT], [E])
        k.act(Einv[:], bT[:], AF.Exp, [bT], [Einv], scale=-1.0)
        k.op("act", lambda e: e.activation(out=dcol[:], in_=bT[:, 31:128:32], func=AF.Exp), reads=[bT], writes=[dcol])
        k.tt("dve", qdec[:], sq[:], E[:], ALU.mult, [sq, E], [qdec])
        k.cp("pool", qdec3[:, 32:64], qdec[:, 96:128], [qdec], [qdec3])
        k.ts("pool", omf[:], fT[:], -1.0, 1.0, ALU.mult, ALU.add, [fT], [omf])
        k.tt("dve", kdec32[:], omf[:], Einv[:], ALU.mult, [omf, Einv], [kdec32])
        k.cp("pool", kdecb[:], kdec32[:], [kdec32], [kdecb])
        for n in range(4):
            k.ts("dve", kendT[:, 32 * n:32 * n + 32], kdec32[:, 32 * n:32 * n + 32], dcol[:, n:n + 1], None, ALU.mult, None,
                 [kdec32, dcol], [kendT])
        yield
        k.tr(pEb[:, 0:128], kendT[:], identb[:], [kendT, identb], [pEb])
        k.cp("act", kend[:], pEb[:, 0:128], [pEb], [kend])
        k.ts("dve", kend3[:], pEb[:, 0:128], rm3[:, 0:1], None, ALU.mult, None, [pEb, rm3], [kend3])
        k.mm(pE[:, 128:256], kdecb[:], qdec[:], True, True, [kdecb, qdec], [psc_t])
        k.tt("dve", scT[:], pE[:, 128:256], maskT[:], ALU.mult, [psc_t, maskT], [scT])
        yield
        k.mm(po[:, 0:128], scT[:], vb[:], True, False, [scT, vb], [po])
        for n in range(4):
            sb_cur = Sbf[scur]
            pi_ = pinc[0]
            if n < 3:
                k.mm(po[32 * n:32 * n + 32, 0:128], qdec[:, 32 * n:32 * n + 32], sb_cur[:], False, n < 2,
                     [qdec, sb_cur], [po])
                k.mm(pi_[:, 0:128], kend[32 * n:32 * n + 32, :], vb[32 * n:32 * n + 32, :], True, True, [kend, vb], [pi_])
            else:
                k.mm(po[64:128, 0:128], qdec3[:, 0:64], sb_cur[:], False, True, [qdec3, sb_cur], [po])
                k.mm(pi_[:, 0:128], kend3[64:128, :], vb[64:128, :], True, True, [kend3, vb], [pi_])
            k.stt(S32[:], S32[:], dcol[:, n:n + 1], pi_[:, 0:128], ALU.mult, ALU.add, [S32, dcol, pi_], [S32])
            yield
            scur = (scur + 1) % 4
            k.cp("act", Sbf[scur][:], S32[:], [S32], [Sbf[scur]])
        yield
        k.act(junk2[:], po[:, 0:128], AF.Square, [po], [junk2, sso], accum=sso[:])
        k.act(sdo[:], sso[:], AF.Sqrt, [sso, epsr], [sdo], scale=1.0 / 128, bias=epsr[:])
        k.recip(ro[:], sdo[:], [sdo], [ro])
        k.stt(on[:], po[:, 0:128], ro[:, 0:1], onb[:], ALU.mult, ALU.mult, [po, ro, onb], [on])
        k.tt("dve", ycat[:, 128:256], on[:], sgt[:], ALU.mult, [on, sgt], [ycat])
        yield
        for c in range(2):
            k.tr(pE[:, 256 + c * 128:256 + (c + 1) * 128], ycat[:, c * 128:(c + 1) * 128], ident[:], [ycat, ident], [pTy_t])
        yb = yTb[(i // 4) % 2]
        j = i % 4
        k.op("act", lambda e, yb=yb, j=j: e.copy(out=yb[:, :, j * 128:(j + 1) * 128],
                                                in_=pE[:, 256:512].rearrange("p (c t) -> p c t", c=2)),
             reads=[pTy_t], writes=[yb])
        if j == 3:
            qr = i // 16
            off = ((i % 16) // 4) * 512
            k.dma("sp", y_loc[qr].rearrange("(c p) t -> p c t", p=128)[:, :, off:off + 512], yb[:, :, :], reads=[yb],
                  writes=[yloc_t[qr]], sem=f"yTb{(i // 4) % 2}")
            if i % 16 == 15:
                gather(qr)
    def drain(g, n=None):
        c = 0
        while n is None or c < n:
            try:
                next(g)
            except StopIteration:
                return False
            c += 1
        return True

    drain(stageA(0))
    for i in range(ntiles):
        gB = stageB(i)
        gA = stageA(i + 1) if i + 1 < ntiles else None
        aliveA, aliveB = gA is not None, True
        while aliveA or aliveB:
            if aliveB:
                aliveB = drain(gB, 2)
            if aliveA:
                aliveA = drain(gA, 1)


def phase_rwkv(nc, k, hn_all, hnall_t, mixc, wproj, w2, a2, g2, cvec, lng, lnb, y_loc, yloc_t, gather):
    ntiles = 64
    ident = make_ident(k)
    identb = k.sb("identb", [128, 128], BF16)
    k.cp("dve", identb[:], ident[:], [ident], [identb])
    ident4 = k.sb("ident4", [128, 4, 128], F32)
    for h in range(4):
        k.cp("pool", ident4[:, h, :], ident[:], [ident], [ident4])

    mx6 = k.sb("mx6", [128, 8, 6], F32)
    omx6 = k.sb("omx6", [128, 8, 6], F32)
    k.dma("sp", mx6[:], mixc, writes=[mx6])
    k.ts("dve", omx6[:], mx6[:], -1.0, 1.0, ALU.mult, ALU.add, [mx6], [omx6])
    Wa = k.sb("Wa", [128, 8, 1024], BF16)
    Wb = k.sb("Wb", [128, 8, 1024], BF16)
    wst = [k.sb(f"wst{i}", [128, 1024], F32) for i in range(2)]
    blocks = [(0, 256, 0), (256, 512, 2), (512, 768, 3), (768, 832, 1), (832, 896, 4), (896, 1024, 5)]
    wp_v = wproj.rearrange("(c p) f -> p c f", p=128)
    for dc in range(8):
        st = wst[dc % 2]
        k.dma("sp", st[:], wp_v[:, dc, :], writes=[st])
        for (c0, c1, mi) in blocks:
            k.ts("dve", Wb[:, dc, c0:c1], st[:, c0:c1], mx6[:, dc, mi:mi + 1], None, ALU.mult, None, [st, mx6], [Wb])
            k.ts("pool", Wa[:, dc, c0:c1], st[:, c0:c1], omx6[:, dc, mi:mi + 1], None, ALU.mult, None, [st, omx6], [Wa])
    w2b = k.sb("w2b", [128, 256], BF16)
    a2b = k.sb("a2b", [128, 256], BF16)
    g2b = k.sb("g2b", [128, 256], BF16)
    k.dma("pool", w2b[0:64, :], w2, writes=[w2b])
    k.dma("pool", a2b[64:128, :], a2, writes=[a2b])
    k.dma("pool", g2b[:], g2, writes=[g2b])
    cv = k.sb("cv", [128, 2, 5], F32)
    k.dma("sp", cv[:], cvec, writes=[cv])
    omka = k.sb("omka", [128, 2], F32)
    k.ts("dve", omka[:], cv[:, :, 3], -1.0, 1.0, ALU.mult, ALU.add, [cv], [omka])
    lngb = k.sb("lngb", [128, 256], F32)
    lnbb = k.sb("lnbb", [128, 256], F32)
    k.dma("sp", lngb[:], lng.partition_broadcast(128), writes=[lngb])
    k.dma("sp", lnbb[:], lnb.partition_broadcast(128), writes=[lnbb])

    def mk_mask(name, strict):
        m = k.sb(name, [128, 128], F32)
        k.op("pool", lambda e: e.memset(m[:], 1.0), writes=[m])
        k.op("pool", lambda e: e.affine_select(out=m[:], in_=m[:], pattern=[[1, 128]],
                                               compare_op=ALU.is_gt if strict else ALU.is_ge, fill=0.0, base=0,
                                               channel_multiplier=-1), reads=[m], writes=[m])
        k.op("pool", lambda e: e.affine_select(out=m[:, 64:128], in_=m[:, 64:128], pattern=[[0, 64]],
                                               compare_op=ALU.is_ge, fill=0.0, base=-64, channel_multiplier=1),
             reads=[m], writes=[m])
        return m
    maskS = mk_mask("maskS", True)
    maskI = mk_mask("maskI", False)
    maskL = k.sb("maskL", [128, 128], F32)
    k.op("pool", lambda e: e.memset(maskL[:], 1.0), writes=[maskL])
    k.op("pool", lambda e: e.affine_select(out=maskL[:], in_=maskL[:], pattern=[[-1, 128]], compare_op=ALU.is_gt,
                                           fill=0.0, base=0, channel_multiplier=1), reads=[maskL], writes=[maskL])
    k.op("pool", lambda e: e.affine_select(out=maskL[:, 0:64], in_=maskL[:, 0:64], pattern=[[0, 64]],
                                           compare_op=ALU.is_ge, fill=0.0, base=63, channel_multiplier=-1),
         reads=[maskL], writes=[maskL])
    maskL4 = k.sb("maskL4", [128, 4, 128], F32)
    for h in range(4):
        k.cp("pool", maskL4[:, h, :], maskL[:], [maskL], [maskL4])
    mask4 = k.sb("mask4", [128, 512], F32)
    k.cp("pool", mask4[:, 0:128], maskS[:], [maskS], [mask4])
    k.cp("pool", mask4[:, 128:256], maskI[:], [maskI], [mask4])
    k.cp("pool", mask4[:, 256:384], maskS[:], [maskS], [mask4])
    k.ts("pool", mask4[:, 384:512], maskI[:], -1.0, None, ALU.mult, None, [maskI], [mask4])
    rmask = k.sb("rmask", [128, 2, 128], F32)
    k.op("pool", lambda e: e.memset(rmask[:], 1.0), writes=[rmask])
    for p in range(2):
        for c in range(2):
            k.op("pool", lambda e, p=p, c=c: e.memset(rmask[:, p, 64 * c:64 * c + 1], 0.0), reads=[rmask], writes=[rmask])
    bones = k.sb("bones", [128, 128], BF16)
    k.op("pool", lambda e: e.memset(bones[:], 0.0), writes=[bones])
    k.op("pool", lambda e: e.memset(bones[0:64, 0:64], 1.0), reads=[bones], writes=[bones])
    k.op("pool", lambda e: e.memset(bones[64:128, 64:128], 1.0), reads=[bones], writes=[bones])
    epsg = k.sb("epsg", [128, 1], F32)
    k.op("dve", lambda e: e.memset(epsg[:], GN_EPS), writes=[epsg])
    epsk = k.sb("epsk", [128, 1], F32)
    k.op("dve", lambda e: e.memset(epsk[:], 1e-24), writes=[epsk])

    M32 = k.sb("M32", [128, 2, 64], F32)
    Mb = [k.sb(f"Mb{i}", [128, 4, 64], BF16) for i in range(2)]
    k.op("dve", lambda e: e.memset(M32[:], 0.0), writes=[M32])
    k.op("dve", lambda e: e.memset(Mb[0][:], 0.0), writes=[Mb[0]])
    hmask = k.sb("hmask", [128, 2], F32)
    k.op("pool", lambda e: e.memset(hmask[:], 0.0), writes=[hmask])
    k.op("pool", lambda e: e.memset(hmask[0:64, 0:1], 1.0), reads=[hmask], writes=[hmask])
    k.op("pool", lambda e: e.memset(hmask[64:128, 1:2], 1.0), reads=[hmask], writes=[hmask])
    mcur = 0

    hx = [k.sb(f"hx{i}", [128, 8, 128], BF16) for i in range(2)]
    hs = k.sb("hs", [128, 8, 128], BF16)
    k.op("pool", lambda e: e.memset(hs[:, :, 0:1], 0.0), writes=[hs])
    l1 = k.sb("l1", [128, 128], BF16)
    sg1 = k.sb("sg1", [128, 128], BF16)
    sigw = k.sb("sigw", [128, 2, 128], F32)
    a_ = k.sb("a_", [128, 2, 128], F32)
    kku = k.sb("kku", [128, 2, 128], F32)
    sqk = k.sb("sqk", [128, 2, 128], BF16)
    sdk = k.sb("sdk", [128, 2, 128], F32)
    rn = k.sb("rn", [128, 2, 128], F32)
    kk = k.sb("kk", [128, 2, 128], F32)
    b_ = k.sb("b_", [128, 2, 128], F32)
    kmf = k.sb("kmf", [128, 2, 128], F32)
    kmod = k.sb("kmod", [128, 2, 128], F32)
    r32 = k.sb("r32", [128, 2, 128], F32)
    gc = k.sb("gc", [128, 2, 128], F32)
    gcx = k.sb("gcx", [128, 2, 128], F32)
    E = k.sb("E", [128, 2, 128], F32)
    Einv = k.sb("Einv", [128, 2, 128], F32)
    Eex = k.sb("Eex", [128, 2, 128], F32)
    KR2 = [k.sb(f"KR{_i}", [128, 2, 2, 128], BF16) for _i in range(2)]
    Ki32 = k.sb("Ki32", [128, 2, 128], F32)
    Bi32 = k.sb("Bi32", [128, 2, 128], F32)
    Ki = k.sb("Ki", [128, 2, 128], BF16)
    Bi = k.sb("Bi", [128, 2, 128], BF16)
    KeT = k.sb("KeT", [128, 2, 128], BF16)
    nBeT = k.sb("nBeT", [128, 2, 128], BF16)
    dcol2 = [k.sb(f"dcol{_i}", [128, 2, 2], F32) for _i in range(2)]
    ndcol = k.sb("ndcol", [128, 2, 2], F32)
    rk = k.sb("rk", [128, 2, 128], BF16)
    rk32 = k.sb("rk32", [128, 2, 128], F32)
    AT2 = [k.sb(f"AT{_i}", [128, 4, 512], BF16) for _i in range(2)]
    P = [k.sb(f"P{i}", [128, 4, 128], BF16) for i in range(2)]
    PT = [k.sb(f"PT{i}", [128, 4, 128], BF16) for i in range(2)]
    R = [k.sb(f"R{i}", [128, 4, 128], BF16) for i in range(2)]
    Vb2 = [k.sb(f"Vb{_i}", [128, 256], BF16) for _i in range(2)]
    V322 = [k.sb(f"V32{_i}", [128, 256], F32) for _i in range(2)]
    Wt = k.sb("Wt", [128, 256], BF16)
    Ut = k.sb("Ut", [128, 256], BF16)
    k.op("dve", lambda e: e.memset(Wt[:], 0.0), writes=[Wt])
    k.op("dve", lambda e: e.memset(Ut[:], 0.0), writes=[Ut])
    Ke2 = [k.sb(f"Ke{_i}", [128, 256], BF16) for _i in range(2)]
    nBe2 = [k.sb(f"nBe{_i}", [128, 256], BF16) for _i in range(2)]
    bon2 = [k.sb(f"bon{_i}", [128, 4], F32) for _i in range(2)]
    bst = k.sb("bst", [128, 4, 6], F32)
    mv = k.sb("mv", [128, 4, 2], F32)
    sdg = k.sb("sdg", [128, 4], F32)
    rg = k.sb("rg", [128, 4], F32)
    yn = k.sb("yn", [128, 256], F32)
    gate2 = [k.sb(f"gate{_i}", [128, 256], F32) for _i in range(2)]
    Rfin2 = [k.sb(f"Rfin{_i}", [128, 4, 128], BF16) for _i in range(2)]
    yg = k.sb("yg", [128, 256], F32)
    yTb = [k.sb(f"yTb{i}", [128, 2, 512], BF16) for i in range(2)]

    b0 = k.ps("b0", [128, 512], F32)
    b1 = k.ps("b1", [128, 512], F32)
    b2 = k.ps("b2", [128, 512], F32)
    b3 = k.ps("b3", [128, 512], F32)
    b4 = k.ps("b4", [128, 512], F32)
    b5 = k.ps("b5", [128, 512], F32)
    b5b = None
    b6 = k.ps("b6", [128, 512], F32)
    b7 = k.ps("b7", [128, 512], F32)
    b45b = k.ps("b45b", [128, 1024], BF16) if False else None


    def bind(i):
        return (KR2[i % 2], AT2[i % 2], Vb2[i % 2], V322[i % 2], Ke2[i % 2], nBe2[i % 2], dcol2[i % 2], bon2[i % 2],
                gate2[i % 2], Rfin2[i % 2])

    def stageA(i):
        KR, AT, Vb, V32, Ke, nBe, dcol, bon, gate, Rfin = bind(i)
        t0 = i * 128
        hxc = hx[i % 2]
        hxp = hx[(i + 1) % 2]
        rr, mm_, tt0 = i // 16, (i % 16) // 4, (i % 4) * 128
        k.dma("sp", hxc[:], hn_all[mm_][rr].rearrange("(c p) t -> p c t", p=128)[:, :, tt0:tt0 + 128],
              reads=[hnall_t[mm_]], writes=[hxc], sem=hxc.trk.name)
        k.cp("pool", hs[:, :, 1:128], hxc[:, :, 0:127], [hxc], [hs])
        if i > 0:
            k.cp("pool", hs[:, :, 0:1], hxp[:, :, 127:128], [hxp], [hs])
        def proj(out, c0, c1, fm):
            for dc in range(8):
                for (Wx, hh) in ((Wa, hxc), (Wb, hs)):
                    first = dc == 0 and Wx is Wa
                    last = dc == 7 and Wx is Wb
                    if fm:
                        k.mm(out, Wx[:, dc, c0:c1], hh[:, dc, :], first, last, [Wx, hh], [out_b])
                    else:
                        k.mm(out, hh[:, dc, :], Wx[:, dc, c0:c1], first, last, [Wx, hh], [out_b])
        out_b = b0
        for j in range(4):
            proj(b0[:, j * 128:(j + 1) * 128], j * 128, (j + 1) * 128, True)
        yield
        out_b = b1
        proj(b1[:, 0:128], 768, 896, True)
        proj(b1[:, 128:256], 896, 1024, True)
        yield
        out_b = b2
        proj(b2[:, 0:256], 512, 768, False)
        yield
        k.act(l1[0:64, :], b1[0:64, 0:128], AF.Tanh, [b1], [l1])
        k.cp("act", l1[64:128, :], b1[64:128, 0:128], [b1], [l1])
        k.act(sg1[:], b1[:, 128:256], AF.Sigmoid, [b1], [sg1])
        k.cp("act", Vb[:], b2[:, 0:256], [b2], [Vb])
        k.cp("dve", V32[:], b2[:, 0:256], [b2], [V32])
        yield
        for p in range(2):
            k.mm(b3[:, p * 128:(p + 1) * 128], w2b[0:64, p * 128:(p + 1) * 128], l1[0:64, :], True, True, [w2b, l1], [b3])
        for p in range(2):
            k.mm(b4[:, p * 128:(p + 1) * 128], a2b[64:128, p * 128:(p + 1) * 128], l1[64:128, :], True, True,
                 [a2b, l1], [b4])
        k.mm(b2[:, 256:512], sg1[:], g2b[:], True, True, [sg1, g2b], [b2])
        k.cp("act", gate[:], b2[:, 256:512], [b2], [gate])
        for p in range(2):
            k.act(sigw[:, p, :], b3[:, p * 128:(p + 1) * 128], AF.Sigmoid, [b3, cv], [sigw], bias=cv[:, p, 0:1])
            k.act(a_[:, p, :], b4[:, p * 128:(p + 1) * 128], AF.Sigmoid, [b4, cv], [a_], bias=cv[:, p, 1:2])
        yield
        k.cp("act", r32[:], b0[:, 0:256].rearrange("p (a t) -> p a t", a=2), [b0], [r32])
        for p in range(2):
            k.ts("dve", kku[:, p, :], b0[:, 256 + p * 128:256 + (p + 1) * 128], cv[:, p, 2:3], None, ALU.mult, None,
                 [b0, cv], [kku])
        k.tt("pool", sqk[:], kku[:], kku[:], ALU.mult, [kku], [sqk])
        for p in range(2):
            k.mm(b3[:, p * 128:(p + 1) * 128], bones[:], sqk[:, p, :], True, True, [bones, sqk], [b3])
        k.act(sdk[:], b3[:, 0:256].rearrange("p (a t) -> p a t", a=2), AF.Sqrt, [b3, epsk], [sdk], bias=epsk[:], scale=1.0)
        k.recip(rn[:], sdk[:], [sdk], [rn])
        k.tt("dve", kk[:], kku[:], rn[:], ALU.mult, [kku, rn], [kk])
        k.tt("pool", b_[:], kk[:], a_[:], ALU.mult, [kk, a_], [b_])
        for p in range(2):
            k.ts("pool", kmf[:, p, :], a_[:, p, :], cv[:, p, 3:4], omka[:, p:p + 1], ALU.mult, ALU.add, [a_, cv, omka], [kmf])
        k.tt("dve", kmod[:], kmf[:], b0[:, 256:512].rearrange("p (a t) -> p a t", a=2), ALU.mult, [kmf, b0], [kmod])
        yield
        for p in range(2):
            k.op("dve", lambda e, p=p: e.tensor_tensor_scan(out=gc[:, p, :], data0=rmask[:, p, :], data1=sigw[:, p, :],
                                                          initial=0.0, op0=ALU.mult, op1=ALU.add),
                 reads=[rmask, sigw], writes=[gc])
        k.tt("pool", gcx[:], gc[:], sigw[:], ALU.subtract, [gc, sigw], [gcx])
        k.act(E[:], gc[:], AF.Exp, [gc], [E], scale=-CW)
        k.act(Einv[:], gc[:], AF.Exp, [gc], [Einv], scale=CW)
        k.act(Eex[:], gcx[:], AF.Exp, [gcx], [Eex], scale=-CW)
        k.cp("pool", dcol[:], E[:, :, 63:128:64], [E], [dcol])
        k.ts("pool", ndcol[:], dcol[:], -1.0, None, ALU.mult, None, [dcol], [ndcol])
        k.tt("dve", KR[:, :, 0, :], kk[:], Eex[:], ALU.mult, [kk, Eex], [KR])
        k.tt("dve", KR[:, :, 1, :], r32[:], E[:], ALU.mult, [r32, E], [KR])
        k.tt("dve", Ki32[:], kmod[:], Einv[:], ALU.mult, [kmod, Einv], [Ki32])
        k.tt("pool", Bi32[:], b_[:], Einv[:], ALU.mult, [b_, Einv], [Bi32])
        k.cp("act", Ki[:], Ki32[:], [Ki32], [Ki])
        k.cp("act", Bi[:], Bi32[:], [Bi32], [Bi])
        for p in range(2):
            for c in range(2):
                k.ts("dve", KeT[:, p, 64 * c:64 * c + 64], Ki32[:, p, 64 * c:64 * c + 64], dcol[:, p, c:c + 1], None,
                     ALU.mult, None, [Ki32, dcol], [KeT])
                k.ts("pool", nBeT[:, p, 64 * c:64 * c + 64], Bi32[:, p, 64 * c:64 * c + 64], ndcol[:, p, c:c + 1], None,
                     ALU.mult, None, [Bi32, ndcol], [nBeT])
        yield
        k.tt("pool", rk32[:], r32[:], kmod[:], ALU.mult, [r32, kmod], [rk32])
        for p in range(2):
            k.ts("pool", rk[:, p, :], rk32[:, p, :], cv[:, p, 4:5], None, ALU.mult, None, [rk32, cv], [rk])
        yield
        for p in range(2):
            k.mm(b4[:, p * 128:(p + 1) * 128], KeT[:, p, :], identb[:], True, True, [KeT, identb], [b4])
            k.mm(b4[:, 256 + p * 128:256 + (p + 1) * 128], nBeT[:, p, :], identb[:], True, True, [nBeT, identb], [b4])
        k.cp("act", Ke[:], b4[:, 0:256], [b4], [Ke])
        k.cp("act", nBe[:], b4[:, 256:512], [b4], [nBe])
        for p in range(2):
            k.mm(b3[:, 256 + 2 * p:256 + 2 * p + 2], rk[:, p, :], bones[:, 0:128:64], True, True, [rk, bones], [b3])
        k.cp("dve", bon[:], b3[:, 256:260], [b3], [bon])
        yield
        for h in range(4):
            p, q = h // 2, h % 2
            bk = b0 if h % 2 == 0 else b1
            ks = slice(q * 64, (q + 1) * 64)
            k.mm(bk[:, 0:256], Ki[ks, p, :], KR[ks, p, :, :].rearrange("k a t -> k (a t)"), True, True, [Ki, KR], [bk])
            k.mm(bk[:, 256:512], Bi[ks, p, :], KR[ks, p, :, :].rearrange("k a t -> k (a t)"), True, True, [Bi, KR], [bk])
            k.tt("dve", AT[:, h, :], bk[:], mask4[:], ALU.mult, [bk, mask4], [AT])
            k.mm(b4[:, h * 128:(h + 1) * 128], KR[ks, p, 0, :], Bi[ks, p, :], True, True, [KR, Bi], [b4])
            yield
        k.tt("dve", PT[0][:], b4[:].rearrange("p (h t) -> p h t", h=4), maskL4[:], ALU.mult, [b4, maskL4], [PT[0]])
        k.cp("pool", P[0][:], AT[:, :, 256:384], [AT], [P[0]])
        k.tt("pool", R[0][:], ident4[:], P[0][:], ALU.subtract, [ident4, P[0]], [R[0]])
        yield
        for j in range(5):
            pc, pn = P[j % 2], P[(j + 1) % 2]
            ptc, ptn = PT[j % 2], PT[(j + 1) % 2]
            rc, rn_ = R[j % 2], (Rfin if j == 4 else R[(j + 1) % 2])
            for h in range(4):
                k.mm(b0[:, h * 128:(h + 1) * 128], pc[:, h, :], ptc[:, h, :], True, True, [pc, ptc], [b0])
            k.cp("act", ptn[:], b0[:].rearrange("p (h t) -> p h t", h=4), [b0], [ptn])
            if j < 4:
                for h in range(4):
                    k.mm(b1[:, h * 128:(h + 1) * 128], ptc[:, h, :], pc[:, h, :], True, True, [pc, ptc], [b1])
                k.cp("act", pn[:], b1[:].rearrange("p (h t) -> p h t", h=4), [b1], [pn])
            for h in range(4):
                k.mm(b3[:, h * 128:(h + 1) * 128], ptn[:, h, :], rc[:, h, :], True, True, [ptn, rc], [b3])
            k.tt("dve", rn_[:], b3[:].rearrange("p (h t) -> p h t", h=4), rc[:], ALU.add, [b3, rc], [rn_])
            yield
        yield

    def stageB(i):
        nonlocal mcur
        KR, AT, Vb, V32, Ke, nBe, dcol, bon, gate, Rfin = bind(i)
        Rf = Rfin
        for c in range(2):
            cs = slice(64 * c, 64 * c + 64)
            mb = Mb[mcur]
            for h in range(4):
                p, q = h // 2, h % 2
                ks = slice(q * 64, (q + 1) * 64)
                hv = slice(h * 64, (h + 1) * 64)
                k.mm(b6[cs, hv], AT[:, h, 64 * c:64 * c + 64], Vb[:, hv], True, False, [AT, Vb], [b6])
                k.mm(b6[cs, hv], KR[:, p, 0, cs], mb[:, h, :], False, True, [KR, mb], [b6])
            k.cp("act", Wt[cs, :], b6[cs, 0:256], [b6], [Wt])
            yield
            for h in range(4):
                hv = slice(h * 64, (h + 1) * 64)
                k.mm(b6[cs, 256 + h * 64:256 + (h + 1) * 64], Rf[:, h, cs], Wt[:, hv], True, True, [Rf, Wt], [b6])
            k.cp("act", Ut[cs, :], b6[cs, 256:512], [b6], [Ut])
            yield
            for h in range(4):
                p, q = h // 2, h % 2
                ks = slice(q * 64, (q + 1) * 64)
                hv = slice(h * 64, (h + 1) * 64)
                k.mm(b7[cs, hv], KR[:, p, 1, cs], mb[:, h, :], True, False, [KR, mb], [b7])
                k.mm(b7[cs, hv], AT[:, h, 128 + 64 * c:128 + 64 * c + 64], Vb[:, hv], False, False, [AT, Vb], [b7])
                k.mm(b7[cs, hv], AT[:, h, 384 + 64 * c:384 + 64 * c + 64], Ut[:, hv], False, True, [AT, Ut], [b7])
            for h in range(4):
                p, q = h // 2, h % 2
                ks = slice(q * 64, (q + 1) * 64)
                hv = slice(h * 64, (h + 1) * 64)
                k.mm(b5[ks, p * 64:(p + 1) * 64], Ke[cs, hv], Vb[cs, hv], True, False, [Ke, Vb], [b5])
                k.mm(b5[ks, p * 64:(p + 1) * 64], nBe[cs, hv], Ut[cs, hv], False, True, [nBe, Ut], [b5])
            for p in range(2):
                k.stt(M32[:, p, :], M32[:, p, :], dcol[:, p, c:c + 1], b5[:, p * 64:(p + 1) * 64], ALU.mult, ALU.add,
                      [M32, dcol, b5], [M32])
            yield
            mcur = (mcur + 1) % 2
            for q in range(2):
                k.ts("pool", Mb[mcur][:, q:4:2, :], M32[:], hmask[:, q:q + 1], None, ALU.mult, None, [M32, hmask], [Mb[mcur]])
        yield
        for h in range(4):
            k.op("dve", lambda e, h=h: e.bn_stats(out=bst[:, h, :], in_=b7[:, h * 64:(h + 1) * 64]), reads=[b7], writes=[bst])
            k.op("dve", lambda e, h=h: e.bn_aggr(out=mv[:, h, :], in_=bst[:, h, :]), reads=[bst], writes=[mv])
        k.act(sdg[:], mv[:, :, 1], AF.Sqrt, [mv, epsg], [sdg], bias=epsg[:], scale=1.0)
        k.recip(rg[:], sdg[:], [sdg], [rg])
        for h in range(4):
            hv = slice(h * 64, (h + 1) * 64)
            k.ts("dve", yn[:, hv], b7[:, hv], mv[:, h, 0:1], rg[:, h:h + 1], ALU.subtract, ALU.mult, [b7, mv, rg], [yn])
        k.tt("pool", yn[:], yn[:], lngb[:], ALU.mult, [yn, lngb], [yn])
        k.tt("pool", yn[:], yn[:], lnbb[:], ALU.add, [yn, lnbb], [yn])
        for h in range(4):
            hv = slice(h * 64, (h + 1) * 64)
            k.stt(yn[:, hv], V32[:, hv], bon[:, h:h + 1], yn[:, hv], ALU.mult, ALU.add, [V32, bon, yn], [yn])
        k.tt("dve", yg[:], yn[:], gate[:], ALU.mult, [yn, gate], [yg])
        for c2 in range(2):
            k.tr(b5[:, c2 * 128:(c2 + 1) * 128], yg[:, c2 * 128:(c2 + 1) * 128], ident[:], [yg, ident], [b5])
        yb = yTb[(i // 4) % 2]
        j = i % 4
        k.op("act", lambda e, yb=yb, j=j: e.copy(out=yb[:, :, j * 128:(j + 1) * 128],
                                                in_=b5[:, 0:256].rearrange("p (c t) -> p c t", c=2)),
             reads=[b5], writes=[yb])
        if j == 3:
            qr = i // 16
            off = ((i % 16) // 4) * 512
            k.dma("sp", y_loc[qr].rearrange("(c p) t -> p c t", p=128)[:, :, off:off + 512], yb[:, :, :], reads=[yb],
                  writes=[yloc_t[qr]], sem=f"ygb{(i // 4) % 2}")
            if i % 16 == 15:
                gather(qr)
    def drain(g, n=None):
        c = 0
        while n is None or c < n:
            try:
                next(g)
            except StopIteration:
                return False
            c += 1
        return True

    drain(stageA(0))
    for i in range(ntiles):
        gB = stageB(i)
        gA = stageA(i + 1) if i + 1 < ntiles else None
        aliveA, aliveB = gA is not None, True
        while aliveA or aliveB:
            if aliveB:
                aliveB = drain(gB, 1)
            if aliveA:
                aliveA = drain(gA, 4)


def phase_ffn(nc, k, mode, hacc, hacc_t, outT, yall, yall_t, wo, g1c, wg, wu, wd, hin=None, g2c=None, hn_loc=None,
              hnloc_t=None, gather=None, rt=None, rb=None, gout=None, out=None):
    moe = mode == "moe"
    NE, FE = (8, 1408) if moe else (1, 2816)
    G = 4
    xnT = k.sb("xnT", [128, 8, TOK], BF16)
    wob = k.sb("wob", [128, 8, D], BF16)
    wgs = [k.sb(f"wgs{i}", [128, 8, G * 128], BF16) for i in range(2)]
    wus = [k.sb(f"wus{i}", [128, 8, G * 128], BF16) for i in range(2)]
    wds = [k.sb(f"wds{i}", [128, G, D], BF16) for i in range(2)]
    actT = [k.sb(f"actT{i}", [128, G, 512], BF16) for i in range(2)]
    sg = [k.sb(f"sg{i}", [128, 512], F32) for i in range(2)]
    xn32 = [k.sb(f"xn32_{i}", [128, D], F32) for i in range(2)]
    junk = k.sb("junk", [128, D], F32)
    ss = k.sb("ss", [128, NT], F32)
    sd = k.sb("sd", [128, NT], F32)
    rstd = k.sb("rstd", [128, NT], F32)
    g1 = k.sb("g1", [128, 8], F32)
    ident = make_ident(k)
    pso = [k.ps(f"pso{i}", [128, D], F32) for i in range(2)]
    psg = [k.ps(f"psg{i}", [128, 512], F32) for i in range(2)]
    psu = [k.ps(f"psu{i}", [128, 512], F32) for i in range(2)]
    xt_t = [k.trk(f"xt{i}") for i in range(NT)]
    if moe:
        rt32 = k.sb("rt32", [128, 8, NE], F32)
        rbb = k.sb("rbb", [128, NE], F32)
        xT32 = k.sb("xT32", [128, D], F32)
        lg = k.sb("lg", [128, NT, NE], F32)
        gates = k.sb("gates", [128, NT, NE], F32)
        mx = k.sb("mx", [128, 8], F32)
        nm1 = k.sb("nm1", [128, 1], F32)
        ex = k.sb("ex", [128, NE], F32)
        msk = k.sb("msk", [128, NE], F32)
        den = k.sb("den", [128, 1], F32)
        rden = k.sb("rden", [128, 1], F32)
        goutb = k.sb("goutb", [128, D], F32)
        xlo = k.sb("xlo", [128, 8, 128], BF16)
        rth = k.sb("rth", [128, 8, NE], BF16)
        rtl = k.sb("rtl", [128, 8, NE], BF16)
        rtd = k.sb("rtd", [128, 8, NE], F32)
        obuf = [k.sb(f"obuf{i}", [128, D], F32) for i in range(2)]
    else:
        g2 = k.sb("g2", [128, 8], F32)

    for m in range(4):
        def src(e, m=m):
            qd = bass.ds(k.pid_sp, 1)
            return yall[qd].rearrange("o r (h p) t -> p (o r h) t", p=128)[:, :, m * 512:(m + 1) * 512]
        k.dma("sp", xnT[:, :, m * 512:(m + 1) * 512], src, reads=yall_t, writes=xt_t[4 * m:4 * m + 4], sem=f"xt{m}")
    if not moe:
        hin_v = hin.rearrange("(n p) d -> p n d", p=128)
        for m in range(4):
            k.dma("act", hacc[:, 4 * m:4 * m + 4, :], hin_v[:, 4 * m:4 * m + 4, :],
                  writes=hacc_t[4 * m:4 * m + 4], sem=f"hacc{m}")
    for c in range(8):
        r0 = c * 128 if moe else ((c % 2) * 512 + (c // 2) * 128)
        k.dma("pool", wob[:, c, :], wo[r0:r0 + 128, :], writes=[wob])
    k.dma("sp", g1[:], g1c, writes=[g1])
    if moe:
        k.dma("sp", rt32[:], rt.rearrange("(c p) e -> p c e", p=128), writes=[rt32])
        k.dma("sp", rbb[:], rb.partition_broadcast(128), writes=[rbb])
        k.dma("sp", goutb[:], gout.partition_broadcast(128), writes=[goutb])
        k.cp("dve", rth[:], rt32[:], [rt32], [rth])
        k.tt("dve", rtd[:], rt32[:], rth[:], ALU.subtract, [rt32, rth], [rtd])
        k.cp("dve", rtl[:], rtd[:], [rtd], [rtl])
    else:
        k.dma("sp", g2[:], g2c, writes=[g2])

    passes = []
    for e in range(NE):
        nchunk = FE // 128
        c0 = 0
        while c0 < nchunk:
            g = min(G, nchunk - c0)
            passes.append((e, c0, g))
            c0 += g

    def load_pass(pi):
        e, c0, g = passes[pi]
        s = pi % 2
        wg_v = wg[e].rearrange("(c p) f -> p c f", p=128)
        wu_v = wu[e].rearrange("(c p) f -> p c f", p=128)
        wd_v = wd[e].rearrange("(c p) d -> p c d", p=128)
        for c in range(0, 8, 4):
            k.dma("pool", wgs[s][:, c:c + 4, 0:g * 128], wg_v[:, c:c + 4, c0 * 128:(c0 + g) * 128], writes=[wgs[s]])
        for c in range(0, 8, 4):
            k.dma("pool", wus[s][:, c:c + 4, 0:g * 128], wu_v[:, c:c + 4, c0 * 128:(c0 + g) * 128], writes=[wus[s]])
        for c in range(0, g, 2):
            c1 = min(c + 2, g)
            k.dma("pool", wds[s][:, c:c1, :], wd_v[:, c0 + c:c0 + c1, :], writes=[wds[s]])

    load_pass(0)

    for i in range(NT):
        p = pso[i % 2]
        for half in range(2):
            for dc in range(8):
                k.op("pe", lambda e, p=p, half=half, dc=dc, i=i: e.matmul(
                    p[:, half * 512:(half + 1) * 512], lhsT=xnT[:, dc, i * 128:(i + 1) * 128],
                    rhs=wob[:, dc, half * 512:(half + 1) * 512], start=(dc == 0), stop=(dc == 7)),
                    reads=[xt_t[i], wob], writes=[p])
        k.op("dve", lambda e, p=p, i=i: e.tensor_tensor(out=hacc[:, i, :], in0=p[:], in1=hacc[:, i, :], op=ALU.add),
             reads=[p, hacc_t[i]], writes=[hacc_t[i]])

    def norm_stats():
        for i in range(NT):
            k.op("act", lambda e, i=i: e.activation(out=junk[:], in_=hacc[:, i, :], func=AF.Square,
                                                     accum_out=ss[:, i:i + 1]),
                 reads=[hacc_t[i]], writes=[junk, ss])
        k.op("act", lambda e: e.activation(out=sd[:], in_=ss[:], func=AF.Sqrt, scale=1.0 / D, bias=epsb[:]),
             reads=[ss, epsb], writes=[sd])
        k.op("dve", lambda e: e.reciprocal(out=rstd[:], in_=sd[:]), reads=[sd], writes=[rstd])

    epsb = k.sb("epsb", [128, 1], F32)
    k.op("dve", lambda e: e.memset(epsb[:], RMS_EPS), writes=[epsb])

    def norm_transpose(gcol, dst_tile_fn, extra=None):
        for i in range(NT):
            xb = xn32[i % 2]
            p = pso[i % 2]
            k.op("act", lambda e, i=i, xb=xb: e.activation(out=xb[:], in_=hacc[:, i, :], func=AF.Copy,
                                                           scale=rstd[:, i:i + 1]),
                 reads=[hacc_t[i], rstd], writes=[xb])
            for dc in range(8):
                k.op("pe", lambda e, dc=dc, xb=xb, p=p: e.transpose(out=p[:, dc * 128:(dc + 1) * 128],
                                                                  in_=xb[:, dc * 128:(dc + 1) * 128],
                                                                  identity=ident[:]),
                     reads=[xb, ident], writes=[p])
            for dc in range(8):
                k.op("dve", lambda e, dc=dc, i=i, p=p: e.tensor_scalar(
                    out=xnT[:, dc, i * 128:(i + 1) * 128], in0=p[:, dc * 128:(dc + 1) * 128],
                    scalar1=gcol[:, dc:dc + 1], scalar2=None, op0=ALU.mult),
                    reads=[p, gcol], writes=[xt_t[i]])
            if extra is not None:
                extra(i, p)

    norm_stats()

    def router(i, p):
        pl = psg[i % 2]
        for dc in range(8):
            k.stt(xlo[:, dc, :], p[:, dc * 128:(dc + 1) * 128], g1[:, dc:dc + 1], xnT[:, dc, i * 128:(i + 1) * 128],
                  ALU.mult, ALU.subtract, [p, g1, xt_t[i]], [xlo])
        for dc in range(8):
            k.mm(pl[:, 0:NE], xnT[:, dc, i * 128:(i + 1) * 128], rth[:, dc, :], dc == 0, False, [xt_t[i], rth], [pl])
            k.mm(pl[:, 0:NE], xlo[:, dc, :], rth[:, dc, :], False, False, [xlo, rth], [pl])
            k.mm(pl[:, 0:NE], xnT[:, dc, i * 128:(i + 1) * 128], rtl[:, dc, :], False, dc == 7, [xt_t[i], rtl], [pl])
        k.op("dve", lambda e, i=i, pl=pl: e.tensor_tensor(out=lg[:, i, :], in0=pl[:, 0:NE], in1=rbb[:], op=ALU.add),
             reads=[pl, rbb], writes=[lg])
        k.op("dve", lambda e, i=i: e.max(out=mx[:], in_=lg[:, i, :]), reads=[lg], writes=[mx])
        k.op("dve", lambda e: e.tensor_scalar(out=nm1[:], in0=mx[:, 0:1], scalar1=-1.0, scalar2=None, op0=ALU.mult),
             reads=[mx], writes=[nm1])
        k.op("act", lambda e, i=i: e.activation(out=ex[:], in_=lg[:, i, :], func=AF.Exp, bias=nm1[:], scale=1.0),
             reads=[lg, nm1], writes=[ex])
        k.op("dve", lambda e, i=i: e.tensor_scalar(out=msk[:], in0=lg[:, i, :], scalar1=mx[:, 1:2], scalar2=None,
                                                 op0=ALU.is_ge), reads=[lg, mx], writes=[msk])
        k.op("dve", lambda e: e.tensor_tensor(out=ex[:], in0=ex[:], in1=msk[:], op=ALU.mult),
             reads=[ex, msk], writes=[ex])
        k.op("dve", lambda e: e.reduce_sum(out=den[:], in_=ex[:], axis=AX.X), reads=[ex], writes=[den])
        k.op("dve", lambda e: e.reciprocal(out=rden[:], in_=den[:]), reads=[den], writes=[rden])
        k.op("dve", lambda e, i=i: e.tensor_scalar(out=gates[:, i, :], in0=ex[:], scalar1=rden[:, 0:1], scalar2=None,
                                                 op0=ALU.mult), reads=[ex, rden], writes=[gates])

    norm_transpose(g1, None, extra=router if moe else None)

    for pi, (e_idx, c0, g) in enumerate(passes):
        s = pi % 2
        if pi + 1 < len(passes):
            load_pass(pi + 1)
        for m in range(4):
            a = actT[m % 2]
            for c in range(g):
                pg = psg[c % 2]
                pu = psu[c % 2]
                for dc in range(8):
                    k.op("pe", lambda e, pg=pg, s=s, c=c, dc=dc, m=m: e.matmul(
                        pg[:], lhsT=wgs[s][:, dc, c * 128:(c + 1) * 128], rhs=xnT[:, dc, m * 512:(m + 1) * 512],
                        start=(dc == 0), stop=(dc == 7)), reads=[wgs[s]] + xt_t[4 * m:4 * m + 4], writes=[pg])
                for dc in range(8):
                    k.op("pe", lambda e, pu=pu, s=s, c=c, dc=dc, m=m: e.matmul(
                        pu[:], lhsT=wus[s][:, dc, c * 128:(c + 1) * 128], rhs=xnT[:, dc, m * 512:(m + 1) * 512],
                        start=(dc == 0), stop=(dc == 7)), reads=[wus[s]] + xt_t[4 * m:4 * m + 4], writes=[pu])
                sgb = sg[c % 2]
                k.op("act", lambda e, sgb=sgb, pg=pg: e.activation(out=sgb[:], in_=pg[:], func=AF.Silu),
                     reads=[pg], writes=[sgb])
                k.op("dve", lambda e, sgb=sgb, pu=pu, a=a, c=c: e.tensor_tensor(out=a[:, c, :], in0=pu[:], in1=sgb[:],
                                                                             op=ALU.mult),
                     reads=[pu, sgb], writes=[a])
            for tt in range(4):
                i = 4 * m + tt
                p = pso[tt % 2]
                for half in range(2):
                    for c in range(g):
                        k.op("pe", lambda e, p=p, half=half, c=c, a=a, tt=tt, s=s, g=g: e.matmul(
                            p[:, half * 512:(half + 1) * 512], lhsT=a[:, c, tt * 128:(tt + 1) * 128],
                            rhs=wds[s][:, c, half * 512:(half + 1) * 512], start=(c == 0), stop=(c == g - 1)),
                            reads=[a, wds[s]], writes=[p])
                if moe:
                    k.op("dve", lambda e, p=p, i=i, e_idx=e_idx: e.scalar_tensor_tensor(
                        out=hacc[:, i, :], in0=p[:], scalar=gates[:, i, e_idx:e_idx + 1], in1=hacc[:, i, :],
                        op0=ALU.mult, op1=ALU.add), reads=[p, gates, hacc_t[i]], writes=[hacc_t[i]])
                else:
                    k.op("dve", lambda e, p=p, i=i: e.tensor_tensor(out=hacc[:, i, :], in0=p[:], in1=hacc[:, i, :],
                                                                   op=ALU.add),
                         reads=[p, hacc_t[i]], writes=[hacc_t[i]])

    out_v = out.rearrange("(n p) d -> p n d", p=128) if moe else None
    if moe:
        norm_stats()
        for i in range(NT):
            ob = obuf[i % 2]
            k.op("dve", lambda e, i=i, ob=ob: e.scalar_tensor_tensor(
                out=ob[:], in0=hacc[:, i, :], scalar=rstd[:, i:i + 1], in1=goutb[:], op0=ALU.mult, op1=ALU.mult),
                reads=[hacc_t[i], rstd, goutb], writes=[ob])
            k.dma("sp", out_v[:, i, :], ob[:], reads=[ob], writes=[outT], sem=f"obuf{i % 2}")
    else:
        norm_stats()
        norm_transpose(g2, None)
        for m in range(4):
            k.dma("act", hn_loc[m].rearrange("(c p) t -> p c t", p=128), xnT[:, :, m * 512:(m + 1) * 512],
                  reads=xt_t[4 * m:4 * m + 4], writes=[hnloc_t[m]], sem=f"xst{m}")
            gather(m)


GROUPS = [[0, 1, 2, 3], [4, 5, 6, 7]]


def build_fused(upto=4, skip=()):
    nc = bass.Bass("TRN2", target_bir_lowering=False)

    def inp(name, shape, dt=F32):
        return nc.dram_tensor(name, list(shape), dt, kind="ExternalInput").ap()
    m_x = inp("m_x", [T, D]); m_win = inp("m_win", [D, 768]); m_gcol = inp("m_gcol", [128, 8])
    m_wsT = inp("m_wsT", [128, 128]); m_gng = inp("m_gng", [128]); m_bsc = inp("m_bsc", [128, 1])
    m_lbl = inp("m_lbl", [128, 2]); m_ong = inp("m_ong", [128])
    f_hin = inp("f_hin", [TOK, D]); f_wo = inp("f_wo", [D, D]); f_g1c = inp("f_g1c", [128, 8])
    f_wg = inp("f_wg", [1, D, 2816]); f_wu = inp("f_wu", [1, D, 2816]); f_wd = inp("f_wd", [1, 2816, D])
    f_g2c = inp("f_g2c", [128, 8])
    r_mixc = inp("r_mixc", [128, 8, 6]); r_wproj = inp("r_wproj", [D, 1024]); r_w2 = inp("r_w2", [64, 256])
    r_a2 = inp("r_a2", [64, 256]); r_g2 = inp("r_g2", [128, 256]); r_cvec = inp("r_cvec", [128, 2, 5])
    r_lng = inp("r_lng", [256]); r_lnb = inp("r_lnb", [256])
    e_wo = inp("e_wo", [D, D]); e_g1c = inp("e_g1c", [128, 8])
    e_wg = inp("e_wg", [8, D, 1408]); e_wu = inp("e_wu", [8, D, 1408]); e_wd = inp("e_wd", [8, 1408, D])
    e_rt = inp("e_rt", [D, 8]); e_rb = inp("e_rb", [8]); e_gout = inp("e_gout", [D])
    out = nc.dram_tensor("out", [TOK, D], F32, kind="ExternalOutput").ap()
    y_loc = nc.dram_tensor("y_loc", [4, 256, 2048], BF16).ap()
    y_all = nc.dram_tensor("y_all", [4, 4, 256, 2048], BF16).ap()
    hn_loc = nc.dram_tensor("hn_loc", [4, 1024, 512], BF16).ap()
    hn_all = nc.dram_tensor("hn_all", [4, 4, 1024, 512], BF16).ap()
    yg_loc = y_loc
    yg_all = y_all

    k = KB(nc)
    hacc = k.sb("hacc", [128, NT, D], F32, glob=True)
    hacc_t = [k.trk(f"hacc{i}") for i in range(NT)]
    outT = k.trk("outT")
    yloc_t = [k.trk(f"yloc{i}") for i in range(4)]
    yall_t = [k.trk(f"yall{i}") for i in range(4)]
    hnloc_t = [k.trk(f"hnloc{i}") for i in range(4)]
    hnall_t = [k.trk(f"hnall{i}") for i in range(4)]
    ygloc_t = [k.trk(f"ygloc{i}") for i in range(4)]
    ygall_t = [k.trk(f"ygall{i}") for i in range(4)]

    def mk_gather(loc, allb, loc_t, all_t, pat, nm):
        def gather(i):
            if nm in _NOCOLL:
                return
            k.coll(lambda e: e.collective_compute("AllGather", ALU.bypass, replica_groups=GROUPS,
                                                  ins=[loc[i].opt()], outs=[allb[i].rearrange(pat).opt()]),
                   f"{nm}{i}", reads=[loc_t[i]], writes=[all_t[i]])
        return gather

    if 1 not in skip:
     k.push("m_")
     phase_mix0(nc, k, m_x, m_win, m_gcol, m_wsT, m_gng, m_bsc, m_lbl, m_ong, y_loc, yloc_t,
               mk_gather(y_loc, y_all, yloc_t, yall_t, "r c t -> (r c) t", "ga"))
     k.pop()
    if upto >= 2 and 2 not in skip:
      k.push("f_")
      phase_ffn(nc, k, "ffn", hacc, hacc_t, outT, y_all, yall_t, f_wo, f_g1c, f_wg, f_wu, f_wd, hin=f_hin, g2c=f_g2c,
              hn_loc=hn_loc, hnloc_t=hnloc_t, gather=mk_gather(hn_loc, hn_all, hnloc_t, hnall_t, "r c t -> (r c) t", "gb"))
      k.pop()
    if upto >= 3:
      k.push("r_")
      phase_rwkv(nc, k, hn_all, hnall_t, r_mixc, r_wproj, r_w2, r_a2, r_g2, r_cvec, r_lng, r_lnb, yg_loc, ygloc_t,
               mk_gather(yg_loc, yg_all, ygloc_t, ygall_t, "r c t -> (r c) t", "gc"))
      k.pop()
    if upto >= 4:
      k.push("e_")
      phase_ffn(nc, k, "moe", hacc, hacc_t, outT, yg_all, ygall_t, e_wo, e_g1c, e_wg, e_wu, e_wd, rt=e_rt, rb=e_rb,
              gout=e_gout, out=out)
      k.wait_all("sp", [outT])
      k.pop()
    k.emit()
    k.close()
    return nc


_UPTO = 4
_NOCOLL = ()
_SKIP = ()
def _gc(g):
    return np.ascontiguousarray(np.asarray(g, np.float32).reshape(8, 128).T)


_NC = {}


def kernel(x, norm_mix_g, norm_ffn_g, norm_out_g,
           mix_w_in, mix_w_out, gmlp_norm_g, gmlp_w_s, gmlp_b_s, hgrn_lb_logits, hgrn_onorm_g,
           ffn_w_gate, ffn_w_up, ffn_w_down,
           rwkv_mix, rwkv_w_r, rwkv_w_k, rwkv_w_v, rwkv_w_o, rwkv_w0, rwkv_w1, rwkv_w2,
           rwkv_a0, rwkv_a1, rwkv_a2, rwkv_g1, rwkv_g2, rwkv_k_k, rwkv_k_a, rwkv_r_k,
           rwkv_ln_g, rwkv_ln_b,
           moe_router, moe_router_b, moe_w_gate, moe_w_up, moe_w_down, _trace=False):
    f32 = np.float32
    A = lambda a: np.ascontiguousarray(np.asarray(a, f32))
    x = A(x)
    xf = x.reshape(16384, 1024)
    cores = list(range(8))
    w = np.asarray(mix_w_in, f32)[0]
    colb = [0 * 512, 1 * 512, 4 * 512, 5 * 512, 2 * 512, 3 * 512]
    mixc = A(np.asarray(rwkv_mix, f32)[0].reshape(6, 8, 128).transpose(2, 1, 0))
    shared = dict(
        m_gcol=_gc(norm_mix_g[0]), m_ong=A(np.asarray(hgrn_onorm_g)[0]),
        f_wo=A(mix_w_out[0]), f_g1c=_gc(norm_ffn_g[0]), f_wg=A(ffn_w_gate), f_wu=A(ffn_w_up), f_wd=A(ffn_w_down),
        f_g2c=_gc(norm_mix_g[1]),
        r_mixc=mixc,
        e_wo=A(rwkv_w_o[0]), e_g1c=_gc(norm_ffn_g[1]), e_wg=A(moe_w_gate[0]), e_wu=A(moe_w_up[0]), e_wd=A(moe_w_down[0]),
        e_rt=A(moe_router[0]), e_rb=A(moe_router_b[0]), e_gout=A(norm_out_g))
    in_maps = []
    for c in cores:
        b, j = c // 4, c % 4
        cs = slice(256 * j, 256 * (j + 1))
        win = np.concatenate([w[:, cb + j * 128: cb + (j + 1) * 128] for cb in colb], axis=1)
        wproj = np.concatenate([np.asarray(rwkv_w_r)[0][:, cs], np.asarray(rwkv_w_k)[0][:, cs],
                                np.asarray(rwkv_w_v)[0][:, cs], np.asarray(rwkv_w1)[0], np.asarray(rwkv_a1)[0],
                                np.asarray(rwkv_g1)[0]], axis=1)
        pv = lambda v: np.asarray(v, f32).reshape(-1)[cs].reshape(2, 128).T
        cvec = np.stack([pv(rwkv_w0[0]), pv(rwkv_a0[0]), pv(rwkv_k_k[0]), pv(rwkv_k_a[0]), pv(rwkv_r_k[0])], axis=-1)
        m = dict(shared)
        m.update(m_x=x[b], m_win=A(win), m_wsT=A(np.asarray(gmlp_w_s)[0, j].T), m_gng=A(np.asarray(gmlp_norm_g)[0, j]),
                 m_bsc=A(np.asarray(gmlp_b_s)[0, j].reshape(128, 1)),
                 m_lbl=A(np.asarray(hgrn_lb_logits)[:, j * 128:(j + 1) * 128].T),
                 f_hin=A(xf[c * 2048:(c + 1) * 2048]),
                 r_wproj=A(wproj), r_w2=A(np.asarray(rwkv_w2)[0][:, cs]), r_a2=A(np.asarray(rwkv_a2)[0][:, cs]),
                 r_g2=A(np.asarray(rwkv_g2)[0][:, cs]), r_cvec=A(cvec),
                 r_lng=A(np.asarray(rwkv_ln_g)[0][cs]), r_lnb=A(np.asarray(rwkv_ln_b)[0][cs]))
        in_maps.append(m)
    if "nc" not in _NC:
        _NC["nc"] = build_fused(_UPTO, _SKIP)
    res = run_bass_kernel_spmd(_NC["nc"], in_maps, core_ids=cores, **({"trace": True} if _trace else {}))
    if _trace:
        print("exec_time_ns", res.exec_time_ns)
    out = np.concatenate([res.results[c]["out"] for c in cores], axis=0).reshape(2, 8192, 1024)
    return np.ascontiguousarray(out.astype(np.float32))
```

```python
import contextlib
import numpy as np
import concourse.bass as bass
import concourse.mybir as mybir
from concourse.bass_utils import run_bass_kernel_spmd

F32 = mybir.dt.float32
BF16 = mybir.dt.bfloat16
I32 = mybir.dt.int32
AF = mybir.ActivationFunctionType
ALU = mybir.AluOpType
AX = mybir.AxisListType


class Trk:
    __slots__ = ("name", "w", "r", "bank")

    def __init__(self, name, bank=None):
        self.name = name
        self.w = []
        self.r = []
        self.bank = bank


class Bank:
    def __init__(self):
        self.last = {}


class Buf:
    def __init__(self, t, name):
        self.t = t
        self.trk = Trk(name)

    def __getitem__(self, idx):
        return self.t[idx]


class KB:
    ENG = ("pe", "act", "dve", "pool", "sp")

    def __init__(self, nc, same_engine_sync=True):
        self.nc = nc
        self.es = contextlib.ExitStack()
        self.sems = {}
        self.cnt = {}
        self.ops = {e: [] for e in self.ENG}
        self.known = {e: {} for e in self.ENG}
        self.same_engine_sync = same_engine_sync
        for e in self.ENG:
            self.sems[e] = self.es.enter_context(nc.semaphore("sem_" + e))
            self.cnt[e] = 0
        self.free_sems = []
        self.phase_keys = []
        self.nsem = 0
        self.pes = None
        self.prefix = ""

    def push(self, prefix=""):
        assert self.pes is None
        self.pes = contextlib.ExitStack()
        self.phase_keys = []
        self.prefix = prefix

    def barrier(self):
        allk = [(k_, v) for k_, v in self.cnt.items() if v > 0]
        for e in self.ENG:
            waits = []
            kn = self.known[e]
            for k_, v in allk:
                if k_ != e and kn.get(k_, 0) < v:
                    kn[k_] = v
                    waits.append((k_, v))
            if waits:
                self.ops[e].append((waits, None, None, 0))

    def pop(self):
        self.barrier()
        self.pes.close()
        self.pes = None
        for key in self.phase_keys:
            self.free_sems.append((self.sems[key], self.cnt[key]))
        self.phase_keys = []

    def sb(self, name, shape, dt, glob=False):
        name = self.prefix + name
        es = self.es if (glob or self.pes is None) else self.pes
        t = es.enter_context(self.nc.sbuf_tensor(name, list(shape), dt))
        return Buf(t, name)

    def ps(self, name, shape, dt=F32):
        name = self.prefix + name
        es = self.es if self.pes is None else self.pes
        t = es.enter_context(self.nc.psum_tensor(name, list(shape), dt))
        b = Buf(t, name)
        b.trk.bank = Bank()
        return b

    def trk(self, name, bank=None):
        return Trk(self.prefix + name, bank.trk.bank if bank is not None else None)

    def dsem(self, name):
        key = "d_" + name
        if key not in self.sems:
            if self.free_sems:
                h, c = self.free_sems.pop()
                self.sems[key] = h
                self.cnt[key] = c
            else:
                self.nsem += 1
                self.sems[key] = self.es.enter_context(self.nc.semaphore("dsem%d" % self.nsem))
                self.cnt[key] = 0
            if self.pes is not None:
                self.phase_keys.append(key)
        return key

    @staticmethod
    def _t(b):
        return b.trk if isinstance(b, Buf) else b

    def _deps(self, eng, reads, writes):
        need = {}

        def add(tok):
            k, v = tok
            if k == eng and (eng == "pe" or not self.same_engine_sync):
                return
            if need.get(k, 0) < v:
                need[k] = v
        for b in reads:
            for tok in self._t(b).w:
                add(tok)
        for b in writes:
            t = self._t(b)
            for tok in t.w:
                add(tok)
            for tok in t.r:
                add(tok)
        for b in list(reads) + list(writes):
            bk = self._t(b).bank
            if bk is not None:
                for e2, v in bk.last.items():
                    if e2 != eng and need.get(e2, 0) < v:
                        need[e2] = v
        waits = []
        kn = self.known[eng]
        for k, v in need.items():
            if kn.get(k, 0) < v:
                kn[k] = v
                waits.append((k, v))
        return waits

    def _commit(self, tok, reads, writes, dma_group=False):
        for b in list(reads) + list(writes):
            bk = self._t(b).bank
            if bk is not None and not tok[0].startswith("d_"):
                bk.last[tok[0]] = tok[1]
        for b in reads:
            t = self._t(b)
            t.r = [x for x in t.r if x[0] != tok[0]] + [tok]
        for b in writes:
            t = self._t(b)
            if dma_group and t.w and all(x[0].startswith("d_") for x in t.w) and not t.r:
                t.w = [x for x in t.w if x[0] != tok[0]] + [tok]
            else:
                t.w = [tok]
                t.r = []

    def op(self, eng, fn, reads=(), writes=()):
        waits = self._deps(eng, reads, writes)
        self.cnt[eng] += 1
        tok = (eng, self.cnt[eng])
        self.ops[eng].append((waits, fn, eng, 1))
        self._commit(tok, reads, writes)

    def dma(self, eng, out, in_, reads=(), writes=(), sem=None, **kw):
        if sem is None:
            b = (list(writes) + list(reads))[0]
            sem = self._t(b).name
        elif not sem.startswith(self.prefix):
            sem = self.prefix + sem
        key = self.dsem(sem)
        waits = self._deps_dma(eng, reads, writes)
        self.cnt[key] += 16
        tok = (key, self.cnt[key])
        self.ops[eng].append((waits, lambda e: e.dma_start(out=out(e) if callable(out) else out,
                                                            in_=in_(e) if callable(in_) else in_, **kw), key, 16))
        self._commit(tok, reads, writes, dma_group=True)

    def coll(self, fn, name, reads=(), writes=()):
        key = self.dsem("cc_" + name)
        waits = self._deps_dma("pool", reads, writes)
        self.cnt[key] += 1
        tok = (key, self.cnt[key])
        self.ops["pool"].append((waits, fn, key, 1))
        self._commit(tok, reads, writes, dma_group=True)

    def _deps_dma(self, eng, reads, writes):
        need = {}

        def add(tok):
            k, v = tok
            if need.get(k, 0) < v:
                need[k] = v
        for b in reads:
            for tok in self._t(b).w:
                add(tok)
        for b in writes:
            t = self._t(b)
            if not (t.w and all(x[0].startswith("d_") for x in t.w) and not t.r):
                for tok in t.w:
                    add(tok)
            for tok in t.r:
                add(tok)
        waits = []
        kn = self.known[eng]
        for k, v in need.items():
            if kn.get(k, 0) < v:
                kn[k] = v
                waits.append((k, v))
        return waits

    def wait_all(self, eng, bufs):
        need = {}
        for b in bufs:
            t = self._t(b)
            for k, v in t.w + t.r:
                if need.get(k, 0) < v:
                    need[k] = v
        waits = [(k, v) for k, v in need.items()]
        self.ops[eng].append((waits, None, None, 0))

    def emit(self):
        nc = self.nc
        sems = self.sems
        ops = self.ops
        with nc.Block() as block:
            def run(e, lst):
                for waits, fn, key, inc in lst:
                    for k, v in waits:
                        e.wait_ge(sems[k], v)
                    if fn is not None:
                        fn(e).then_inc(sems[key], inc)

            @block.tensor
            def _(e):
                run(e, ops["pe"])

            @block.scalar
            def _(e):
                run(e, ops["act"])

            @block.vector
            def _(e):
                run(e, ops["dve"])

            @block.gpsimd
            def _(e):
                run(e, ops["pool"])

            @block.sync
            def _(e):
                self.pid_sp = e.partition_id() % 4
                run(e, ops["sp"])

    def close(self):
        self.es.close()


def _act(self, out, in_, func, R, W, scale=None, bias=None, accum=None):
    kw = {}
    if scale is not None:
        kw["scale"] = scale
    if bias is not None:
        kw["bias"] = bias
    if accum is not None:
        kw["accum_out"] = accum
    self.op("act", lambda e: e.activation(out=out, in_=in_, func=func, **kw), reads=R, writes=W)


def _tt(self, eng, out, in0, in1, op, R, W):
    self.op(eng, lambda e: e.tensor_tensor(out=out, in0=in0, in1=in1, op=op), reads=R, writes=W)


def _ts(self, eng, out, in0, s1, s2, op0, op1, R, W):
    if op1 is None:
        self.op(eng, lambda e: e.tensor_scalar(out=out, in0=in0, scalar1=s1, scalar2=None, op0=op0), reads=R, writes=W)
    else:
        self.op(eng, lambda e: e.tensor_scalar(out=out, in0=in0, scalar1=s1, scalar2=s2, op0=op0, op1=op1), reads=R, writes=W)


def _stt(self, out, in0, scalar, in1, op0, op1, R, W):
    self.op("dve", lambda e: e.scalar_tensor_tensor(out=out, in0=in0, scalar=scalar, in1=in1, op0=op0, op1=op1),
            reads=R, writes=W)


def _mm(self, out, lhsT, rhs, start, stop, R, W):
    self.op("pe", lambda e: e.matmul(out, lhsT=lhsT, rhs=rhs, start=start, stop=stop), reads=R, writes=W)


def _tr(self, out, in_, ident, R, W):
    self.op("pe", lambda e: e.transpose(out=out, in_=in_, identity=ident), reads=R, writes=W)


def _cp(self, eng, out, in_, R, W):
    if eng == "act":
        self.op("act", lambda e: e.copy(out=out, in_=in_), reads=R, writes=W)
    else:
        self.op(eng, lambda e: e.tensor_copy(out=out, in_=in_), reads=R, writes=W)


def _recip(self, out, in_, R, W):
    self.op("dve", lambda e: e.reciprocal(out=out, in_=in_), reads=R, writes=W)


KB.act = _act
KB.tt = _tt
KB.ts = _ts
KB.stt = _stt
KB.mm = _mm
KB.tr = _tr
KB.cp = _cp
KB.recip = _recip


D = 1024
NT = 16
TOK = 2048
RMS_EPS = 1e-6


def make_ident(k, name="ident32", dt=F32):
    ident = k.sb(name, [128, 128], dt)
    k.op("pool", lambda e: e.memset(ident[:], 0.0), writes=[ident])
    k.op("pool", lambda e: e.affine_select(out=ident[:], in_=ident[:], pattern=[[-1, 128]],
                                           compare_op=ALU.not_equal, fill=1.0, base=0,
                                           channel_multiplier=1), reads=[ident], writes=[ident])
    return ident


T = 8192
LN_EPS = 1e-5
GN_EPS = 64e-5
CW = 0.6065306597126334


def phase_mix0(nc, k, x, win, gcol, wsT, gng, bsc, lbl, ong, y_loc, yloc_t, gather):
    ntiles = 64
    ident = make_ident(k)
    identb = k.sb("identb", [128, 128], BF16)
    k.cp("dve", identb[:], ident[:], [ident], [identb])
    winb = k.sb("winb", [128, 8, 768], BF16)
    g1 = k.sb("g1", [128, 8], F32)
    wsf = k.sb("wsf", [128, 128], F32)
    wsb = k.sb("wsb", [128, 128], BF16)
    gnb = k.sb("gnb", [128, 128], F32)
    onb = k.sb("onb", [128, 128], F32)
    bs = k.sb("bs", [128, 1], F32)
    lb2 = k.sb("lb2", [128, 2], F32)
    lbd = k.sb("lbd", [128, 1], F32)
    lb = k.sb("lb", [128, 1], F32)
    oml = k.sb("oml", [128, 1], F32)
    maskT = k.sb("maskT", [128, 128], F32)
    rmask = k.sb("rmask", [128, 128], F32)
    epsr = k.sb("epsr", [128, 1], F32)
    epsl = k.sb("epsl", [128, 1], F32)
    S32 = k.sb("S32", [128, 128], F32)
    Sbf = [k.sb(f"Sbf{i}", [128, 128], BF16) for i in range(4)]

    win_v = win.rearrange("(c p) f -> p c f", p=128)
    for c in range(0, 8, 2):
        k.dma("pool", winb[:, c:c + 2, :], win_v[:, c:c + 2, :], writes=[winb])
    k.dma("sp", g1[:], gcol, writes=[g1])
    k.dma("sp", wsf[:], wsT, writes=[wsf])
    k.dma("sp", gnb[:], gng.partition_broadcast(128), writes=[gnb])
    k.dma("sp", onb[:], ong.partition_broadcast(128), writes=[onb])
    k.dma("sp", bs[:], bsc, writes=[bs])
    k.dma("sp", lb2[:], lbl, writes=[lb2])

    k.op("dve", lambda e: e.memset(epsr[:], RMS_EPS), writes=[epsr])
    k.op("dve", lambda e: e.memset(epsl[:], LN_EPS), writes=[epsl])
    k.op("dve", lambda e: e.memset(S32[:], 0.0), writes=[S32])
    k.op("dve", lambda e: e.memset(Sbf[0][:], 0.0), writes=[Sbf[0]])
    k.op("pool", lambda e: e.affine_select(out=wsf[:], in_=wsf[:], pattern=[[1, 128]], compare_op=ALU.is_ge,
                                           fill=0.0, base=0, channel_multiplier=-1), reads=[wsf], writes=[wsf])
    k.cp("dve", wsb[:], wsf[:], [wsf], [wsb])
    k.op("pool", lambda e: e.memset(maskT[:], 1.0), writes=[maskT])
    k.op("pool", lambda e: e.affine_select(out=maskT[:], in_=maskT[:], pattern=[[1, 128]], compare_op=ALU.is_ge,
                                           fill=0.0, base=0, channel_multiplier=-1), reads=[maskT], writes=[maskT])
    for n in range(1, 4):
        k.op("pool", lambda e, n=n: e.affine_select(out=maskT[:, 32 * n:32 * n + 32], in_=maskT[:, 32 * n:32 * n + 32],
                                                    pattern=[[0, 32]], compare_op=ALU.is_ge, fill=0.0,
                                                    base=-32 * n, channel_multiplier=1),
             reads=[maskT], writes=[maskT])
    k.op("pool", lambda e: e.memset(rmask[:], 1.0), writes=[rmask])
    for n in range(4):
        k.op("pool", lambda e, n=n: e.memset(rmask[:, 32 * n:32 * n + 1], 0.0), reads=[rmask], writes=[rmask])
    rm3 = k.sb("rm3", [128, 1], F32)
    k.op("pool", lambda e: e.memset(rm3[:], 1.0), writes=[rm3])
    k.op("pool", lambda e: e.affine_select(out=rm3[:], in_=rm3[:], pattern=[[0, 1]], compare_op=ALU.is_ge, fill=0.0,
                                           base=-96, channel_multiplier=1), reads=[rm3], writes=[rm3])
    qdec3 = k.sb("qdec3", [128, 64], BF16)
    k.op("pool", lambda e: e.memset(qdec3[:], 0.0), writes=[qdec3])
    kend3 = k.sb("kend3", [128, 128], BF16)
    k.tt("dve", lbd[:], lb2[:, 0:1], lb2[:, 1:2], ALU.subtract, [lb2], [lbd])
    k.act(lb[:], lbd[:], AF.Sigmoid, [lbd], [lb])
    k.ts("dve", oml[:], lb[:], -1.0, 1.0, ALU.mult, ALU.add, [lb], [oml])

    xt = [k.sb(f"xt{i}", [128, D], F32) for i in range(3)]
    junk = k.sb("junk", [128, D], F32)
    xn32 = [k.sb(f"xn32_{i}", [128, D], F32) for i in range(2)]
    xnT = [k.sb(f"xnT{i}", [128, 8, 128], BF16) for i in range(2)]
    ss = k.sb("ss", [128, 1], F32)
    sd = k.sb("sd", [128, 1], F32)
    rstd = k.sb("rstd", [128, 1], F32)
    uv2 = [k.sb(f"uv{_i}", [128, 256], F32) for _i in range(2)]
    bst = k.sb("bst", [128, 6], F32)
    mv = k.sb("mv", [128, 2], F32)
    sdv = k.sb("sdv", [128, 1], F32)
    rv = k.sb("rv", [128, 1], F32)
    vn = k.sb("vn", [128, 128], F32)
    vnb = k.sb("vnb", [128, 128], BF16)
    ycat = k.sb("ycat", [128, 256], F32)
    sigT2 = [k.sb(f"sigT{_i}", [128, 128], F32) for _i in range(2)]
    fT = k.sb("fT", [128, 128], F32)
    logfT = k.sb("logfT", [128, 128], F32)
    bT = k.sb("bT", [128, 128], F32)
    E = k.sb("E", [128, 128], F32)
    Einv = k.sb("Einv", [128, 128], F32)
    sq2 = [k.sb(f"sq{_i}", [128, 128], F32) for _i in range(2)]
    qdec = k.sb("qdec", [128, 128], BF16)
    omf = k.sb("omf", [128, 128], F32)
    kdec32 = k.sb("kdec32", [128, 128], F32)
    kdecb = k.sb("kdecb", [128, 128], BF16)
    dcol = k.sb("dcol", [128, 4], F32)
    kendT = k.sb("kendT", [128, 128], BF16)
    kend = k.sb("kend", [128, 128], BF16)
    vb2 = [k.sb(f"vb{_i}", [128, 128], BF16) for _i in range(2)]
    scT = k.sb("scT", [128, 128], BF16)
    sso = k.sb("sso", [128, 1], F32)
    sdo = k.sb("sdo", [128, 1], F32)
    ro = k.sb("ro", [128, 1], F32)
    on = k.sb("on", [128, 128], F32)
    sgt2 = [k.sb(f"sgt{_i}", [128, 128], F32) for _i in range(2)]
    junk2 = k.sb("junk2", [128, 128], F32)
    yTb = [k.sb(f"yTb{i}", [128, 2, 512], BF16) for i in range(2)]

    pt = k.ps("pt", [128, D], F32)
    ptok = k.ps("ptok", [128, 512], F32)
    pqf = k.ps("pqf", [128, 512], F32)
    pE = k.ps("pE", [128, 512], F32)
    pEb = k.ps("pEb", [128, 1024], BF16)
    po = k.ps("po", [128, 512], F32)
    pinc = [k.ps(f"pinc{i}", [128, 512], F32) for i in range(1)]
    pm_t, psc_t, pTy_t = k.trk("pm", pE), k.trk("psc", pE), k.trk("pTy", pE)

    x_v = x.rearrange("(n p) d -> n p d", p=128)
    scur = 0
    def bind(i):
        return (uv2[i % 2], sigT2[i % 2], sq2[i % 2], sgt2[i % 2], vb2[i % 2])

    def stageA(i):
        uv, sigT, sq, sgt, vb = bind(i)
        xb = xt[i % 3]
        k.dma("sp", xb[:], x_v[i], writes=[xb])
        k.act(junk[:], xb[:], AF.Square, [xb], [junk, ss], accum=ss[:])
        k.act(sd[:], ss[:], AF.Sqrt, [ss, epsr], [sd], scale=1.0 / D, bias=epsr[:])
        k.recip(rstd[:], sd[:], [sd], [rstd])
        yield
        xn = xn32[i % 2]
        k.act(xn[:], xb[:], AF.Copy, [xb, rstd], [xn], scale=rstd[:])
        for dc in range(8):
            k.tr(pt[:, dc * 128:(dc + 1) * 128], xn[:, dc * 128:(dc + 1) * 128], ident[:], [xn, ident], [pt])
        xT = xnT[i % 2]
        for dc in range(8):
            k.ts("dve", xT[:, dc, :], pt[:, dc * 128:(dc + 1) * 128], g1[:, dc:dc + 1], None, ALU.mult, None,
                 [pt, g1], [xT])
        yield
        for dc in range(8):
            k.mm(ptok[:], xT[:, dc, :], winb[:, dc, 0:512], dc == 0, dc == 7, [xT, winb], [ptok])
        for dc in range(8):
            k.mm(pqf[:, 0:128], winb[:, dc, 512:640], xT[:, dc, :], dc == 0, dc == 7, [xT, winb], [pqf])
        for dc in range(8):
            k.mm(pqf[:, 128:256], winb[:, dc, 640:768], xT[:, dc, :], dc == 0, dc == 7, [xT, winb], [pqf])
        yield
        k.act(uv[:], ptok[:, 0:256], AF.Gelu_apprx_tanh, [ptok], [uv])
        k.act(sigT[:], pqf[:, 128:256], AF.Sigmoid, [pqf], [sigT])
        k.act(sq[:], pqf[:, 0:128], AF.Silu, [pqf], [sq])
        k.act(sgt[:], ptok[:, 384:512], AF.Silu, [ptok], [sgt])
        k.cp("act", vb[:], ptok[:, 256:384], [ptok], [vb])
        yield

    def stageB(i):
        nonlocal scur
        uv, sigT, sq, sgt, vb = bind(i)
        k.op("dve", lambda e: e.bn_stats(out=bst[:], in_=uv[:, 128:256]), reads=[uv], writes=[bst])
        k.op("dve", lambda e: e.bn_aggr(out=mv[:], in_=bst[:]), reads=[bst], writes=[mv])
        k.act(sdv[:], mv[:, 1:2], AF.Sqrt, [mv, epsl], [sdv], scale=1.0, bias=epsl[:])
        k.recip(rv[:], sdv[:], [sdv], [rv])
        k.ts("dve", vn[:], uv[:, 128:256], mv[:, 0:1], rv[:, 0:1], ALU.subtract, ALU.mult, [uv, mv, rv], [vn])
        k.tt("pool", vnb[:], vn[:], gnb[:], ALU.mult, [vn, gnb], [vnb])
        k.mm(pE[:, 0:128], wsb[:], vnb[:], True, True, [wsb, vnb], [pm_t])
        k.stt(ycat[:, 0:128], pE[:, 0:128], bs[:, 0:1], uv[:, 0:128], ALU.add, ALU.mult, [pm_t, bs, uv], [ycat])
        yield
        k.ts("dve", fT[:], sigT[:], oml[:, 0:1], lb[:, 0:1], ALU.mult, ALU.add, [sigT, oml, lb], [fT])
        k.act(logfT[:], fT[:], AF.Ln, [fT], [logfT])
        k.op("dve", lambda e: e.tensor_tensor_scan(out=bT[:], data0=rmask[:], data1=logfT[:], initial=0.0,
                                                   op0=ALU.mult, op1=ALU.add), reads=[rmask, logfT], writes=[bT])
        yield
        k.act(E[:], bT[:], AF.Exp, [bT], [E])
        k.act(Einv[:], bT[:], AF.Exp, [bT], [Einv], scale=-1.0)
        k.op("act", lambda e: e.activation(out=dcol[:], in_=bT[:, 31:128:32], func=AF.Exp), reads=[bT], writes=[dcol])
        k.tt("dve", qdec[:], sq[:], E[:], ALU.mult, [sq, E], [qdec])
        k.cp("pool", qdec3[:, 32:64], qdec[:, 96:128], [qdec], [qdec3])
        k.ts("pool", omf[:], fT[:], -1.0, 1.0, ALU.mult, ALU.add, [fT], [omf])
        k.tt("dve", kdec32[:], omf[:], Einv[:], ALU.mult, [omf, Einv], [kdec32])
        k.cp("pool", kdecb[:], kdec32[:], [kdec32], [kdecb])
        for n in range(4):
            k.ts("dve", kendT[:, 32 * n:32 * n + 32], kdec32[:, 32 * n:32 * n + 32], dcol[:, n:n + 1], None, ALU.mult, None,
                 [kdec32, dcol], [kendT])
        yield
        k.tr(pEb[:, 0:128], kendT[:], identb[:], [kendT, identb], [pEb])
        k.cp("act", kend[:], pEb[:, 0:128], [pEb], [kend])
        k.ts("dve", kend3[:], pEb[:, 0:128], rm3[:, 0:1], None, ALU.mult, None, [pEb, rm3], [kend3])
        k.mm(pE[:, 128:256], kdecb[:], qdec[:], True, True, [kdecb, qdec], [psc_t])
        k.tt("dve", scT[:], pE[:, 128:256], maskT[:], ALU.mult, [psc_t, maskT], [scT])
        yield
        k.mm(po[:, 0:128], scT[:], vb[:], True, False, [scT, vb], [po])
        for n in range(4):
            sb_cur = Sbf[scur]
            pi_ = pinc[0]
            if n < 3:
                k.mm(po[32 * n:32 * n + 32, 0:128], qdec[:, 32 * n:32 * n + 32], sb_cur[:], False, n < 2,
                     [qdec, sb_cur], [po])
                k.mm(pi_[:, 0:128], kend[32 * n:32 * n + 32, :], vb[32 * n:32 * n + 32, :], True, True, [kend, vb], [pi_])
            else:
                k.mm(po[64:128, 0:128], qdec3[:, 0:64], sb_cur[:], False, True, [qdec3, sb_cur], [po])
                k.mm(pi_[:, 0:128], kend3[64:128, :], vb[64:128, :], True, True, [kend3, vb], [pi_])
            k.stt(S32[:], S32[:], dcol[:, n:n + 1], pi_[:, 0:128], ALU.mult, ALU.add, [S32, dcol, pi_], [S32])
            yield
            scur = (scur + 1) % 4
            k.cp("act", Sbf[scur][:], S32[:], [S32], [Sbf[scur]])
        yield
        k.act(junk2[:], po[:, 0:128], AF.Square, [po], [junk2, sso], accum=sso[:])
        k.act(sdo[:], sso[:], AF.Sqrt, [sso, epsr], [sdo], scale=1.0 / 128, bias=epsr[:])
        k.recip(ro[:], sdo[:], [sdo], [ro])
        k.stt(on[:], po[:, 0:128], ro[:, 0:1], onb[:], ALU.mult, ALU.mult, [po, ro, onb], [on])
        k.tt("dve", ycat[:, 128:256], on[:], sgt[:], ALU.mult, [on, sgt], [ycat])
        yield
        for c in range(2):
            k.tr(pE[:, 256 + c * 128:256 + (c + 1) * 128], ycat[:, c * 128:(c + 1) * 128], ident[:], [ycat, ident], [pTy_t])
        yb = yTb[(i // 4) % 2]
        j = i % 4
        k.op("act", lambda e, yb=yb, j=j: e.copy(out=yb[:, :, j * 128:(j + 1) * 128],
                                                in_=pE[:, 256:512].rearrange("p (c t) -> p c t", c=2)),
             reads=[pTy_t], writes=[yb])
        if j == 3:
            qr = i // 16
            off = ((i % 16) // 4) * 512
            k.dma("sp", y_loc[qr].rearrange("(c p) t -> p c t", p=128)[:, :, off:off + 512], yb[:, :, :], reads=[yb],
                  writes=[yloc_t[qr]], sem=f"yTb{(i // 4) % 2}")
            if i % 16 == 15:
                gather(qr)
    def drain(g, n=None):
        c = 0
        while n is None or c < n:
            try:
                next(g)
            except StopIteration:
                return False
            c += 1
        return True

    drain(stageA(0))
    for i in range(ntiles):
        gB = stageB(i)
        gA = stageA(i + 1) if i + 1 < ntiles else None
        aliveA, aliveB = gA is not None, True
        while aliveA or aliveB:
            if aliveB:
                aliveB = drain(gB, 1)
            if aliveA:
                aliveA = drain(gA, 1)


def phase_rwkv(nc, k, hn_all, hnall_t, mixc, wproj, w2, a2, g2, cvec, lng, lnb, y_loc, yloc_t, gather):
    ntiles = 64
    ident = make_ident(k)
    identb = k.sb("identb", [128, 128], BF16)
    k.cp("dve", identb[:], ident[:], [ident], [identb])
    ident4 = k.sb("ident4", [128, 4, 128], F32)
    for h in range(4):
        k.cp("pool", ident4[:, h, :], ident[:], [ident], [ident4])

    mx6 = k.sb("mx6", [128, 8, 6], F32)
    omx6 = k.sb("omx6", [128, 8, 6], F32)
    k.dma("sp", mx6[:], mixc, writes=[mx6])
    k.ts("dve", omx6[:], mx6[:], -1.0, 1.0, ALU.mult, ALU.add, [mx6], [omx6])
    Wa = k.sb("Wa", [128, 8, 1024], BF16)
    Wb = k.sb("Wb", [128, 8, 1024], BF16)
    wst = [k.sb(f"wst{i}", [128, 1024], F32) for i in range(2)]
    blocks = [(0, 256, 0), (256, 512, 2), (512, 768, 3), (768, 832, 1), (832, 896, 4), (896, 1024, 5)]
    wp_v = wproj.rearrange("(c p) f -> p c f", p=128)
    for dc in range(8):
        st = wst[dc % 2]
        k.dma("sp", st[:], wp_v[:, dc, :], writes=[st])
        for (c0, c1, mi) in blocks:
            k.ts("dve", Wb[:, dc, c0:c1], st[:, c0:c1], mx6[:, dc, mi:mi + 1], None, ALU.mult, None, [st, mx6], [Wb])
            k.ts("pool", Wa[:, dc, c0:c1], st[:, c0:c1], omx6[:, dc, mi:mi + 1], None, ALU.mult, None, [st, omx6], [Wa])
    w2b = k.sb("w2b", [128, 256], BF16)
    a2b = k.sb("a2b", [128, 256], BF16)
    g2b = k.sb("g2b", [128, 256], BF16)
    k.dma("pool", w2b[0:64, :], w2, writes=[w2b])
    k.dma("pool", a2b[64:128, :], a2, writes=[a2b])
    k.dma("pool", g2b[:], g2, writes=[g2b])
    cv = k.sb("cv", [128, 2, 5], F32)
    k.dma("sp", cv[:], cvec, writes=[cv])
    omka = k.sb("omka", [128, 2], F32)
    k.ts("dve", omka[:], cv[:, :, 3], -1.0, 1.0, ALU.mult, ALU.add, [cv], [omka])
    lngb = k.sb("lngb", [128, 256], F32)
    lnbb = k.sb("lnbb", [128, 256], F32)
    k.dma("sp", lngb[:], lng.partition_broadcast(128), writes=[lngb])
    k.dma("sp", lnbb[:], lnb.partition_broadcast(128), writes=[lnbb])

    def mk_mask(name, strict):
        m = k.sb(name, [128, 128], F32)
        k.op("pool", lambda e: e.memset(m[:], 1.0), writes=[m])
        k.op("pool", lambda e: e.affine_select(out=m[:], in_=m[:], pattern=[[1, 128]],
                                               compare_op=ALU.is_gt if strict else ALU.is_ge, fill=0.0, base=0,
                                               channel_multiplier=-1), reads=[m], writes=[m])
        k.op("pool", lambda e: e.affine_select(out=m[:, 64:128], in_=m[:, 64:128], pattern=[[0, 64]],
                                               compare_op=ALU.is_ge, fill=0.0, base=-64, channel_multiplier=1),
             reads=[m], writes=[m])
        return m
    maskS = mk_mask("maskS", True)
    maskI = mk_mask("maskI", False)
    maskL = k.sb("maskL", [128, 128], F32)
    k.op("pool", lambda e: e.memset(maskL[:], 1.0), writes=[maskL])
    k.op("pool", lambda e: e.affine_select(out=maskL[:], in_=maskL[:], pattern=[[-1, 128]], compare_op=ALU.is_gt,
                                           fill=0.0, base=0, channel_multiplier=1), reads=[maskL], writes=[maskL])
    k.op("pool", lambda e: e.affine_select(out=maskL[:, 0:64], in_=maskL[:, 0:64], pattern=[[0, 64]],
                                           compare_op=ALU.is_ge, fill=0.0, base=63, channel_multiplier=-1),
         reads=[maskL], writes=[maskL])
    maskL4 = k.sb("maskL4", [128, 4, 128], F32)
    for h in range(4):
        k.cp("pool", maskL4[:, h, :], maskL[:], [maskL], [maskL4])
    mask4 = k.sb("mask4", [128, 512], F32)
    k.cp("pool", mask4[:, 0:128], maskS[:], [maskS], [mask4])
    k.cp("pool", mask4[:, 128:256], maskI[:], [maskI], [mask4])
    k.cp("pool", mask4[:, 256:384], maskS[:], [maskS], [mask4])
    k.ts("pool", mask4[:, 384:512], maskI[:], -1.0, None, ALU.mult, None, [maskI], [mask4])
    rmask = k.sb("rmask", [128, 2, 128], F32)
    k.op("pool", lambda e: e.memset(rmask[:], 1.0), writes=[rmask])
    for p in range(2):
        for c in range(2):
            k.op("pool", lambda e, p=p, c=c: e.memset(rmask[:, p, 64 * c:64 * c + 1], 0.0), reads=[rmask], writes=[rmask])
    bones = k.sb("bones", [128, 128], BF16)
    k.op("pool", lambda e: e.memset(bones[:], 0.0), writes=[bones])
    k.op("pool", lambda e: e.memset(bones[0:64, 0:64], 1.0), reads=[bones], writes=[bones])
    k.op("pool", lambda e: e.memset(bones[64:128, 64:128], 1.0), reads=[bones], writes=[bones])
    epsg = k.sb("epsg", [128, 1], F32)
    k.op("dve", lambda e: e.memset(epsg[:], GN_EPS), writes=[epsg])
    epsk = k.sb("epsk", [128, 1], F32)
    k.op("dve", lambda e: e.memset(epsk[:], 1e-24), writes=[epsk])

    M32 = k.sb("M32", [128, 2, 64], F32)
    Mb = [k.sb(f"Mb{i}", [128, 4, 64], BF16) for i in range(2)]
    k.op("dve", lambda e: e.memset(M32[:], 0.0), writes=[M32])
    k.op("dve", lambda e: e.memset(Mb[0][:], 0.0), writes=[Mb[0]])
    hmask = k.sb("hmask", [128, 2], F32)
    k.op("pool", lambda e: e.memset(hmask[:], 0.0), writes=[hmask])
    k.op("pool", lambda e: e.memset(hmask[0:64, 0:1], 1.0), reads=[hmask], writes=[hmask])
    k.op("pool", lambda e: e.memset(hmask[64:128, 1:2], 1.0), reads=[hmask], writes=[hmask])
    mcur = 0

    hx = [k.sb(f"hx{i}", [128, 8, 128], BF16) for i in range(2)]
    hs = k.sb("hs", [128, 8, 128], BF16)
    k.op("pool", lambda e: e.memset(hs[:, :, 0:1], 0.0), writes=[hs])
    l1 = k.sb("l1", [128, 128], BF16)
    sg1 = k.sb("sg1", [128, 128], BF16)
    sigw = k.sb("sigw", [128, 2, 128], F32)
    a_ = k.sb("a_", [128, 2, 128], F32)
    kku = k.sb("kku", [128, 2, 128], F32)
    sqk = k.sb("sqk", [128, 2, 128], BF16)
    sdk = k.sb("sdk", [128, 2, 128], F32)
    rn = k.sb("rn", [128, 2, 128], F32)
    kk = k.sb("kk", [128, 2, 128], F32)
    b_ = k.sb("b_", [128, 2, 128], F32)
    kmf = k.sb("kmf", [128, 2, 128], F32)
    kmod = k.sb("kmod", [128, 2, 128], F32)
    r32 = k.sb("r32", [128, 2, 128], F32)
    gc = k.sb("gc", [128, 2, 128], F32)
    gcx = k.sb("gcx", [128, 2, 128], F32)
    E = k.sb("E", [128, 2, 128], F32)
    Einv = k.sb("Einv", [128, 2, 128], F32)
    Eex = k.sb("Eex", [128, 2, 128], F32)
    KR2 = [k.sb(f"KR{_i}", [128, 2, 2, 128], BF16) for _i in range(2)]
    Ki32 = k.sb("Ki32", [128, 2, 128], F32)
    Bi32 = k.sb("Bi32", [128, 2, 128], F32)
    Ki = k.sb("Ki", [128, 2, 128], BF16)
    Bi = k.sb("Bi", [128, 2, 128], BF16)
    KeT = k.sb("KeT", [128, 2, 128], BF16)
    nBeT = k.sb("nBeT", [128, 2, 128], BF16)
    dcol2 = [k.sb(f"dcol{_i}", [128, 2, 2], F32) for _i in range(2)]
    ndcol = k.sb("ndcol", [128, 2, 2], F32)
    rk = k.sb("rk", [128, 2, 128], BF16)
    rk32 = k.sb("rk32", [128, 2, 128], F32)
    AT2 = [k.sb(f"AT{_i}", [128, 4, 512], BF16) for _i in range(2)]
    P = [k.sb(f"P{i}", [128, 4, 128], BF16) for i in range(2)]
    PT = [k.sb(f"PT{i}", [128, 4, 128], BF16) for i in range(2)]
    R = [k.sb(f"R{i}", [128, 4, 128], BF16) for i in range(2)]
    Vb2 = [k.sb(f"Vb{_i}", [128, 256], BF16) for _i in range(2)]
    V322 = [k.sb(f"V32{_i}", [128, 256], F32) for _i in range(2)]
    Wt = k.sb("Wt", [128, 256], BF16)
    Ut = k.sb("Ut", [128, 256], BF16)
    k.op("dve", lambda e: e.memset(Wt[:], 0.0), writes=[Wt])
    k.op("dve", lambda e: e.memset(Ut[:], 0.0), writes=[Ut])
    Ke2 = [k.sb(f"Ke{_i}", [128, 256], BF16) for _i in range(2)]
    nBe2 = [k.sb(f"nBe{_i}", [128, 256], BF16) for _i in range(2)]
    bon2 = [k.sb(f"bon{_i}", [128, 4], F32) for _i in range(2)]
    bst = k.sb("bst", [128, 4, 6], F32)
    mv = k.sb("mv", [128, 4, 2], F32)
    sdg = k.sb("sdg", [128, 4], F32)
    rg = k.sb("rg", [128, 4], F32)
    yn = k.sb("yn", [128, 256], F32)
    gate2 = [k.sb(f"gate{_i}", [128, 256], F32) for _i in range(2)]
    Rfin2 = [k.sb(f"Rfin{_i}", [128, 4, 128], BF16) for _i in range(2)]
    yg = k.sb("yg", [128, 256], F32)
    yTb = [k.sb(f"yTb{i}", [128, 2, 512], BF16) for i in range(2)]

    b0 = k.ps("b0", [128, 512], F32)
    b1 = k.ps("b1", [128, 512], F32)
    b2 = k.ps("b2", [128, 512], F32)
    b3 = k.ps("b3", [128, 512], F32)
    b4 = k.ps("b4", [128, 512], F32)
    b5 = k.ps("b5", [128, 512], F32)
    b5b = None
    b6 = k.ps("b6", [128, 512], F32)
    b7 = k.ps("b7", [128, 512], F32)
    b45b = k.ps("b45b", [128, 1024], BF16) if False else None


    def bind(i):
        return (KR2[i % 2], AT2[i % 2], Vb2[i % 2], V322[i % 2], Ke2[i % 2], nBe2[i % 2], dcol2[i % 2], bon2[i % 2],
                gate2[i % 2], Rfin2[i % 2])

    def stageA(i):
        KR, AT, Vb, V32, Ke, nBe, dcol, bon, gate, Rfin = bind(i)
        t0 = i * 128
        hxc = hx[i % 2]
        hxp = hx[(i + 1) % 2]
        rr, mm_, tt0 = i // 16, (i % 16) // 4, (i % 4) * 128
        k.dma("sp", hxc[:], hn_all[mm_][rr].rearrange("(c p) t -> p c t", p=128)[:, :, tt0:tt0 + 128],
              reads=[hnall_t[mm_]], writes=[hxc], sem=hxc.trk.name)
        k.cp("pool", hs[:, :, 1:128], hxc[:, :, 0:127], [hxc], [hs])
        if i > 0:
            k.cp("pool", hs[:, :, 0:1], hxp[:, :, 127:128], [hxp], [hs])
        def proj(out, c0, c1, fm):
            for dc in range(8):
                for (Wx, hh) in ((Wa, hxc), (Wb, hs)):
                    first = dc == 0 and Wx is Wa
                    last = dc == 7 and Wx is Wb
                    if fm:
                        k.mm(out, Wx[:, dc, c0:c1], hh[:, dc, :], first, last, [Wx, hh], [out_b])
                    else:
                        k.mm(out, hh[:, dc, :], Wx[:, dc, c0:c1], first, last, [Wx, hh], [out_b])
        out_b = b0
        for j in range(4):
            proj(b0[:, j * 128:(j + 1) * 128], j * 128, (j + 1) * 128, True)
        yield
        out_b = b1
        proj(b1[:, 0:128], 768, 896, True)
        proj(b1[:, 128:256], 896, 1024, True)
        yield
        out_b = b2
        proj(b2[:, 0:256], 512, 768, False)
        yield
        k.act(l1[0:64, :], b1[0:64, 0:128], AF.Tanh, [b1], [l1])
        k.cp("act", l1[64:128, :], b1[64:128, 0:128], [b1], [l1])
        k.act(sg1[:], b1[:, 128:256], AF.Sigmoid, [b1], [sg1])
        k.cp("act", Vb[:], b2[:, 0:256], [b2], [Vb])
        k.cp("dve", V32[:], b2[:, 0:256], [b2], [V32])
        yield
        for p in range(2):
            k.mm(b3[:, p * 128:(p + 1) * 128], w2b[0:64, p * 128:(p + 1) * 128], l1[0:64, :], True, True, [w2b, l1], [b3])
        for p in range(2):
            k.mm(b4[:, p * 128:(p + 1) * 128], a2b[64:128, p * 128:(p + 1) * 128], l1[64:128, :], True, True,
                 [a2b, l1], [b4])
        k.mm(b2[:, 256:512], sg1[:], g2b[:], True, True, [sg1, g2b], [b2])
        k.cp("act", gate[:], b2[:, 256:512], [b2], [gate])
        for p in range(2):
            k.act(sigw[:, p, :], b3[:, p * 128:(p + 1) * 128], AF.Sigmoid, [b3, cv], [sigw], bias=cv[:, p, 0:1])
            k.act(a_[:, p, :], b4[:, p * 128:(p + 1) * 128], AF.Sigmoid, [b4, cv], [a_], bias=cv[:, p, 1:2])
        yield
        k.cp("act", r32[:], b0[:, 0:256].rearrange("p (a t) -> p a t", a=2), [b0], [r32])
        for p in range(2):
            k.ts("dve", kku[:, p, :], b0[:, 256 + p * 128:256 + (p + 1) * 128], cv[:, p, 2:3], None, ALU.mult, None,
                 [b0, cv], [kku])
        k.tt("pool", sqk[:], kku[:], kku[:], ALU.mult, [kku], [sqk])
        for p in range(2):
            k.mm(b3[:, p * 128:(p + 1) * 128], bones[:], sqk[:, p, :], True, True, [bones, sqk], [b3])
        k.act(sdk[:], b3[:, 0:256].rearrange("p (a t) -> p a t", a=2), AF.Sqrt, [b3, epsk], [sdk], bias=epsk[:], scale=1.0)
        k.recip(rn[:], sdk[:], [sdk], [rn])
        k.tt("dve", kk[:], kku[:], rn[:], ALU.mult, [kku, rn], [kk])
        k.tt("pool", b_[:], kk[:], a_[:], ALU.mult, [kk, a_], [b_])
        for p in range(2):
            k.ts("pool", kmf[:, p, :], a_[:, p, :], cv[:, p, 3:4], omka[:, p:p + 1], ALU.mult, ALU.add, [a_, cv, omka], [kmf])
        k.tt("dve", kmod[:], kmf[:], b0[:, 256:512].rearrange("p (a t) -> p a t", a=2), ALU.mult, [kmf, b0], [kmod])
        yield
        for p in range(2):
            k.op("dve", lambda e, p=p: e.tensor_tensor_scan(out=gc[:, p, :], data0=rmask[:, p, :], data1=sigw[:, p, :],
                                                          initial=0.0, op0=ALU.mult, op1=ALU.add),
                 reads=[rmask, sigw], writes=[gc])
        k.tt("pool", gcx[:], gc[:], sigw[:], ALU.subtract, [gc, sigw], [gcx])
        k.act(E[:], gc[:], AF.Exp, [gc], [E], scale=-CW)
        k.act(Einv[:], gc[:], AF.Exp, [gc], [Einv], scale=CW)
        k.act(Eex[:], gcx[:], AF.Exp, [gcx], [Eex], scale=-CW)
        k.cp("pool", dcol[:], E[:, :, 63:128:64], [E], [dcol])
        k.ts("pool", ndcol[:], dcol[:], -1.0, None, ALU.mult, None, [dcol], [ndcol])
        k.tt("dve", KR[:, :, 0, :], kk[:], Eex[:], ALU.mult, [kk, Eex], [KR])
        k.tt("dve", KR[:, :, 1, :], r32[:], E[:], ALU.mult, [r32, E], [KR])
        k.tt("dve", Ki32[:], kmod[:], Einv[:], ALU.mult, [kmod, Einv], [Ki32])
        k.tt("pool", Bi32[:], b_[:], Einv[:], ALU.mult, [b_, Einv], [Bi32])
        k.cp("act", Ki[:], Ki32[:], [Ki32], [Ki])
        k.cp("act", Bi[:], Bi32[:], [Bi32], [Bi])
        for p in range(2):
            for c in range(2):
                k.ts("dve", KeT[:, p, 64 * c:64 * c + 64], Ki32[:, p, 64 * c:64 * c + 64], dcol[:, p, c:c + 1], None,
                     ALU.mult, None, [Ki32, dcol], [KeT])
                k.ts("pool", nBeT[:, p, 64 * c:64 * c + 64], Bi32[:, p, 64 * c:64 * c + 64], ndcol[:, p, c:c + 1], None,
                     ALU.mult, None, [Bi32, ndcol], [nBeT])
        yield
        k.tt("pool", rk32[:], r32[:], kmod[:], ALU.mult, [r32, kmod], [rk32])
        for p in range(2):
            k.ts("pool", rk[:, p, :], rk32[:, p, :], cv[:, p, 4:5], None, ALU.mult, None, [rk32, cv], [rk])
        yield
        for p in range(2):
            k.mm(b4[:, p * 128:(p + 1) * 128], KeT[:, p, :], identb[:], True, True, [KeT, identb], [b4])
            k.mm(b4[:, 256 + p * 128:256 + (p + 1) * 128], nBeT[:, p, :], identb[:], True, True, [nBeT, identb], [b4])
        k.cp("act", Ke[:], b4[:, 0:256], [b4], [Ke])
        k.cp("act", nBe[:], b4[:, 256:512], [b4], [nBe])
        for p in range(2):
            k.mm(b3[:, 256 + 2 * p:256 + 2 * p + 2], rk[:, p, :], bones[:, 0:128:64], True, True, [rk, bones], [b3])
        k.cp("dve", bon[:], b3[:, 256:260], [b3], [bon])
        yield
        for h in range(4):
            p, q = h // 2, h % 2
            bk = b0 if h % 2 == 0 else b1
            ks = slice(q * 64, (q + 1) * 64)
            k.mm(bk[:, 0:256], Ki[ks, p, :], KR[ks, p, :, :].rearrange("k a t -> k (a t)"), True, True, [Ki, KR], [bk])
            k.mm(bk[:, 256:512], Bi[ks, p, :], KR[ks, p, :, :].rearrange("k a t -> k (a t)"), True, True, [Bi, KR], [bk])
            k.tt("dve", AT[:, h, :], bk[:], mask4[:], ALU.mult, [bk, mask4], [AT])
            k.mm(b4[:, h * 128:(h + 1) * 128], KR[ks, p, 0, :], Bi[ks, p, :], True, True, [KR, Bi], [b4])
            yield
        k.tt("dve", PT[0][:], b4[:].rearrange("p (h t) -> p h t", h=4), maskL4[:], ALU.mult, [b4, maskL4], [PT[0]])
        k.cp("pool", P[0][:], AT[:, :, 256:384], [AT], [P[0]])
        k.tt("pool", R[0][:], ident4[:], P[0][:], ALU.subtract, [ident4, P[0]], [R[0]])
        yield
        for j in range(5):
            pc, pn = P[j % 2], P[(j + 1) % 2]
            ptc, ptn = PT[j % 2], PT[(j + 1) % 2]
            rc, rn_ = R[j % 2], (Rfin if j == 4 else R[(j + 1) % 2])
            for h in range(4):
                k.mm(b0[:, h * 128:(h + 1) * 128], pc[:, h, :], ptc[:, h, :], True, True, [pc, ptc], [b0])
            k.cp("act", ptn[:], b0[:].rearrange("p (h t) -> p h t", h=4), [b0], [ptn])
            if j < 4:
                for h in range(4):
                    k.mm(b1[:, h * 128:(h + 1) * 128], ptc[:, h, :], pc[:, h, :], True, True, [pc, ptc], [b1])
                k.cp("act", pn[:], b1[:].rearrange("p (h t) -> p h t", h=4), [b1], [pn])
            for h in range(4):
                k.mm(b3[:, h * 128:(h + 1) * 128], ptn[:, h, :], rc[:, h, :], True, True, [ptn, rc], [b3])
            k.tt("dve", rn_[:], b3[:].rearrange("p (h t) -> p h t", h=4), rc[:], ALU.add, [b3, rc], [rn_])
            yield
        yield

    def stageB(i):
        nonlocal mcur
        KR, AT, Vb, V32, Ke, nBe, dcol, bon, gate, Rfin = bind(i)
        Rf = Rfin
        for c in range(2):
            cs = slice(64 * c, 64 * c + 64)
            mb = Mb[mcur]
            for h in range(4):
                p, q = h // 2, h % 2
                ks = slice(q * 64, (q + 1) * 64)
                hv = slice(h * 64, (h + 1) * 64)
                k.mm(b6[cs, hv], AT[:, h, 64 * c:64 * c + 64], Vb[:, hv], True, False, [AT, Vb], [b6])
                k.mm(b6[cs, hv], KR[:, p, 0, cs], mb[:, h, :], False, True, [KR, mb], [b6])
            k.cp("act", Wt[cs, :], b6[cs, 0:256], [b6], [Wt])
            yield
            for h in range(4):
                hv = slice(h * 64, (h + 1) * 64)
                k.mm(b6[cs, 256 + h * 64:256 + (h + 1) * 64], Rf[:, h, cs], Wt[:, hv], True, True, [Rf, Wt], [b6])
            k.cp("act", Ut[cs, :], b6[cs, 256:512], [b6], [Ut])
            yield
            for h in range(4):
                p, q = h // 2, h % 2
                ks = slice(q * 64, (q + 1) * 64)
                hv = slice(h * 64, (h + 1) * 64)
                k.mm(b7[cs, hv], KR[:, p, 1, cs], mb[:, h, :], True, False, [KR, mb], [b7])
                k.mm(b7[cs, hv], AT[:, h, 128 + 64 * c:128 + 64 * c + 64], Vb[:, hv], False, False, [AT, Vb], [b7])
                k.mm(b7[cs, hv], AT[:, h, 384 + 64 * c:384 + 64 * c + 64], Ut[:, hv], False, True, [AT, Ut], [b7])
            for h in range(4):
                p, q = h // 2, h % 2
                ks = slice(q * 64, (q + 1) * 64)
                hv = slice(h * 64, (h + 1) * 64)
                k.mm(b5[ks, p * 64:(p + 1) * 64], Ke[cs, hv], Vb[cs, hv], True, False, [Ke, Vb], [b5])
                k.mm(b5[ks, p * 64:(p + 1) * 64], nBe[cs, hv], Ut[cs, hv], False, True, [nBe, Ut], [b5])
            for p in range(2):
                k.stt(M32[:, p, :], M32[:, p, :], dcol[:, p, c:c + 1], b5[:, p * 64:(p + 1) * 64], ALU.mult, ALU.add,
                      [M32, dcol, b5], [M32])
            yield
            mcur = (mcur + 1) % 2
            for q in range(2):
                k.ts("pool", Mb[mcur][:, q:4:2, :], M32[:], hmask[:, q:q + 1], None, ALU.mult, None, [M32, hmask], [Mb[mcur]])
        yield
        for h in range(4):
            k.op("dve", lambda e, h=h: e.bn_stats(out=bst[:, h, :], in_=b7[:, h * 64:(h + 1) * 64]), reads=[b7], writes=[bst])
            k.op("dve", lambda e, h=h: e.bn_aggr(out=mv[:, h, :], in_=bst[:, h, :]), reads=[bst], writes=[mv])
        k.act(sdg[:], mv[:, :, 1], AF.Sqrt, [mv, epsg], [sdg], bias=epsg[:], scale=1.0)
        k.recip(rg[:], sdg[:], [sdg], [rg])
        for h in range(4):
            hv = slice(h * 64, (h + 1) * 64)
            k.ts("dve", yn[:, hv], b7[:, hv], mv[:, h, 0:1], rg[:, h:h + 1], ALU.subtract, ALU.mult, [b7, mv, rg], [yn])
        k.tt("pool", yn[:], yn[:], lngb[:], ALU.mult, [yn, lngb], [yn])
        k.tt("pool", yn[:], yn[:], lnbb[:], ALU.add, [yn, lnbb], [yn])
        for h in range(4):
            hv = slice(h * 64, (h + 1) * 64)
            k.stt(yn[:, hv], V32[:, hv], bon[:, h:h + 1], yn[:, hv], ALU.mult, ALU.add, [V32, bon, yn], [yn])
        k.tt("dve", yg[:], yn[:], gate[:], ALU.mult, [yn, gate], [yg])
        for c2 in range(2):
            k.tr(b5[:, c2 * 128:(c2 + 1) * 128], yg[:, c2 * 128:(c2 + 1) * 128], ident[:], [yg, ident], [b5])
        yb = yTb[(i // 4) % 2]
        j = i % 4
        k.op("act", lambda e, yb=yb, j=j: e.copy(out=yb[:, :, j * 128:(j + 1) * 128],
                                                in_=b5[:, 0:256].rearrange("p (c t) -> p c t", c=2)),
             reads=[b5], writes=[yb])
        if j == 3:
            qr = i // 16
            off = ((i % 16) // 4) * 512
            k.dma("sp", y_loc[qr].rearrange("(c p) t -> p c t", p=128)[:, :, off:off + 512], yb[:, :, :], reads=[yb],
                  writes=[yloc_t[qr]], sem=f"ygb{(i // 4) % 2}")
            if i % 16 == 15:
                gather(qr)
    def drain(g, n=None):
        c = 0
        while n is None or c < n:
            try:
                next(g)
            except StopIteration:
                return False
            c += 1
        return True

    drain(stageA(0))
    for i in range(ntiles):
        gB = stageB(i)
        gA = stageA(i + 1) if i + 1 < ntiles else None
        aliveA, aliveB = gA is not None, True
        while aliveA or aliveB:
            if aliveB:
                aliveB = drain(gB, 1)
            if aliveA:
                aliveA = drain(gA, 4)


def phase_ffn(nc, k, mode, hacc, hacc_t, outT, yall, yall_t, wo, g1c, wg, wu, wd, hin=None, g2c=None, hn_loc=None,
              hnloc_t=None, gather=None, rt=None, rb=None, gout=None, out=None):
    moe = mode == "moe"
    NE, FE = (8, 1408) if moe else (1, 2816)
    G = 4
    xnT = k.sb("xnT", [128, 8, TOK], BF16)
    wob = k.sb("wob", [128, 8, D], BF16)
    wgs = [k.sb(f"wgs{i}", [128, 8, G * 128], BF16) for i in range(2)]
    wus = [k.sb(f"wus{i}", [128, 8, G * 128], BF16) for i in range(2)]
    wds = [k.sb(f"wds{i}", [128, G, D], BF16) for i in range(2)]
    actT = [k.sb(f"actT{i}", [128, G, 512], BF16) for i in range(2)]
    sg = [k.sb(f"sg{i}", [128, 512], F32) for i in range(2)]
    xn32 = [k.sb(f"xn32_{i}", [128, D], F32) for i in range(2)]
    junk = k.sb("junk", [128, D], F32)
    ss = k.sb("ss", [128, NT], F32)
    sd = k.sb("sd", [128, NT], F32)
    rstd = k.sb("rstd", [128, NT], F32)
    g1 = k.sb("g1", [128, 8], F32)
    ident = make_ident(k)
    pso = [k.ps(f"pso{i}", [128, D], F32) for i in range(2)]
    psg = [k.ps(f"psg{i}", [128, 512], F32) for i in range(2)]
    psu = [k.ps(f"psu{i}", [128, 512], F32) for i in range(2)]
    xt_t = [k.trk(f"xt{i}") for i in range(NT)]
    if moe:
        rt32 = k.sb("rt32", [128, 8, NE], F32)
        rbb = k.sb("rbb", [128, NE], F32)
        xT32 = k.sb("xT32", [128, D], F32)
        lg = k.sb("lg", [128, NT, NE], F32)
        gates = k.sb("gates", [128, NT, NE], F32)
        mx = k.sb("mx", [128, 8], F32)
        nm1 = k.sb("nm1", [128, 1], F32)
        ex = k.sb("ex", [128, NE], F32)
        msk = k.sb("msk", [128, NE], F32)
        den = k.sb("den", [128, 1], F32)
        rden = k.sb("rden", [128, 1], F32)
        goutb = k.sb("goutb", [128, D], F32)
        xlo = k.sb("xlo", [128, 8, 128], BF16)
        rth = k.sb("rth", [128, 8, NE], BF16)
        rtl = k.sb("rtl", [128, 8, NE], BF16)
        rtd = k.sb("rtd", [128, 8, NE], F32)
        obuf = [k.sb(f"obuf{i}", [128, D], F32) for i in range(2)]
    else:
        g2 = k.sb("g2", [128, 8], F32)

    for m in range(4):
        def src(e, m=m):
            qd = bass.ds(k.pid_sp, 1)
            return yall[qd].rearrange("o r (h p) t -> p (o r h) t", p=128)[:, :, m * 512:(m + 1) * 512]
        k.dma("sp", xnT[:, :, m * 512:(m + 1) * 512], src, reads=yall_t, writes=xt_t[4 * m:4 * m + 4], sem=f"xt{m}")
    if not moe:
        hin_v = hin.rearrange("(n p) d -> p n d", p=128)
        for m in range(4):
            k.dma("act", hacc[:, 4 * m:4 * m + 4, :], hin_v[:, 4 * m:4 * m + 4, :],
                  writes=hacc_t[4 * m:4 * m + 4], sem=f"hacc{m}")
    for c in range(8):
        r0 = c * 128 if moe else ((c % 2) * 512 + (c // 2) * 128)
        k.dma("pool", wob[:, c, :], wo[r0:r0 + 128, :], writes=[wob])
    k.dma("sp", g1[:], g1c, writes=[g1])
    if moe:
        k.dma("sp", rt32[:], rt.rearrange("(c p) e -> p c e", p=128), writes=[rt32])
        k.dma("sp", rbb[:], rb.partition_broadcast(128), writes=[rbb])
        k.dma("sp", goutb[:], gout.partition_broadcast(128), writes=[goutb])
        k.cp("dve", rth[:], rt32[:], [rt32], [rth])
        k.tt("dve", rtd[:], rt32[:], rth[:], ALU.subtract, [rt32, rth], [rtd])
        k.cp("dve", rtl[:], rtd[:], [rtd], [rtl])
    else:
        k.dma("sp", g2[:], g2c, writes=[g2])

    passes = []
    for e in range(NE):
        nchunk = FE // 128
        c0 = 0
        while c0 < nchunk:
            g = min(G, nchunk - c0)
            passes.append((e, c0, g))
            c0 += g

    def load_pass(pi):
        e, c0, g = passes[pi]
        s = pi % 2
        wg_v = wg[e].rearrange("(c p) f -> p c f", p=128)
        wu_v = wu[e].rearrange("(c p) f -> p c f", p=128)
        wd_v = wd[e].rearrange("(c p) d -> p c d", p=128)
        for c in range(0, 8, 4):
            k.dma("pool", wgs[s][:, c:c + 4, 0:g * 128], wg_v[:, c:c + 4, c0 * 128:(c0 + g) * 128], writes=[wgs[s]])
        for c in range(0, 8, 4):
            k.dma("pool", wus[s][:, c:c + 4, 0:g * 128], wu_v[:, c:c + 4, c0 * 128:(c0 + g) * 128], writes=[wus[s]])
        for c in range(0, g, 2):
            c1 = min(c + 2, g)
            k.dma("pool", wds[s][:, c:c1, :], wd_v[:, c0 + c:c0 + c1, :], writes=[wds[s]])

    load_pass(0)

    for i in range(NT):
        p = pso[i % 2]
        for half in range(2):
            for dc in range(8):
                k.op("pe", lambda e, p=p, half=half, dc=dc, i=i: e.matmul(
                    p[:, half * 512:(half + 1) * 512], lhsT=xnT[:, dc, i * 128:(i + 1) * 128],
                    rhs=wob[:, dc, half * 512:(half + 1) * 512], start=(dc == 0), stop=(dc == 7)),
                    reads=[xt_t[i], wob], writes=[p])
        k.op("dve", lambda e, p=p, i=i: e.tensor_tensor(out=hacc[:, i, :], in0=p[:], in1=hacc[:, i, :], op=ALU.add),
             reads=[p, hacc_t[i]], writes=[hacc_t[i]])

    def norm_stats():
        for i in range(NT):
            k.op("act", lambda e, i=i: e.activation(out=junk[:], in_=hacc[:, i, :], func=AF.Square,
                                                     accum_out=ss[:, i:i + 1]),
                 reads=[hacc_t[i]], writes=[junk, ss])
        k.op("act", lambda e: e.activation(out=sd[:], in_=ss[:], func=AF.Sqrt, scale=1.0 / D, bias=epsb[:]),
             reads=[ss, epsb], writes=[sd])
        k.op("dve", lambda e: e.reciprocal(out=rstd[:], in_=sd[:]), reads=[sd], writes=[rstd])

    epsb = k.sb("epsb", [128, 1], F32)
    k.op("dve", lambda e: e.memset(epsb[:], RMS_EPS), writes=[epsb])

    def norm_transpose(gcol, dst_tile_fn, extra=None):
        for i in range(NT):
            xb = xn32[i % 2]
            p = pso[i % 2]
            k.op("act", lambda e, i=i, xb=xb: e.activation(out=xb[:], in_=hacc[:, i, :], func=AF.Copy,
                                                           scale=rstd[:, i:i + 1]),
                 reads=[hacc_t[i], rstd], writes=[xb])
            for dc in range(8):
                k.op("pe", lambda e, dc=dc, xb=xb, p=p: e.transpose(out=p[:, dc * 128:(dc + 1) * 128],
                                                                  in_=xb[:, dc * 128:(dc + 1) * 128],
                                                                  identity=ident[:]),
                     reads=[xb, ident], writes=[p])
            for dc in range(8):
                k.op("dve", lambda e, dc=dc, i=i, p=p: e.tensor_scalar(
                    out=xnT[:, dc, i * 128:(i + 1) * 128], in0=p[:, dc * 128:(dc + 1) * 128],
                    scalar1=gcol[:, dc:dc + 1], scalar2=None, op0=ALU.mult),
                    reads=[p, gcol], writes=[xt_t[i]])
            if extra is not None:
                extra(i, p)

    norm_stats()

    def router(i, p):
        pl = psg[i % 2]
        for dc in range(8):
            k.stt(xlo[:, dc, :], p[:, dc * 128:(dc + 1) * 128], g1[:, dc:dc + 1], xnT[:, dc, i * 128:(i + 1) * 128],
                  ALU.mult, ALU.subtract, [p, g1, xt_t[i]], [xlo])
        for dc in range(8):
            k.mm(pl[:, 0:NE], xnT[:, dc, i * 128:(i + 1) * 128], rth[:, dc, :], dc == 0, False, [xt_t[i], rth], [pl])
            k.mm(pl[:, 0:NE], xlo[:, dc, :], rth[:, dc, :], False, False, [xlo, rth], [pl])
            k.mm(pl[:, 0:NE], xnT[:, dc, i * 128:(i + 1) * 128], rtl[:, dc, :], False, dc == 7, [xt_t[i], rtl], [pl])
        k.op("dve", lambda e, i=i, pl=pl: e.tensor_tensor(out=lg[:, i, :], in0=pl[:, 0:NE], in1=rbb[:], op=ALU.add),
             reads=[pl, rbb], writes=[lg])
        k.op("dve", lambda e, i=i: e.max(out=mx[:], in_=lg[:, i, :]), reads=[lg], writes=[mx])
        k.op("dve", lambda e: e.tensor_scalar(out=nm1[:], in0=mx[:, 0:1], scalar1=-1.0, scalar2=None, op0=ALU.mult),
             reads=[mx], writes=[nm1])
        k.op("act", lambda e, i=i: e.activation(out=ex[:], in_=lg[:, i, :], func=AF.Exp, bias=nm1[:], scale=1.0),
             reads=[lg, nm1], writes=[ex])
        k.op("dve", lambda e, i=i: e.tensor_scalar(out=msk[:], in0=lg[:, i, :], scalar1=mx[:, 1:2], scalar2=None,
                                                 op0=ALU.is_ge), reads=[lg, mx], writes=[msk])
        k.op("dve", lambda e: e.tensor_tensor(out=ex[:], in0=ex[:], in1=msk[:], op=ALU.mult),
             reads=[ex, msk], writes=[ex])
        k.op("dve", lambda e: e.reduce_sum(out=den[:], in_=ex[:], axis=AX.X), reads=[ex], writes=[den])
        k.op("dve", lambda e: e.reciprocal(out=rden[:], in_=den[:]), reads=[den], writes=[rden])
        k.op("dve", lambda e, i=i: e.tensor_scalar(out=gates[:, i, :], in0=ex[:], scalar1=rden[:, 0:1], scalar2=None,
                                                 op0=ALU.mult), reads=[ex, rden], writes=[gates])

    norm_transpose(g1, None, extra=router if moe else None)

    for pi, (e_idx, c0, g) in enumerate(passes):
        s = pi % 2
        if pi + 1 < len(passes):
            load_pass(pi + 1)
        for m in range(4):
            a = actT[m % 2]
            for c in range(g):
                pg = psg[c % 2]
                pu = psu[c % 2]
                for dc in range(8):
                    k.op("pe", lambda e, pg=pg, s=s, c=c, dc=dc, m=m: e.matmul(
                        pg[:], lhsT=wgs[s][:, dc, c * 128:(c + 1) * 128], rhs=xnT[:, dc, m * 512:(m + 1) * 512],
                        start=(dc == 0), stop=(dc == 7)), reads=[wgs[s]] + xt_t[4 * m:4 * m + 4], writes=[pg])
                for dc in range(8):
                    k.op("pe", lambda e, pu=pu, s=s, c=c, dc=dc, m=m: e.matmul(
                        pu[:], lhsT=wus[s][:, dc, c * 128:(c + 1) * 128], rhs=xnT[:, dc, m * 512:(m + 1) * 512],
                        start=(dc == 0), stop=(dc == 7)), reads=[wus[s]] + xt_t[4 * m:4 * m + 4], writes=[pu])
                sgb = sg[c % 2]
                k.op("act", lambda e, sgb=sgb, pg=pg: e.activation(out=sgb[:], in_=pg[:], func=AF.Silu),
                     reads=[pg], writes=[sgb])
                k.op("dve", lambda e, sgb=sgb, pu=pu, a=a, c=c: e.tensor_tensor(out=a[:, c, :], in0=pu[:], in1=sgb[:],
                                                                             op=ALU.mult),
                     reads=[pu, sgb], writes=[a])
            for tt in range(4):
                i = 4 * m + tt
                p = pso[tt % 2]
                for half in range(2):
                    for c in range(g):
                        k.op("pe", lambda e, p=p, half=half, c=c, a=a, tt=tt, s=s, g=g: e.matmul(
                            p[:, half * 512:(half + 1) * 512], lhsT=a[:, c, tt * 128:(tt + 1) * 128],
                            rhs=wds[s][:, c, half * 512:(half + 1) * 512], start=(c == 0), stop=(c == g - 1)),
                            reads=[a, wds[s]], writes=[p])
                if moe:
                    k.op("dve", lambda e, p=p, i=i, e_idx=e_idx: e.scalar_tensor_tensor(
                        out=hacc[:, i, :], in0=p[:], scalar=gates[:, i, e_idx:e_idx + 1], in1=hacc[:, i, :],
                        op0=ALU.mult, op1=ALU.add), reads=[p, gates, hacc_t[i]], writes=[hacc_t[i]])
                else:
                    k.op("dve", lambda e, p=p, i=i: e.tensor_tensor(out=hacc[:, i, :], in0=p[:], in1=hacc[:, i, :],
                                                                   op=ALU.add),
                         reads=[p, hacc_t[i]], writes=[hacc_t[i]])

    out_v = out.rearrange("(n p) d -> p n d", p=128) if moe else None
    if moe:
        norm_stats()
        for i in range(NT):
            ob = obuf[i % 2]
            k.op("dve", lambda e, i=i, ob=ob: e.scalar_tensor_tensor(
                out=ob[:], in0=hacc[:, i, :], scalar=rstd[:, i:i + 1], in1=goutb[:], op0=ALU.mult, op1=ALU.mult),
                reads=[hacc_t[i], rstd, goutb], writes=[ob])
            k.dma("sp", out_v[:, i, :], ob[:], reads=[ob], writes=[outT], sem=f"obuf{i % 2}")
    else:
        norm_stats()
        norm_transpose(g2, None)
        for m in range(4):
            k.dma("act", hn_loc[m].rearrange("(c p) t -> p c t", p=128), xnT[:, :, m * 512:(m + 1) * 512],
                  reads=xt_t[4 * m:4 * m + 4], writes=[hnloc_t[m]], sem=f"xst{m}")
            gather(m)


GROUPS = [[0, 1, 2, 3], [4, 5, 6, 7]]


def build_fused(upto=4, skip=()):
    nc = bass.Bass("TRN2", target_bir_lowering=False)

    def inp(name, shape, dt=F32):
        return nc.dram_tensor(name, list(shape), dt, kind="ExternalInput").ap()
    m_x = inp("m_x", [T, D]); m_win = inp("m_win", [D, 768]); m_gcol = inp("m_gcol", [128, 8])
    m_wsT = inp("m_wsT", [128, 128]); m_gng = inp("m_gng", [128]); m_bsc = inp("m_bsc", [128, 1])
    m_lbl = inp("m_lbl", [128, 2]); m_ong = inp("m_ong", [128])
    f_hin = inp("f_hin", [TOK, D]); f_wo = inp("f_wo", [D, D]); f_g1c = inp("f_g1c", [128, 8])
    f_wg = inp("f_wg", [1, D, 2816]); f_wu = inp("f_wu", [1, D, 2816]); f_wd = inp("f_wd", [1, 2816, D])
    f_g2c = inp("f_g2c", [128, 8])
    r_mixc = inp("r_mixc", [128, 8, 6]); r_wproj = inp("r_wproj", [D, 1024]); r_w2 = inp("r_w2", [64, 256])
    r_a2 = inp("r_a2", [64, 256]); r_g2 = inp("r_g2", [128, 256]); r_cvec = inp("r_cvec", [128, 2, 5])
    r_lng = inp("r_lng", [256]); r_lnb = inp("r_lnb", [256])
    e_wo = inp("e_wo", [D, D]); e_g1c = inp("e_g1c", [128, 8])
    e_wg = inp("e_wg", [8, D, 1408]); e_wu = inp("e_wu", [8, D, 1408]); e_wd = inp("e_wd", [8, 1408, D])
    e_rt = inp("e_rt", [D, 8]); e_rb = inp("e_rb", [8]); e_gout = inp("e_gout", [D])
    out = nc.dram_tensor("out", [TOK, D], F32, kind="ExternalOutput").ap()
    y_loc = nc.dram_tensor("y_loc", [4, 256, 2048], BF16).ap()
    y_all = nc.dram_tensor("y_all", [4, 4, 256, 2048], BF16).ap()
    hn_loc = nc.dram_tensor("hn_loc", [4, 1024, 512], BF16).ap()
    hn_all = nc.dram_tensor("hn_all", [4, 4, 1024, 512], BF16).ap()
    yg_loc = y_loc
    yg_all = y_all

    k = KB(nc)
    hacc = k.sb("hacc", [128, NT, D], F32, glob=True)
    hacc_t = [k.trk(f"hacc{i}") for i in range(NT)]
    outT = k.trk("outT")
    yloc_t = [k.trk(f"yloc{i}") for i in range(4)]
    yall_t = [k.trk(f"yall{i}") for i in range(4)]
    hnloc_t = [k.trk(f"hnloc{i}") for i in range(4)]
    hnall_t = [k.trk(f"hnall{i}") for i in range(4)]
    ygloc_t = [k.trk(f"ygloc{i}") for i in range(4)]
    ygall_t = [k.trk(f"ygall{i}") for i in range(4)]

    def mk_gather(loc, allb, loc_t, all_t, pat, nm):
        def gather(i):
            if nm in _NOCOLL:
                return
            k.coll(lambda e: e.collective_compute("AllGather", ALU.bypass, replica_groups=GROUPS,
                                                  ins=[loc[i].opt()], outs=[allb[i].rearrange(pat).opt()]),
                   f"{nm}{i}", reads=[loc_t[i]], writes=[all_t[i]])
        return gather

    if 1 not in skip:
     k.push("m_")
     phase_mix0(nc, k, m_x, m_win, m_gcol, m_wsT, m_gng, m_bsc, m_lbl, m_ong, y_loc, yloc_t,
               mk_gather(y_loc, y_all, yloc_t, yall_t, "r c t -> (r c) t", "ga"))
     k.pop()
    if upto >= 2 and 2 not in skip:
      k.push("f_")
      phase_ffn(nc, k, "ffn", hacc, hacc_t, outT, y_all, yall_t, f_wo, f_g1c, f_wg, f_wu, f_wd, hin=f_hin, g2c=f_g2c,
              hn_loc=hn_loc, hnloc_t=hnloc_t, gather=mk_gather(hn_loc, hn_all, hnloc_t, hnall_t, "r c t -> (r c) t", "gb"))
      k.pop()
    if upto >= 3:
      k.push("r_")
      phase_rwkv(nc, k, hn_all, hnall_t, r_mixc, r_wproj, r_w2, r_a2, r_g2, r_cvec, r_lng, r_lnb, yg_loc, ygloc_t,
               mk_gather(yg_loc, yg_all, ygloc_t, ygall_t, "r c t -> (r c) t", "gc"))
      k.pop()
    if upto >= 4:
      k.push("e_")
      phase_ffn(nc, k, "moe", hacc, hacc_t, outT, yg_all, ygall_t, e_wo, e_g1c, e_wg, e_wu, e_wd, rt=e_rt, rb=e_rb,
              gout=e_gout, out=out)
      k.wait_all("sp", [outT])
      k.pop()
    k.emit()
    k.close()
    return nc


_UPTO = 4
_NOCOLL = ()
_SKIP = ()
def _gc(g):
    return np.ascontiguousarray(np.asarray(g, np.float32).reshape(8, 128).T)


_NC = {}


def kernel(x, norm_mix_g, norm_ffn_g, norm_out_g,
           mix_w_in, mix_w_out, gmlp_norm_g, gmlp_w_s, gmlp_b_s, hgrn_lb_logits, hgrn_onorm_g,
           ffn_w_gate, ffn_w_up, ffn_w_down,
           rwkv_mix, rwkv_w_r, rwkv_w_k, rwkv_w_v, rwkv_w_o, rwkv_w0, rwkv_w1, rwkv_w2,
           rwkv_a0, rwkv_a1, rwkv_a2, rwkv_g1, rwkv_g2, rwkv_k_k, rwkv_k_a, rwkv_r_k,
           rwkv_ln_g, rwkv_ln_b,
           moe_router, moe_router_b, moe_w_gate, moe_w_up, moe_w_down, _trace=False):
    f32 = np.float32
    A = lambda a: np.ascontiguousarray(np.asarray(a, f32))
    x = A(x)
    xf = x.reshape(16384, 1024)
    cores = list(range(8))
    w = np.asarray(mix_w_in, f32)[0]
    colb = [0 * 512, 1 * 512, 4 * 512, 5 * 512, 2 * 512, 3 * 512]
    mixc = A(np.asarray(rwkv_mix, f32)[0].reshape(6, 8, 128).transpose(2, 1, 0))
    shared = dict(
        m_gcol=_gc(norm_mix_g[0]), m_ong=A(np.asarray(hgrn_onorm_g)[0]),
        f_wo=A(mix_w_out[0]), f_g1c=_gc(norm_ffn_g[0]), f_wg=A(ffn_w_gate), f_wu=A(ffn_w_up), f_wd=A(ffn_w_down),
        f_g2c=_gc(norm_mix_g[1]),
        r_mixc=mixc,
        e_wo=A(rwkv_w_o[0]), e_g1c=_gc(norm_ffn_g[1]), e_wg=A(moe_w_gate[0]), e_wu=A(moe_w_up[0]), e_wd=A(moe_w_down[0]),
        e_rt=A(moe_router[0]), e_rb=A(moe_router_b[0]), e_gout=A(norm_out_g))
    in_maps = []
    for c in cores:
        b, j = c // 4, c % 4
        cs = slice(256 * j, 256 * (j + 1))
        win = np.concatenate([w[:, cb + j * 128: cb + (j + 1) * 128] for cb in colb], axis=1)
        wproj = np.concatenate([np.asarray(rwkv_w_r)[0][:, cs], np.asarray(rwkv_w_k)[0][:, cs],
                                np.asarray(rwkv_w_v)[0][:, cs], np.asarray(rwkv_w1)[0], np.asarray(rwkv_a1)[0],
                                np.asarray(rwkv_g1)[0]], axis=1)
        pv = lambda v: np.asarray(v, f32).reshape(-1)[cs].reshape(2, 128).T
        cvec = np.stack([pv(rwkv_w0[0]), pv(rwkv_a0[0]), pv(rwkv_k_k[0]), pv(rwkv_k_a[0]), pv(rwkv_r_k[0])], axis=-1)
        m = dict(shared)
        m.update(m_x=x[b], m_win=A(win), m_wsT=A(np.asarray(gmlp_w_s)[0, j].T), m_gng=A(np.asarray(gmlp_norm_g)[0, j]),
                 m_bsc=A(np.asarray(gmlp_b_s)[0, j].reshape(128, 1)),
                 m_lbl=A(np.asarray(hgrn_lb_logits)[:, j * 128:(j + 1) * 128].T),
                 f_hin=A(xf[c * 2048:(c + 1) * 2048]),
                 r_wproj=A(wproj), r_w2=A(np.asarray(rwkv_w2)[0][:, cs]), r_a2=A(np.asarray(rwkv_a2)[0][:, cs]),
                 r_g2=A(np.asarray(rwkv_g2)[0][:, cs]), r_cvec=A(cvec),
                 r_lng=A(np.asarray(rwkv_ln_g)[0][cs]), r_lnb=A(np.asarray(rwkv_ln_b)[0][cs]))
        in_maps.append(m)
    if "nc" not in _NC:
        _NC["nc"] = build_fused(_UPTO, _SKIP)
    res = run_bass_kernel_spmd(_NC["nc"], in_maps, core_ids=cores, **({"trace": True} if _trace else {}))
    if _trace:
        print("exec_time_ns", res.exec_time_ns)
    out = np.concatenate([res.results[c]["out"] for c in cores], axis=0).reshape(2, 8192, 1024)
    return np.ascontiguousarray(out.astype(np.float32))
```
